# Optimizing a Trainium2 kernel written in Bass

```python
import jax
import jax.numpy as jnp
from jax import lax
import numpy as np

D_MODEL = 2048
BATCH = 4
SEQ = 4096
DEPTH = 4

GRID_W = 64
CTX_LEN = 256
EPS = 1e-6
N_MOD = 6

MLA_HEADS = 8
MLA_Q_RANK = 512
MLA_KV_RANK = 256
MLA_NOPE = 128
MLA_ROPE = 64
MLA_V = 128
MLA_QK = MLA_NOPE + MLA_ROPE
ROPE_THETA = 10000.0
Q_BLOCK = 128

GDN_HEADS = 8
GDN_DK = 128
GDN_DV = 128
GDN_CONV = 5
GDN_CHUNK = 64

MLA_WIDTH = MLA_HEADS * MLA_V
GDN_WIDTH = GDN_HEADS * GDN_DV
MIX_WIDTH = MLA_WIDTH + GDN_WIDTH

P_Q_LAT = MLA_Q_RANK
P_KV_LAT = MLA_KV_RANK
P_K_PE = MLA_ROPE
P_GDN_QKV = GDN_HEADS * (2 * GDN_DK + GDN_DV)
P_GDN_Z = GDN_WIDTH
P_GDN_BETA = 2 * GDN_HEADS
P_GDN_ALPHA = 2 * GDN_HEADS
IN_WIDTH = P_Q_LAT + P_KV_LAT + P_K_PE + P_GDN_QKV + P_GDN_Z + P_GDN_BETA + P_GDN_ALPHA
SPLIT_POINTS = (P_Q_LAT,
                P_Q_LAT + P_KV_LAT,
                P_Q_LAT + P_KV_LAT + P_K_PE,
                P_Q_LAT + P_KV_LAT + P_K_PE + P_GDN_QKV,
                P_Q_LAT + P_KV_LAT + P_K_PE + P_GDN_QKV + P_GDN_Z,
                P_Q_LAT + P_KV_LAT + P_K_PE + P_GDN_QKV + P_GDN_Z + P_GDN_BETA)

N_EXPERTS = 64
N_GROUPS = 8
EXPERTS_PER_GROUP = N_EXPERTS // N_GROUPS
TOPK_GROUPS = 4
TOP_K = 8
EXPERT_FF = 256
SHARED_FF = 256
ROUTED_SCALE = 2.5
DISPATCH_BLOCK = 128

kernel_name = 'hybrid_mla_gdn_moe_dit'


def rmsnorm(x, g):
    xf = x.astype(jnp.float32)
    y = xf * lax.rsqrt(jnp.mean(xf * xf, axis=-1, keepdims=True) + EPS)
    return (y * g.astype(jnp.float32)).astype(x.dtype)


def l2norm(x):
    return x * lax.rsqrt(jnp.sum(x * x, axis=-1, keepdims=True) + EPS)


def modulate(h, shift, scale):
    return h * (1 + scale) + shift


def axial_rope_tables(rows):
    r = jnp.repeat(jnp.arange(rows, dtype=jnp.float32), GRID_W)
    col = jnp.tile(jnp.arange(GRID_W, dtype=jnp.float32), rows)
    half = MLA_ROPE // 2
    inv = jnp.power(ROPE_THETA, -jnp.arange(0, half, 2, dtype=jnp.float32) / half)
    ar = r[:, None] * inv
    ac = col[:, None] * inv
    ang = jnp.concatenate([ar, ar, ac, ac], axis=-1)
    return jnp.cos(ang), jnp.sin(ang)


def apply_axial_rope(x, cos, sin):
    a, b, cq, d = jnp.split(x, 4, axis=-1)
    rot = jnp.concatenate([-b, a, -d, cq], axis=-1)
    return (x * cos + rot * sin).astype(x.dtype)


def mla_q(q_lat, q_norm, w_q_up, rope):
    B, L, _ = q_lat.shape
    q = (rmsnorm(q_lat, q_norm) @ w_q_up).reshape(B, L, MLA_HEADS, MLA_QK)
    q_nope, q_pe = q[..., :MLA_NOPE], q[..., MLA_NOPE:]
    if rope is not None:
        cos, sin = rope
        q_pe = apply_axial_rope(q_pe, cos[:, None, :], sin[:, None, :])
    return jnp.concatenate([q_nope, q_pe], axis=-1)


def mla_kv(kv_lat, k_pe, kv_norm, w_kv_up, rope):
    B, L, _ = kv_lat.shape
    kv = (rmsnorm(kv_lat, kv_norm) @ w_kv_up).reshape(B, L, MLA_HEADS, MLA_NOPE + MLA_V)
    k_nope, v = kv[..., :MLA_NOPE], kv[..., MLA_NOPE:]
    if rope is not None:
        cos, sin = rope
        k_pe = apply_axial_rope(k_pe, cos, sin)
    k_pe = jnp.broadcast_to(k_pe[:, :, None, :], (B, L, MLA_HEADS, MLA_ROPE))
    return jnp.concatenate([k_nope, k_pe], axis=-1), v


def softmax_attend(q, k, v):
    s = jnp.einsum('bqhd,bkhd->bhqk', q, k, preferred_element_type=jnp.float32) * (MLA_QK ** -0.5)
    p = jax.nn.softmax(s, axis=-1).astype(v.dtype)
    return jnp.einsum('bhqk,bkhd->bqhd', p, v)


def latent_attention(q, k_all, v_all):
    B, S, H, Dq = q.shape
    nb = S // Q_BLOCK
    qb = q.reshape(B, nb, Q_BLOCK, H, Dq).swapaxes(0, 1)
    o = lax.map(lambda qi: softmax_attend(qi, k_all, v_all), qb)
    return o.swapaxes(0, 1).reshape(B, S, MLA_WIDTH)


def centred_conv(u, w):
    ch = u.shape[-1]
    pad = (GDN_CONV - 1) // 2
    return lax.conv_general_dilated(u, w[:, None, :].astype(u.dtype), window_strides=(1,),
                                    padding=[(pad, pad)], dimension_numbers=('NWC', 'WIO', 'NWC'),
                                    feature_group_count=ch)


def gdn_prepare(qkv, beta_raw, alpha_raw, conv_w, a_log, dt_bias):
    B, L, _ = qkv.shape
    u = jax.nn.silu(centred_conv(qkv, conv_w)).astype(jnp.float32)
    q, k, v = jnp.split(u, [GDN_HEADS * GDN_DK, 2 * GDN_HEADS * GDN_DK], axis=-1)
    q = l2norm(q.reshape(B, L, GDN_HEADS, GDN_DK)) * (GDN_DK ** -0.5)
    k = l2norm(k.reshape(B, L, GDN_HEADS, GDN_DK))
    v = v.reshape(B, L, GDN_HEADS, GDN_DV)
    beta = jax.nn.sigmoid(beta_raw.astype(jnp.float32)).reshape(B, L, 2, GDN_HEADS)
    g = -jnp.exp(a_log.astype(jnp.float32)) * jax.nn.softplus(
        alpha_raw.astype(jnp.float32).reshape(B, L, 2, GDN_HEADS) + dt_bias.astype(jnp.float32))
    return q, k, v, beta, g


def gated_delta_chunked(q, k, v, g, beta, state0):
    B, L, H, DK = q.shape
    DV = v.shape[-1]
    NC = L // GDN_CHUNK

    def chunks(t):
        return t.reshape(B, NC, GDN_CHUNK, H, -1).transpose(1, 0, 3, 2, 4)

    qc, kc, vc = chunks(q), chunks(k), chunks(v)
    gc = jnp.cumsum(g.reshape(B, NC, GDN_CHUNK, H).transpose(1, 0, 3, 2), axis=-1)
    bc = beta.reshape(B, NC, GDN_CHUNK, H).transpose(1, 0, 3, 2)
    incl = jnp.tril(jnp.ones((GDN_CHUNK, GDN_CHUNK), dtype=bool))
    strict = jnp.tril(jnp.ones((GDN_CHUNK, GDN_CHUNK), dtype=bool), -1)
    diff = gc[..., :, None] - gc[..., None, :]
    decay = jnp.where(incl, jnp.exp(jnp.where(incl, diff, 0.0)), 0.0)
    kb = kc * bc[..., None]
    a = jnp.where(strict, jnp.einsum('nbhid,nbhjd->nbhij', kb, kc) * decay, 0.0)
    t_sys = a + jnp.eye(GDN_CHUNK, dtype=a.dtype)
    u = lax.linalg.triangular_solve(t_sys, vc * bc[..., None], left_side=True, lower=True)
    w = lax.linalg.triangular_solve(t_sys, kb * jnp.exp(gc)[..., None], left_side=True, lower=True)
    qk = jnp.einsum('nbhid,nbhjd->nbhij', qc, kc) * decay

    def step(state, xs):
        q_i, k_i, u_i, w_i, qk_i, g_i = xs
        v_new = u_i - jnp.einsum('bhck,bhkv->bhcv', w_i, state)
        o_i = (jnp.einsum('bhck,bhkv->bhcv', q_i * jnp.exp(g_i)[..., None], state)
               + jnp.einsum('bhij,bhjv->bhiv', qk_i, v_new))
        g_last = g_i[..., -1:]
        k_dec = k_i * jnp.exp(g_last - g_i)[..., None]
        state = state * jnp.exp(g_last)[..., None] + jnp.einsum('bhck,bhcv->bhkv', k_dec, v_new)
        return state, o_i

    state, o = lax.scan(step, state0, (qc, kc, u, w, qk, gc))
    return state, o.transpose(1, 0, 3, 2, 4).reshape(B, L, H, DV)


def bidirectional_gdn(prep_ctx, prep_lat, need_ctx_out):
    qc, kc, vc, bc, gc = prep_ctx
    ql, kl, vl, bl, gl = prep_lat
    B = ql.shape[0]
    zero = jnp.zeros((B, GDN_HEADS, GDN_DK, GDN_DV), jnp.float32)

    def rev(t):
        return jnp.flip(t, axis=1)

    sc_f, oc_f = gated_delta_chunked(qc, kc, vc, gc[:, :, 0], bc[:, :, 0], zero)
    _, ol_f = gated_delta_chunked(ql, kl, vl, gl[:, :, 0], bl[:, :, 0], sc_f)
    sc_b, oc_b = gated_delta_chunked(rev(qc), rev(kc), rev(vc), rev(gc[:, :, 1]), rev(bc[:, :, 1]), zero)
    _, ol_b = gated_delta_chunked(rev(ql), rev(kl), rev(vl), rev(gl[:, :, 1]), rev(bl[:, :, 1]), sc_b)
    o_lat = ol_f + rev(ol_b)
    o_ctx = oc_f + rev(oc_b) if need_ctx_out else None
    return o_lat, o_ctx


def gdn_gated_norm(o, z, norm_w):
    B, L = o.shape[:2]
    y = rmsnorm(o, norm_w) * jax.nn.silu(z.astype(jnp.float32).reshape(B, L, GDN_HEADS, GDN_DV))
    return y.reshape(B, L, GDN_WIDTH)


def moe_ffn(h, router_w, router_bias, w_gate, w_up, w_down, ws_gate, ws_up, ws_down):
    T, D = h.shape
    scores = jax.nn.sigmoid(jnp.matmul(h, router_w, preferred_element_type=jnp.float32))
    sel = scores + router_bias.astype(jnp.float32)
    grp_score = lax.top_k(sel.reshape(T, N_GROUPS, EXPERTS_PER_GROUP), 2)[0].sum(-1)
    _, top_groups = lax.top_k(grp_score, TOPK_GROUPS)
    group_mask = jnp.any(top_groups[:, :, None] == jnp.arange(N_GROUPS)[None, None, :], axis=1)
    expert_mask = jnp.repeat(group_mask, EXPERTS_PER_GROUP, axis=1)
    _, idx = lax.top_k(jnp.where(expert_mask, sel, -jnp.inf), TOP_K)
    wts = jnp.take_along_axis(scores, idx, axis=1)
    wts = wts / jnp.sum(wts, axis=-1, keepdims=True) * ROUTED_SCALE

    n_pairs = T * TOP_K
    e_flat = idx.reshape(n_pairs)
    tok_flat = jnp.arange(n_pairs, dtype=jnp.int32) // TOP_K
    order = jnp.argsort(e_flat)
    e_sorted = e_flat[order]
    counts = jax.ops.segment_sum(jnp.ones((n_pairs,), jnp.int32), e_flat, num_segments=N_EXPERTS)
    padded = (counts + DISPATCH_BLOCK - 1) // DISPATCH_BLOCK * DISPATCH_BLOCK
    pad_end = jnp.cumsum(padded)
    pad_start = pad_end - padded
    cnt_start = jnp.cumsum(counts) - counts
    dest = pad_start[e_sorted] + jnp.arange(n_pairs, dtype=jnp.int32) - cnt_start[e_sorted]
    n_blocks = -(-n_pairs // DISPATCH_BLOCK) + N_EXPERTS
    n_slots = n_blocks * DISPATCH_BLOCK
    slot_tok = jnp.zeros((n_slots,), jnp.int32).at[dest].set(tok_flat[order])
    slot_w = jnp.zeros((n_slots,), jnp.float32).at[dest].set(wts.reshape(n_pairs)[order])
    block_start = jnp.arange(n_blocks, dtype=jnp.int32) * DISPATCH_BLOCK
    block_e = jnp.minimum(jnp.sum(pad_end[None, :] <= block_start[:, None], axis=1), N_EXPERTS - 1)

    def expert_block(y, blk):
        tok, wt, e = blk
        xb = h[tok]
        hid = jax.nn.silu(xb @ w_gate[e]) * (xb @ w_up[e])
        return y.at[tok].add((hid @ w_down[e]) * wt[:, None].astype(h.dtype)), None

    routed, _ = lax.scan(expert_block, jnp.zeros_like(h),
                         (slot_tok.reshape(n_blocks, DISPATCH_BLOCK), slot_w.reshape(n_blocks, DISPATCH_BLOCK), block_e))
    shared = (jax.nn.silu(h @ ws_gate) * (h @ ws_up)) @ ws_down
    return routed + shared


def setup_inputs(seed: int = 0) -> dict:
    key = jax.random.key(seed)
    ks = jax.random.split(key, 32)
    f32 = jnp.float32
    L = DEPTH
    D = D_MODEL

    def nrm(k, shape, fan_in, gain=1.0):
        return jax.random.normal(k, shape, f32) * (gain * fan_in ** -0.5)

    def gain_vec(k, shape):
        return 1.0 + 0.05 * jax.random.normal(k, shape, f32)

    dt = jax.random.uniform(ks[17], (L, 2, GDN_HEADS), f32, 0.001, 0.1)
    return {
        'x': jax.random.normal(ks[0], (BATCH, SEQ, D), f32),
        'c': jax.random.normal(ks[1], (BATCH, D), f32),
        'ctx': jax.random.normal(ks[2], (BATCH, CTX_LEN, D), f32),
        'c_ctx': jax.random.normal(ks[3], (D,), f32),
        'ada_w': nrm(ks[4], (L, D, N_MOD * D), D, 0.2),
        'ada_b': 0.02 * jax.random.normal(ks[5], (L, N_MOD * D), f32),
        'norm_mix_pre': gain_vec(ks[6], (L, D)),
        'norm_mix_post': gain_vec(ks[7], (L, D)),
        'norm_ffn_pre': gain_vec(ks[8], (L, D)),
        'norm_ffn_post': gain_vec(ks[9], (L, D)),
        'w_in': nrm(ks[10], (L, D, IN_WIDTH), D),
        'mla_q_norm': gain_vec(ks[11], (L, MLA_Q_RANK)),
        'mla_w_q_up': nrm(ks[12], (L, MLA_Q_RANK, MLA_HEADS * MLA_QK), MLA_Q_RANK),
        'mla_kv_norm': gain_vec(ks[13], (L, MLA_KV_RANK)),
        'mla_w_kv_up': nrm(ks[14], (L, MLA_KV_RANK, MLA_HEADS * (MLA_NOPE + MLA_V)), MLA_KV_RANK),
        'gdn_conv': nrm(ks[15], (L, GDN_CONV, P_GDN_QKV), GDN_CONV),
        'gdn_a_log': jnp.log(jax.random.uniform(ks[16], (L, 2, GDN_HEADS), f32, 1.0, 16.0)),
        'gdn_dt_bias': jnp.log(jnp.expm1(dt)),
        'gdn_norm': gain_vec(ks[18], (L, GDN_DV)),
        'w_out': nrm(ks[19], (L, MIX_WIDTH, D), MIX_WIDTH),
        'router_w': nrm(ks[20], (L, D, N_EXPERTS), D),
        'router_bias': 0.01 * jax.random.normal(ks[21], (L, N_EXPERTS), f32),
        'exp_w_gate': nrm(ks[22], (L, N_EXPERTS, D, EXPERT_FF), D),
        'exp_w_up': nrm(ks[23], (L, N_EXPERTS, D, EXPERT_FF), D),
        'exp_w_down': nrm(ks[24], (L, N_EXPERTS, EXPERT_FF, D), EXPERT_FF),
        'sh_w_gate': nrm(ks[25], (L, D, SHARED_FF), D),
        'sh_w_up': nrm(ks[26], (L, D, SHARED_FF), D),
        'sh_w_down': nrm(ks[27], (L, SHARED_FF, D), SHARED_FF),
    }


def reference(x, c, ctx, c_ctx, ada_w, ada_b, norm_mix_pre, norm_mix_post, norm_ffn_pre, norm_ffn_post,
              w_in, mla_q_norm, mla_w_q_up, mla_kv_norm, mla_w_kv_up, gdn_conv, gdn_a_log, gdn_dt_bias,
              gdn_norm, w_out, router_w, router_bias, exp_w_gate, exp_w_up, exp_w_down,
              sh_w_gate, sh_w_up, sh_w_down):
    B, S, D = x.shape
    C = ctx.shape[1]
    rows = S // GRID_W
    rope = axial_rope_tables(rows)
    xc = ctx
    for i in range(DEPTH):
        last = i == DEPTH - 1
        mod = jax.nn.silu(c) @ ada_w[i] + ada_b[i]
        mod_c = jax.nn.silu(c_ctx) @ ada_w[i] + ada_b[i]
        sh1, sc1, g1, sh2, sc2, g2 = [m[:, None, :] for m in jnp.split(mod, N_MOD, axis=-1)]
        sh1c, sc1c, g1c, sh2c, sc2c, g2c = jnp.split(mod_c, N_MOD, axis=-1)

        h = modulate(rmsnorm(x, norm_mix_pre[i]), sh1, sc1)
        hc = modulate(rmsnorm(xc, norm_mix_pre[i]), sh1c, sc1c)
        pl = jnp.split(h @ w_in[i], SPLIT_POINTS, axis=-1)
        pc = jnp.split(hc @ w_in[i], SPLIT_POINTS, axis=-1)

        k_c, v_c = mla_kv(pc[1], pc[2], mla_kv_norm[i], mla_w_kv_up[i], None)
        k_l, v_l = mla_kv(pl[1], pl[2], mla_kv_norm[i], mla_w_kv_up[i], rope)
        q_l = mla_q(pl[0], mla_q_norm[i], mla_w_q_up[i], rope)
        attn_l = latent_attention(q_l, jnp.concatenate([k_c, k_l], axis=1), jnp.concatenate([v_c, v_l], axis=1))

        prep_c = gdn_prepare(pc[3], pc[5], pc[6], gdn_conv[i], gdn_a_log[i], gdn_dt_bias[i])
        prep_l = gdn_prepare(pl[3], pl[5], pl[6], gdn_conv[i], gdn_a_log[i], gdn_dt_bias[i])
        o_l, o_c = bidirectional_gdn(prep_c, prep_l, not last)
        gdn_l = gdn_gated_norm(o_l, pl[4], gdn_norm[i]).astype(x.dtype)

        y = jnp.concatenate([attn_l, gdn_l], axis=-1) @ w_out[i]
        x = x + g1 * rmsnorm(y, norm_mix_post[i])
        if not last:
            q_c = mla_q(pc[0], mla_q_norm[i], mla_w_q_up[i], None)
            attn_c = softmax_attend(q_c, k_c, v_c).reshape(B, C, MLA_WIDTH)
            gdn_c = gdn_gated_norm(o_c, pc[4], gdn_norm[i]).astype(xc.dtype)
            yc = jnp.concatenate([attn_c, gdn_c], axis=-1) @ w_out[i]
            xc = xc + g1c * rmsnorm(yc, norm_mix_post[i])

        h2 = modulate(rmsnorm(x, norm_ffn_pre[i]), sh2, sc2).reshape(B * S, D)
        if not last:
            h2c = modulate(rmsnorm(xc, norm_ffn_pre[i]), sh2c, sc2c).reshape(B * C, D)
            tokens = jnp.concatenate([h2, h2c], axis=0)
        else:
            tokens = h2
        f = moe_ffn(tokens, router_w[i], router_bias[i], exp_w_gate[i], exp_w_up[i], exp_w_down[i],
                    sh_w_gate[i], sh_w_up[i], sh_w_down[i])
        x = x + g2 * rmsnorm(f[:B * S].reshape(B, S, D), norm_ffn_post[i])
        if not last:
            xc = xc + g2c * rmsnorm(f[B * S:].reshape(B, C, D), norm_ffn_post[i])
    return x
```

```python
import contextlib
import numpy as np
import concourse.bass as bass
import concourse.mybir as mybir
from concourse.bass_utils import run_bass_kernel_spmd

F32 = mybir.dt.float32
ALU = mybir.AluOpType
AF = mybir.ActivationFunctionType
AX = mybir.AxisListType

ENGS = ['pe', 'act', 'dve', 'pool', 'sp']

D = 2048
BATCH = 4
SEQ = 4096
DEPTH = 4
GRID_W = 64
CTX = 256
EPS = 1e-6
H = 8
QR = 512
KVR = 256
NOPE = 128
ROPE = 64
VD = 128
QK = NOPE + ROPE
DK = 128
DV = 128
CONV = 5
NE = 64
EFF = 256
IN_W = 4960
O_Q, O_KV, O_KPE, O_QKV, O_Z, O_BA = 0, 512, 768, 832, 3904, 4928
TOK = 2176
NT = 17
BTOK = CTX + SEQ
BLOCKS = [(0, 256), (256, 512), (768, 512), (1280, 512), (1792, 384)]
NB = len(BLOCKS)


class Buf:
    __slots__ = ('name', 'w', 'r', 'excl')

    def __init__(self, name=''):
        self.name = name
        self.w = None
        self.r = {}
        self.excl = False


class V:
    __slots__ = ('tile', 'ap')

    def __init__(self, tile, ap):
        self.tile = tile
        self.ap = ap


class T:
    def __init__(self, t, name, buf=None, track=True):
        self.t = t
        self.name = name
        self.b = buf if buf is not None else Buf(name)
        self.track = track

    def __getitem__(self, idx):
        return V(self, self.t[idx])

    def re(self, pat, **kw):
        base = self.t if hasattr(self.t, 'rearrange') else self.t[:]
        return T(base.rearrange(pat, **kw), self.name, self.b, self.track)


def _tiles(vs):
    out = []
    for v in vs:
        if isinstance(v, V) and v.tile.track and v.tile not in out:
            out.append(v.tile)
    return out


def _ap(x):
    return x.ap if isinstance(x, V) else x


class Prog:
    def __init__(self, nc, same_engine_sync=True):
        self.nc = nc
        self.ops = {e: [] for e in ENGS}
        self.cnt = {}
        self.seen = {e: {} for e in ENGS}
        self.same = same_engine_sync
        self.st = contextlib.ExitStack()
        self.rr = 0

    def sb(self, name, shape, dtype=F32):
        return T(self.st.enter_context(self.nc.sbuf_tensor(name, list(shape), dtype)), name)

    def ps(self, name, shape, dtype=F32):
        t = T(self.st.enter_context(self.nc.psum_tensor(name, list(shape), dtype)), name)
        t.b.excl = True
        return t

    def dram(self, name, shape, kind, dtype=F32, track=False):
        return T(self.nc.dram_tensor(name, list(shape), dtype, kind=kind).ap(), name, track=track)

    def op(self, eng, fn, reads=(), writes=(), dma=None):
        waits = {}
        seen = self.seen[eng]

        def need(dep):
            if dep is None:
                return
            k, v = dep
            if k == eng and (eng == 'pe' or not self.same):
                return
            if seen.get(k, 0) >= v:
                return
            if waits.get(k, 0) < v:
                waits[k] = v

        for t in reads:
            need(t.b.w)
            if t.b.excl:
                for k, v in t.b.r.items():
                    if k != eng:
                        need((k, v))
        for t in writes:
            need(t.b.w)
            for k, v in t.b.r.items():
                need((k, v))
        for k, v in waits.items():
            seen[k] = v
        key = dma if dma else eng
        inc = 16 if dma else 1
        c = self.cnt.get(key, 0) + inc
        self.cnt[key] = c
        for t in reads:
            t.b.r[key] = c
        for t in writes:
            t.b.w = (key, c)
            t.b.r = {}
        self.ops[eng].append((list(waits.items()), fn, key, inc))

    def dma(self, eng, out, in_):
        sbside = in_.tile if out.tile.name.startswith('dr_') else out.tile
        key = 'd_' + sbside.name + '_' + eng
        o, i = out.ap, in_.ap
        self.op(eng, lambda e: e.dma_start(out=o, in_=i), _tiles([in_]), _tiles([out]), dma=key)

    def dq(self):
        self.rr += 1
        return ('sp', 'pool')[self.rr % 2]

    def mm(self, out, lhsT, rhs, start=True, stop=True):
        o, l, r = out.ap, lhsT.ap, rhs.ap
        self.op('pe', lambda e: e.matmul(o, l, r, start=start, stop=stop), _tiles([lhsT, rhs]), _tiles([out]))

    def transpose(self, out, in_, ident):
        self.mm(out, in_, ident)

    def act(self, out, in_, func, bias=None, scale=1.0, eng='act'):
        o, i, b, s = out.ap, in_.ap, _ap(bias), _ap(scale)
        if b is None:
            self.op('act', lambda e: e.activation(out=o, in_=i, func=func, scale=s), _tiles([in_, scale]), _tiles([out]))
        else:
            self.op('act', lambda e: e.activation(out=o, in_=i, func=func, bias=b, scale=s),
                    _tiles([in_, bias, scale]), _tiles([out]))

    def copy(self, eng, out, in_):
        o, i = out.ap, in_.ap
        if eng == 'act':
            self.op('act', lambda e: e.copy(out=o, in_=i), _tiles([in_]), _tiles([out]))
        else:
            self.op(eng, lambda e: e.tensor_copy(out=o, in_=i), _tiles([in_]), _tiles([out]))

    def tt(self, eng, out, in0, in1, op):
        o, a, b = out.ap, in0.ap, in1.ap
        self.op(eng, lambda e: e.tensor_tensor(out=o, in0=a, in1=b, op=op), _tiles([in0, in1]), _tiles([out]))

    def ts(self, eng, out, in0, s1, s2, op0, op1=None):
        o, a, x1, x2 = out.ap, in0.ap, _ap(s1), _ap(s2)
        if op1 is None:
            self.op(eng, lambda e: e.tensor_scalar(out=o, in0=a, scalar1=x1, scalar2=None, op0=op0),
                    _tiles([in0, s1]), _tiles([out]))
        else:
            self.op(eng, lambda e: e.tensor_scalar(out=o, in0=a, scalar1=x1, scalar2=x2, op0=op0, op1=op1),
                    _tiles([in0, s1, s2]), _tiles([out]))

    def stt(self, eng, out, in0, scalar, in1, op0, op1):
        o, a, s, b = out.ap, in0.ap, _ap(scalar), in1.ap
        self.op(eng, lambda e: e.scalar_tensor_tensor(out=o, in0=a, scalar=s, in1=b, op0=op0, op1=op1),
                _tiles([in0, scalar, in1]), _tiles([out]))

    def memset(self, eng, out, val):
        o = out.ap
        self.op(eng, lambda e: e.memset(o, val), [], _tiles([out]))

    def recip(self, out, in_):
        o, i = out.ap, in_.ap
        self.op('dve', lambda e: e.reciprocal(out=o, in_=i), _tiles([in_]), _tiles([out]))

    def max8(self, out, in_):
        o, i = out.ap, in_.ap
        self.op('dve', lambda e: e.max(out=o, in_=i), _tiles([in_]), _tiles([out]))

    def reduce(self, eng, out, in_, op, axis=AX.X):
        o, i = out.ap, in_.ap
        self.op(eng, lambda e: e.tensor_reduce(out=o, in_=i, axis=axis, op=op), _tiles([in_]), _tiles([out]))

    def finish(self):
        waits = [(k, v) for k, v in self.cnt.items() if k.startswith('d_')]
        self.ops['sp'].append((waits, None, None, 0))
        self.emit()

    def emit(self):
        nc = self.nc
        keys = list(self.cnt.keys())
        with self.st as st:
            sems = {k: st.enter_context(nc.semaphore("s_" + k)) for k in keys}
            block = st.enter_context(nc.Block())

            def run(e):
                def f(engobj):
                    for waits, fn, key, inc in self.ops[e]:
                        for k, v in waits:
                            engobj.wait_ge(sems[k], v)
                        if fn is not None:
                            fn(engobj).then_inc(sems[key], inc)
                return f

            block.tensor(run('pe'))
            block.scalar(run('act'))
            block.vector(run('dve'))
            block.gpsimd(run('pool'))
            block.sync(run('sp'))


def new_nc():
    return bass.Bass("TRN2", target_bir_lowering=False)


class PsumPool:
    def __init__(self, p, n=8):
        self.tiles = [p.ps("psb%d" % i, [128, 512]) for i in range(n)]
        self.i = 0

    def get(self):
        t = self.tiles[self.i % len(self.tiles)]
        self.i += 1
        return t


class Ring:
    def __init__(self, tiles):
        self.tiles = tiles
        self.i = 0

    def get(self):
        t = self.tiles[self.i % len(self.tiles)]
        self.i += 1
        return t


def rstd_from_ps(p, ps, N, rstd, dim):
    p.ts('dve', rstd[:, 0:N], ps[:, 0:N], 1.0 / dim, EPS, ALU.mult, ALU.add)
    p.act(rstd[:, 0:N], rstd[:, 0:N], AF.Sqrt)
    p.recip(rstd[:, 0:N], rstd[:, 0:N])


def rms_rstd(p, pp, src, c0, nch, N, ones, sq, rstd, dim):
    p.act(sq[:, 0:nch, 0:N], src[:, c0:c0 + nch, 0:N], AF.Square)
    ps = pp.get()
    for c in range(nch):
        p.mm(ps[:, 0:N], ones[:, :], sq[:, c, 0:N], c == 0, c == nch - 1)
    rstd_from_ps(p, ps, N, rstd, dim)


IN_, OUT_ = "ExternalInput", "ExternalOutput"


def build_s1():
    nc = new_nc()
    p = Prog(nc)
    xT = p.dram("dr_xT", [D, TOK], IN_).re("(c p) n -> p c n", p=128)
    gpre = p.dram("dr_gpre", [128, 16], IN_)
    msc = p.dram("dr_msc", [128, NB, 16], IN_)
    msh = p.dram("dr_msh", [128, NB, 16], IN_)
    w_in = p.dram("dr_w_in", [D, IN_W], IN_).re("(c p) n -> p c n", p=128)
    gq = p.dram("dr_gq", [128, 4], IN_)
    wq = p.dram("dr_wq", [QR, H * QK], IN_).re("(c p) n -> p c n", p=128)
    gkv = p.dram("dr_gkv", [128, 2], IN_)
    wkv = p.dram("dr_wkv", [KVR, 2 * H * 128], IN_).re("(c p) n -> p c n", p=128)
    cosT = p.dram("dr_cosT", [64, TOK], IN_)
    sinT = p.dram("dr_sinT", [64, TOK], IN_)
    rmat = p.dram("dr_rmat", [64, 64], IN_)
    o_qn = p.dram("dr_o_qn", [H, 128, TOK], OUT_)
    o_qp = p.dram("dr_o_qp", [H, 64, TOK], OUT_)
    o_kn = p.dram("dr_o_kn", [H, 128, TOK], OUT_)
    o_kp = p.dram("dr_o_kp", [64, TOK], OUT_)
    o_v = p.dram("dr_o_v", [TOK, H * 128], OUT_)
    o_qkv = p.dram("dr_o_qkv", [24, 128, TOK], OUT_)
    o_z = p.dram("dr_o_z", [8, 128, TOK], OUT_)
    o_ba = p.dram("dr_o_ba", [TOK, 32], OUT_)

    pp = PsumPool(p)
    ones = p.sb("ones", [128, 128])
    xb = p.sb("xb", [128, 16, 512])
    hb = p.sb("hb", [128, 16, 512])
    wt = Ring([p.sb("wt%d" % i, [128, 16, 320]) for i in range(2)])
    rstd = p.sb("rstd", [128, 512])
    A_t = p.sb("A_t", [128, NB, 16])
    S_t = p.sb("S_t", [128, NB, 16])
    gpre_t = p.sb("gpre_t", [128, 16])
    gq_t = p.sb("gq_t", [128, 4])
    gkv_t = p.sb("gkv_t", [128, 2])
    wq_t = p.sb("wq_t", [128, 4, H * QK])
    wkv_t = p.sb("wkv_t", [128, 2, 2 * H * 128])
    cos_t = p.sb("cos_t", [64, 512])
    sin_t = p.sb("sin_t", [64, 512])
    rm_t = p.sb("rm_t", [64, 64])
    lat = p.sb("lat", [128, 7, 512])
    latn = p.sb("latn", [128, 6, 512])
    lsq = xb
    rq = p.sb("rq", [128, 512])
    stg = Ring([p.sb("stg%d" % i, [128, 512]) for i in range(4)])
    pe_a = p.sb("pe_a", [64, 512])
    pe_b = p.sb("pe_b", [64, 512])
    pe_c = Ring([p.sb("pe_c%d" % i, [64, 512]) for i in range(2)])
    vst = Ring([p.sb("vst%d" % i, [128, 512]) for i in range(2)])
    bast = Ring([p.sb("bast%d" % i, [128, 32]) for i in range(2)])

    p.memset('pool', ones[:, :], 1.0)
    p.dma('sp', gpre_t[:, :], gpre[:, :])
    p.dma('sp', A_t[:, :, :], msc[:, :, :])
    p.dma('sp', S_t[:, :, :], msh[:, :, :])
    p.dma('sp', gq_t[:, :], gq[:, :])
    p.dma('sp', gkv_t[:, :], gkv[:, :])
    p.dma('sp', rm_t[:, :], rmat[:, :])
    p.dma('pool', wq_t[:, :, :], wq[:, :, :])
    p.dma('pool', wkv_t[:, :, :], wkv[:, :, :])
    for b in range(NB):
        p.stt('dve', A_t[:, b, :], A_t[:, b, :], 1.0, gpre_t[:, :], ALU.add, ALU.mult)

    groups = [(0, 256), (256, 256), (512, 320)] + [(O_QKV + i * 256, 256) for i in range(12)] + \
             [(O_Z + i * 256, 256) for i in range(4)] + [(O_BA, 32)]

    def rope(src, N, dst):
        p.copy('act', pe_a[:, 0:N], src)
        ps2 = pp.get()
        p.mm(ps2[0:64, 0:N], rm_t[:, :], pe_a[:, 0:N])
        p.tt('dve', pe_b[:, 0:N], ps2[0:64, 0:N], sin_t[:, 0:N], ALU.mult)
        pc = pe_c.get()
        p.tt('pool', pc[:, 0:N], pe_a[:, 0:N], cos_t[:, 0:N], ALU.mult)
        p.tt('dve', pc[:, 0:N], pc[:, 0:N], pe_b[:, 0:N], ALU.add)
        p.dma(p.dq(), dst, pc[:, 0:N])

    def mla_up(t0, N):
        rms_rstd(p, pp, lat, 0, 4, N, ones, lsq, rq, QR)
        for c in range(4):
            p.stt('dve', latn[:, c, 0:N], lat[:, c, 0:N], gq_t[:, c:c + 1], rq[:, 0:N], ALU.mult, ALU.mult)
        rms_rstd(p, pp, lat, 4, 2, N, ones, lsq, rq, KVR)
        for c in range(2):
            p.stt('dve', latn[:, 4 + c, 0:N], lat[:, 4 + c, 0:N], gkv_t[:, c:c + 1], rq[:, 0:N], ALU.mult, ALU.mult)
        rope(lat[0:64, 6, 0:N], N, o_kp[:, t0:t0 + N])
        for h in range(H):
            ps = pp.get()
            for kc in range(4):
                p.mm(ps[:, 0:N], wq_t[:, kc, h * 128:(h + 1) * 128], latn[:, kc, 0:N], kc == 0, kc == 3)
            s = stg.get()
            p.copy('act', s[:, 0:N], ps[:, 0:N])
            p.dma(p.dq(), o_qn[h, :, t0:t0 + N], s[:, 0:N])
            ps = pp.get()
            for kc in range(4):
                p.mm(ps[0:64, 0:N], wq_t[:, kc, H * 128 + h * 64:H * 128 + (h + 1) * 64], latn[:, kc, 0:N],
                     kc == 0, kc == 3)
            rope(ps[0:64, 0:N], N, o_qp[h, :, t0:t0 + N])
        for h in range(H):
            ps = pp.get()
            for kc in range(2):
                p.mm(ps[:, 0:N], wkv_t[:, kc, h * 128:(h + 1) * 128], latn[:, 4 + kc, 0:N], kc == 0, kc == 1)
            s = stg.get()
            p.copy('dve', s[:, 0:N], ps[:, 0:N])
            p.dma(p.dq(), o_kn[h, :, t0:t0 + N], s[:, 0:N])
        for ti in range(N // 128):
            for half in range(2):
                vs = vst.get()
                ps = pp.get()
                for kc in range(2):
                    p.mm(ps[:, :], latn[:, 4 + kc, ti * 128:(ti + 1) * 128],
                         wkv_t[:, kc, H * 128 + half * 512:H * 128 + (half + 1) * 512], kc == 0, kc == 1)
                p.copy('act', vs[:, :], ps[:, :])
                p.dma(p.dq(), o_v[t0 + ti * 128:t0 + (ti + 1) * 128, half * 512:(half + 1) * 512], vs[:, :])

    def do_block(bi, t0, N):
        p.dma('sp', xb[:, :, 0:N], xT[:, :, t0:t0 + N])
        p.dma('pool', cos_t[:, 0:N], cosT[:, t0:t0 + N])
        p.dma('pool', sin_t[:, 0:N], sinT[:, t0:t0 + N])
        rms_rstd(p, pp, xb, 0, 16, N, ones, hb, rstd, D)
        for c in range(16):
            p.stt('dve', hb[:, c, 0:N], xb[:, c, 0:N], A_t[:, bi, c:c + 1], rstd[:, 0:N], ALU.mult, ALU.mult)
            p.act(hb[:, c, 0:N], hb[:, c, 0:N], AF.Identity, bias=S_t[:, bi, c:c + 1])
        for (c0, cw) in groups:
            w = wt.get()
            p.dma(p.dq(), w[:, :, 0:cw], w_in[:, :, c0:c0 + cw])
            if c0 == O_BA:
                for ti in range(N // 128):
                    ps = pp.get()
                    for kc in range(16):
                        p.mm(ps[:, 0:32], hb[:, kc, ti * 128:(ti + 1) * 128], w[:, kc, 0:32], kc == 0, kc == 15)
                    bs = bast.get()
                    p.copy('act', bs[:, :], ps[:, 0:32])
                    p.dma('sp', o_ba[t0 + ti * 128:t0 + (ti + 1) * 128, :], bs[:, :])
                continue
            for m in range((cw + 127) // 128):
                mw = min(128, cw - m * 128)
                ps = pp.get()
                for kc in range(16):
                    p.mm(ps[0:mw, 0:N], w[:, kc, m * 128:m * 128 + mw], hb[:, kc, 0:N], kc == 0, kc == 15)
                col = c0 + m * 128
                if col < O_QKV:
                    p.copy('act', lat[0:mw, col // 128, 0:N], ps[0:mw, 0:N])
                else:
                    s = stg.get()
                    p.copy('act' if m % 2 == 0 else 'dve', s[:, 0:N], ps[:, 0:N])
                    if col < O_Z:
                        p.dma(p.dq(), o_qkv[(col - O_QKV) // 128, :, t0:t0 + N], s[:, 0:N])
                    else:
                        p.dma(p.dq(), o_z[(col - O_Z) // 128, :, t0:t0 + N], s[:, 0:N])
            if c0 == 512:
                mla_up(t0, N)

    for bi, (t0, N) in enumerate(BLOCKS):
        do_block(bi, t0, N)
    p.finish()
    return nc


MASK_BIG = 1.0e5


def build_s2():
    nc = new_nc()
    p = Prog(nc)
    qn = p.dram("dr_qn", [H, 128, TOK], IN_)
    qp = p.dram("dr_qp", [H, 65, TOK], IN_)
    kn = p.dram("dr_kn", [H, 128, BTOK], IN_)
    kp = p.dram("dr_kp", [65, BTOK], IN_)
    v = p.dram("dr_v", [BTOK, H * 128], IN_).re("(kt p) n -> p kt n", p=128)
    o_at = p.dram("dr_o_at", [H, 128, TOK], OUT_)
    NKT = BTOK // 128
    st_ps = Ring([p.ps("st_ps%d" % i, [128, 512]) for i in range(4)])
    ot_ps = Ring([p.ps("ot_ps%d" % i, [128, 512]) for i in range(2)])
    su_ps = Ring([p.ps("su_ps%d" % i, [128, 512]) for i in range(2)])
    ones = p.sb("ones", [128, 128])
    kn_t = Ring([p.sb("kn_t%d" % i, [128, BTOK]) for i in range(2)])
    kp_t = p.sb("kp_t", [65, BTOK])
    v_t = Ring([p.sb("v_t%d" % i, [128, NKT, 128]) for i in range(2)])
    qn_t = Ring([p.sb("qn_t%d" % i, [128, TOK]) for i in range(2)])
    qp_t = Ring([p.sb("qp_t%d" % i, [65, TOK]) for i in range(2)])
    pt = Ring([p.sb("pt%d" % i, [128, 512]) for i in range(4)])
    rs = p.sb("rs", [128, 512])
    og = Ring([p.sb("og%d" % i, [128, 512]) for i in range(2)])
    p.memset('pool', ones[:, :], 1.0)
    p.dma('sp', kp_t[:, :], kp[:, :])
    scale = float(QK) ** -0.5
    for h in range(H):
        knh, vh, qnh, qph = kn_t.get(), v_t.get(), qn_t.get(), qp_t.get()
        p.dma('sp', knh[:, :], kn[h, :, :])
        p.dma('pool', vh[:, :, :], v[:, :, h * 128:(h + 1) * 128])
        p.dma('sp', qnh[:, :], qn[h, :, :])
        p.dma('pool', qph[:, :], qp[h, :, :])
        for (t0, N) in BLOCKS:
            ot, su = ot_ps.get(), su_ps.get()
            for kt in range(NKT):
                st = st_ps.get()
                p.mm(st[:, 0:N], knh[:, kt * 128:(kt + 1) * 128], qnh[:, t0:t0 + N], True, False)
                p.mm(st[:, 0:N], kp_t[:, kt * 128:(kt + 1) * 128], qph[:, t0:t0 + N], False, True)
                pk = pt.get()
                p.act(pk[:, 0:N], st[:, 0:N], AF.Exp, scale=scale)
                p.mm(ot[:, 0:N], vh[:, kt, :], pk[:, 0:N], kt == 0, kt == NKT - 1)
                p.mm(su[:, 0:N], ones[:, :], pk[:, 0:N], kt == 0, kt == NKT - 1)
            p.recip(rs[:, 0:N], su[:, 0:N])
            o = og.get()
            p.tt('dve', o[:, 0:N], ot[:, 0:N], rs[:, 0:N], ALU.mult)
            p.dma(p.dq(), o_at[h, :, t0:t0 + N], o[:, 0:N])
    p.finish()
    return nc


NPAIR = 4
NTB = BTOK // 128
SEGS = [(0, CTX), (CTX, BTOK)]
FWD_ORDER = list(range(NTB))
BWD_ORDER = [1, 0] + list(range(NTB - 1, 1, -1))


class QPsum:
    def __init__(self, p, nbanks=8):
        self.q = []
        for i in range(nbanks):
            bank = p.ps("qb%d" % i, [128, 512])
            for j in range(4):
                self.q.append(T(bank.t[:, j * 128:(j + 1) * 128], "qb%d_%d" % (i, j), buf=bank.b))
        self.i = 0

    def get(self):
        t = self.q[self.i % len(self.q)]
        self.i += 1
        return t


def build_s3(npair=NPAIR, nsteps=NTB, dbg=0):
    nc = new_nc()
    p = Prog(nc)
    raw = p.dram("dr_raw", [NPAIR, 3, 128, BTOK], IN_)
    cw = p.dram("dr_cw", [NPAIR, 128, 3, 5], IN_)
    alog = p.dram("dr_alog", [128, 16], IN_)
    dtb = p.dram("dr_dtb", [128, 16], IN_)
    cst = p.dram("dr_cst", [8, 128, 128], IN_)
    o_g = p.dram("dr_o_g", [NPAIR, 128, BTOK], OUT_)
    hsel = p.dram("dr_hsel", [128, NTB, NPAIR * 4], IN_)
    qp_ = QPsum(p, 7)
    big = PsumPool(p, 1)
    C = p.sb("C", [128, 8, 128])
    ones = p.sb("ones", [128, 128])
    p.dma('sp', C[:, :, :], cst.re("c p n -> p c n")[:, :, :])
    p.memset('pool', ones[:, :], 1.0)
    PEm = [C[:, 0, :], C[:, 2, :]]
    PSm = [C[:, 1, :], C[:, 3, :]]
    BD, CH, ident = C[:, 4, :], [C[:, 5, :], C[:, 6, :]], C[:, 7, :]
    bat = p.sb("bat", [128, NTB, NPAIR * 4])
    p.dma('sp', bat[:, :, :], hsel[:, :, :])
    alog_t = p.sb("alog_t", [128, 16])
    dtb_t = p.sb("dtb_t", [128, 16])
    p.dma('sp', alog_t[:, :], alog[:, :])
    p.dma('sp', dtb_t[:, :], dtb[:, :])
    nea = p.sb("nea", [128, 16])
    p.act(nea[:, :], alog_t[:, :], AF.Exp)
    p.ts('dve', nea[:, :], nea[:, :], -1.0, None, ALU.mult)
    gb = p.sb("gb", [128, NTB, NPAIR * 4])
    for t in range(NTB):
        p.tt('pool', gb[:, t, :], bat[:, t, :], dtb_t[:, :], ALU.add)
    p.act(gb[:, :, :], gb[:, :, :], AF.Exp)
    p.act(gb[:, :, :], gb[:, :, :], AF.Ln, bias=1.0)
    for t in range(NTB):
        p.tt('pool', gb[:, t, :], gb[:, t, :], nea[:, :], ALU.mult)
    sig = p.sb("sig", [128, NTB, NPAIR * 4])
    p.act(sig[:, :, :], bat[:, :, :], AF.Sigmoid)

    rawt = Ring([p.sb("rawt%d" % i, [128, BTOK]) for i in range(2)])
    cv = p.sb("cv", [128, 3, BTOK])
    cwt = p.sb("cwt", [128, 3, 5])
    sq = p.sb("sq", [128, 512])
    rn = p.sb("rn", [128, 512])
    oT = p.sb("oT", [128, BTOK])
    vec = {}
    for d in range(2):
        for nm in ("gc", "bg", "kd", "nb", "be", "g"):
            vec[(nm, d)] = p.sb("v_%s%d" % (nm, d), [128, NTB])
        vec[("egl", d)] = p.sb("v_egl%d" % d, [128, NTB, 2])
    S = [p.sb("S%d" % d, [128, 128]) for d in range(2)]

    def ring(nm, n=2):
        return [Ring([p.sb("%s%d_%d" % (nm, d, i), [128, 128]) for i in range(n)]) for d in range(2)]

    r_gpe, r_dm, r_dmt, r_egcb, r_t1, r_t2 = ring("gpe"), ring("dm"), ring("dmt"), ring("egcb"), ring("t1"), ring("t2")
    r_M, r_Mt, r_Pt = ring("M", 3), ring("Mt", 3), ring("Pt", 3)
    r_qkdt, r_kbg, r_kdec, r_vb, r_wT, r_u, r_qgT, r_vnew = (ring("qkdt"), ring("kbg"), ring("kdec"), ring("vb"),
                                                            ring("wT"), ring("u"), ring("qgT"), ring("vnew"))

    def prep_pair(pi):
        p.dma('sp', cwt[:, :, :], cw[pi, :, :, :])
        for w3 in range(3):
            r = rawt.get()
            p.dma(p.dq(), r[:, :], raw[pi, w3, :, :])
            eng = 'dve'
            p.ts(eng, cv[:, w3, :], r[:, :], cwt[:, w3, 2:3], None, ALU.mult)
            for s in (-2, -1, 1, 2):
                for (a, b) in SEGS:
                    lo, hi = max(a, a - s), min(b, b - s)
                    p.stt(eng, cv[:, w3, lo:hi], r[:, lo + s:hi + s], cwt[:, w3, s + 2:s + 3], cv[:, w3, lo:hi],
                          ALU.mult, ALU.add)
            p.act(cv[:, w3, :], cv[:, w3, :], AF.Silu)
        for w3 in range(2):
            for c0 in range(0, BTOK, 512):
                n = min(512, BTOK - c0)
                p.act(sq[:, 0:n], cv[:, w3, c0:c0 + n], AF.Square)
                ps = big.get()
                p.mm(ps[:, 0:n], ones[:, :], sq[:, 0:n])
                if w3 == 0:
                    p.ts('dve', rn[:, 0:n], ps[:, 0:n], EPS, float(DK), ALU.add, ALU.mult)
                else:
                    p.ts('dve', rn[:, 0:n], ps[:, 0:n], EPS, None, ALU.add)
                p.act(rn[:, 0:n], rn[:, 0:n], AF.Sqrt)
                p.recip(rn[:, 0:n], rn[:, 0:n])
                p.tt('dve', cv[:, w3, c0:c0 + n], cv[:, w3, c0:c0 + n], rn[:, 0:n], ALU.mult)
        p.memset('pool', oT[:, :], 0.0)
        for d in range(2):
            cb = pi * 4 + d * 2
            g, be = vec[("g", d)], vec[("be", d)]
            p.copy('pool', g[:, :], gb[:, :, cb + 1])
            p.copy('pool', be[:, :], sig[:, :, cb])
            ps = qp_.get()
            p.mm(ps[:, 0:NTB], PEm[d], g[:, :])
            gc = vec[("gc", d)]
            p.copy('act', gc[:, :], ps[:, 0:NTB])
            ps2 = qp_.get()
            p.mm(ps2[:, 0:NTB], BD, g[:, :])
            kd = vec[("kd", d)]
            p.tt('dve', kd[:, :], ps2[:, 0:NTB], gc[:, :], ALU.subtract)
            p.act(kd[:, :], kd[:, :], AF.Exp)
            bg = vec[("bg", d)]
            p.act(bg[:, :], gc[:, :], AF.Exp)
            p.tt('dve', bg[:, :], bg[:, :], be[:, :], ALU.mult)
            p.ts('dve', vec[("nb", d)][:, :], be[:, :], -1.0, None, ALU.mult)
            egl = vec[("egl", d)]
            for c in range(2):
                ps3 = qp_.get()
                p.mm(ps3[:, 0:NTB], CH[c], g[:, :])
                p.act(egl[:, :, c], ps3[:, 0:NTB], AF.Exp)
            p.memset('pool', S[d][:, :], 0.0)

    def tile_dir(d, t):
        c0 = t * 128
        kT, qT, vT = cv[:, 1, c0:c0 + 128], cv[:, 0, c0:c0 + 128], cv[:, 2, c0:c0 + 128]
        g, be, nb = vec[("g", d)], vec[("be", d)], vec[("nb", d)]
        bg, kd, egl = vec[("bg", d)], vec[("kd", d)], vec[("egl", d)]
        gpe = r_gpe[d].get()
        p.ts('pool', gpe[:, :], PEm[d], g[:, t:t + 1], None, ALU.mult)
        ps_d, ps_dt, ps_gb = qp_.get(), qp_.get(), qp_.get()
        p.mm(ps_d[:, :], gpe[:, :], PSm[d])
        p.mm(ps_dt[:, :], PSm[d], gpe[:, :])
        p.mm(ps_gb[:, :], ones[:, :], gpe[:, :])
        dm, dmt, egcb = r_dm[d].get(), r_dmt[d].get(), r_egcb[d].get()
        p.act(dm[:, :], ps_d[:, :], AF.Exp)
        p.act(dmt[:, :], ps_dt[:, :], AF.Exp)
        p.act(egcb[:, :], ps_gb[:, :], AF.Exp)
        if dbg == 1:
            return
        ps_G, ps_KQ = qp_.get(), qp_.get()
        p.mm(ps_G[:, :], kT, kT)
        p.mm(ps_KQ[:, :], kT, qT)
        if dbg == 12:
            return
        t1, t2 = r_t1[d].get(), r_t2[d].get()
        p.tt('dve', t1[:, :], dm[:, :], PSm[d], ALU.mult)
        p.tt('dve', t2[:, :], dmt[:, :], PEm[d], ALU.mult)
        if dbg == 13:
            return
        M = r_M[d].get()
        p.stt('dve', M[:, :], ps_G[:, :], nb[:, t:t + 1], t1[:, :], ALU.mult, ALU.mult)
        if dbg == 14:
            return
        qkdt = r_qkdt[d].get()
        p.tt('dve', qkdt[:, :], ps_KQ[:, :], t2[:, :], ALU.mult)
        if dbg == 2:
            return
        ps_t = qp_.get()
        p.transpose(ps_t[:, :], M[:, :], ident)
        Mt = r_Mt[d].get()
        p.copy('act', Mt[:, :], ps_t[:, :])
        Pt = r_Pt[d].get()
        p.tt('dve', Pt[:, :], Mt[:, :], ident, ALU.add)
        if dbg == 21:
            return
        for lv in range(5 if dbg != 22 else 1):
            last = lv == 4
            ps_a = qp_.get()
            p.mm(ps_a[:, :], Mt[:, :], M[:, :])
            if dbg == 23:
                return
            if not last:
                ps_b = qp_.get()
                p.mm(ps_b[:, :], M[:, :], Mt[:, :])
            if dbg == 24:
                return
            M2 = r_M[d].get()
            p.copy('act', M2[:, :], ps_a[:, :])
            if dbg == 25:
                return
            if not last:
                Mt2 = r_Mt[d].get()
                p.copy('act' if dbg in (26, 27) else 'dve', Mt2[:, :], ps_b[:, :])
            if dbg in (26, 28):
                return
            ps_c = qp_.get()
            p.mm(ps_c[:, :], M2[:, :], Pt[:, :])
            Pt2 = r_Pt[d].get()
            p.tt('dve', Pt2[:, :], ps_c[:, :], Pt[:, :], ALU.add)
            M, Pt = M2, Pt2
            if not last:
                Mt = Mt2
        Xt = Pt
        if dbg == 3:
            return
        ps_k, ps_v = qp_.get(), qp_.get()
        p.transpose(ps_k[:, :], kT, ident)
        p.transpose(ps_v[:, :], vT, ident)
        kbg, kdec, vb = r_kbg[d].get(), r_kdec[d].get(), r_vb[d].get()
        p.ts('dve', kbg[:, :], ps_k[:, :], bg[:, t:t + 1], None, ALU.mult)
        p.act(kdec[:, :], ps_k[:, :], AF.Copy, scale=kd[:, t:t + 1])
        p.act(vb[:, :], ps_v[:, :], AF.Copy, scale=be[:, t:t + 1])
        ps_w, ps_u = qp_.get(), qp_.get()
        p.mm(ps_w[:, :], kbg[:, :], Xt[:, :])
        p.mm(ps_u[:, :], Xt[:, :], vb[:, :])
        wT, u, qgT = r_wT[d].get(), r_u[d].get(), r_qgT[d].get()
        p.copy('act', wT[:, :], ps_w[:, :])
        p.copy('dve', u[:, :], ps_u[:, :])
        p.tt('dve', qgT[:, :], cv[:, 0, c0:c0 + 128], egcb[:, :], ALU.mult)
        if dbg == 4:
            return
        vnew = r_vnew[d].get()
        for c in ((0, 1) if d == 0 else (1, 0)):
            r0, r1 = c * 64, c * 64 + 64
            ps_ws = qp_.get()
            p.mm(ps_ws[:, :], wT[:, :], S[d][:, :])
            p.tt('dve', vnew[r0:r1, :], u[r0:r1, :], ps_ws[r0:r1, :], ALU.subtract)
            ps_o = qp_.get()
            p.mm(ps_o[:, 0:64], S[d][:, :], qgT[:, r0:r1], True, False)
            p.mm(ps_o[:, 0:64], vnew[r0:r1, :], qkdt[r0:r1, r0:r1], False, True)
            p.tt('dve', oT[:, c0 + r0:c0 + r1], oT[:, c0 + r0:c0 + r1], ps_o[:, 0:64], ALU.add)
            ps_s = qp_.get()
            p.mm(ps_s[:, :], kdec[r0:r1, :], vnew[r0:r1, :])
            p.stt('dve', S[d][:, :], S[d][:, :], egl[:, t, c:c + 1], ps_s[:, :], ALU.mult, ALU.add)

    for pi in range(npair):
        prep_pair(pi)
        for s in range(nsteps):
            tile_dir(0, FWD_ORDER[s])
            tile_dir(1, BWD_ORDER[s])
        p.dma('sp', o_g[pi, :, :], oT[:, :])
    p.finish()
    return nc


BLOCKS4 = [(0, 256)] + [(256 + i * 384, 384) for i in range(5)]
NB4 = len(BLOCKS4)
NEX = NE + 1
ROUTED_SCALE = 2.5


def build_s4(nexp=NEX):
    nc = new_nc()
    p = Prog(nc)
    attn = p.dram("dr_attn", [H, 128, TOK], IN_).re("h p n -> p h n")
    og = p.dram("dr_og", [H, 128, TOK], IN_).re("h p n -> p h n")
    zT = p.dram("dr_zT", [H, 128, TOK], IN_).re("h p n -> p h n")
    xT = p.dram("dr_xT", [D, TOK], IN_).re("(c p) n -> p c n", p=128)
    gn = p.dram("dr_gn", [128, 1], IN_)
    w_out = p.dram("dr_w_out", [D, D], IN_).re("(c p) n -> p c n", p=128)
    vecs = p.dram("dr_vecs", [128, 4, 16], IN_)
    mods = p.dram("dr_mods", [128, 4, NB4, 16], IN_)
    rw = p.dram("dr_rw", [D, NE], IN_).re("(c p) n -> p c n", p=128)
    rb = p.dram("dr_rb", [128, NE], IN_)
    wg = p.dram("dr_wg", [NEX, D, EFF], IN_).re("e (c p) n -> e p c n", p=128)
    wu = p.dram("dr_wu", [NEX, D, EFF], IN_).re("e (c p) n -> e p c n", p=128)
    wd = p.dram("dr_wd", [NEX, EFF, D], IN_).re("e (c p) n -> e p c n", p=128)
    ident_d = p.dram("dr_ident", [128, 128], IN_)
    o_x = p.dram("dr_o_x", [D, TOK], OUT_).re("(c p) n -> p c n", p=128)

    pp = PsumPool(p)
    NM = 384
    ones = p.sb("ones", [128, 128])
    ident = p.sb("ident", [128, 128])
    bufA = p.sb("bufA", [128, 16, NM])
    bufB = p.sb("bufB", [128, 16 * NM])
    xb = p.sb("xb", [128, 16, NM])
    wgu = Ring([p.sb("wgu%d" % i, [128, 16, 512]) for i in range(2)])
    wdr = Ring([p.sb("wdr%d" % i, [128, 2, D]) for i in range(2)])
    hid = p.sb("hid", [128, 2, NM])
    tmp = Ring([p.sb("tmp%d" % i, [128, NM]) for i in range(2)])
    rstd = p.sb("rstd", [128, NM])
    gn_t = p.sb("gn_t", [128, 1])
    vec_t = p.sb("vec_t", [128, 4, 16])
    mod_t = p.sb("mod_t", [128, 4, NB4, 16])
    rw_t = p.sb("rw_t", [128, 16, NE])
    rb_t = p.sb("rb_t", [128, NE])
    gates = p.sb("gates", [128, 3, NEX])
    sc = p.sb("sc", [128, NE])
    sel = p.sb("sel", [128, NE])
    selm = p.sb("selm", [128, NE])
    m8 = p.sb("m8", [128, 8, 8])
    grp = p.sb("grp", [128, 8])
    g8 = p.sb("g8", [128, 8])
    gmask = p.sb("gmask", [128, 8])
    nm = p.sb("nm", [128, 8])
    t8 = p.sb("t8", [128, 8])
    den = p.sb("den", [128, 1])
    yb = bufB.re("p (c n) -> p c n", n=NM)
    acc = bufB.re("p (t d) -> p t d", d=D)

    p.memset('pool', ones[:, :], 1.0)
    p.memset('pool', gates[:, :, :], 1.0)
    p.dma('sp', ident[:, :], ident_d[:, :])
    p.dma('sp', gn_t[:, :], gn[:, :])
    p.dma('sp', vec_t[:, :, :], vecs[:, :, :])
    p.dma('sp', mod_t[:, :, :, :], mods[:, :, :, :])
    p.dma('sp', rw_t[:, :, :], rw[:, :, :])
    p.dma('sp', rb_t[:, :], rb[:, :])
    for b in range(NB4):
        p.tt('dve', mod_t[:, 0, b, :], mod_t[:, 0, b, :], vec_t[:, 0, :], ALU.mult)
        p.stt('dve', mod_t[:, 1, b, :], mod_t[:, 1, b, :], 1.0, vec_t[:, 1, :], ALU.add, ALU.mult)
        p.tt('dve', mod_t[:, 3, b, :], mod_t[:, 3, b, :], vec_t[:, 2, :], ALU.mult)

    def router(ti):
        ps = pp.get()
        for kc in range(16):
            p.mm(ps[:, 0:NE], bufA[:, kc, ti * 128:(ti + 1) * 128], rw_t[:, kc, :], kc == 0, kc == 15)
        p.act(sc[:, :], ps[:, 0:NE], AF.Sigmoid)
        p.tt('dve', sel[:, :], sc[:, :], rb_t[:, :], ALU.add)
        for g in range(8):
            p.max8(m8[:, g, :], sel[:, g * 8:(g + 1) * 8])
        p.tt('dve', grp[:, :], m8[:, :, 0], m8[:, :, 1], ALU.add)
        p.max8(g8[:, :], grp[:, :])
        p.ts('dve', gmask[:, :], grp[:, :], g8[:, 3:4], None, ALU.is_ge)
        p.ts('dve', nm[:, :], gmask[:, :], -1.0, 1.0e30, ALU.add, ALU.mult)
        for g in range(8):
            p.ts('dve', selm[:, g * 8:(g + 1) * 8], sel[:, g * 8:(g + 1) * 8], gmask[:, g:g + 1], nm[:, g:g + 1],
                 ALU.mult, ALU.add)
        p.max8(t8[:, :], selm[:, :])
        p.ts('dve', selm[:, :], selm[:, :], t8[:, 7:8], None, ALU.is_ge)
        p.tt('dve', selm[:, :], selm[:, :], sc[:, :], ALU.mult)
        p.reduce('dve', den[:, :], selm[:, :], ALU.add)
        p.recip(den[:, :], den[:, :])
        p.ts('dve', gates[:, ti, 0:NE], selm[:, :], den[:, 0:1], ROUTED_SCALE, ALU.mult, ALU.mult)

    def do_block(bi, t0, N):
        NTL = N // 128
        w0 = wgu.get()
        w1 = wgu.get()
        ogb, zb, sq = w0, w0, w1
        p.dma('sp', bufA[:, 0:8, 0:N], attn[:, :, t0:t0 + N])
        p.dma('pool', ogb[:, 0:8, 0:N], og[:, :, t0:t0 + N])
        p.dma('sp', zb[:, 8:16, 0:N], zT[:, :, t0:t0 + N])
        p.dma('pool', xb[:, :, 0:N], xT[:, :, t0:t0 + N])
        p.act(sq[:, 0:8, 0:N], ogb[:, 0:8, 0:N], AF.Square)
        p.act(zb[:, 8:16, 0:N], zb[:, 8:16, 0:N], AF.Silu)
        for h in range(H):
            ps = pp.get()
            p.mm(ps[:, 0:N], ones[:, :], sq[:, h, 0:N])
            rstd_from_ps(p, ps, N, rstd, DV)
            p.stt('dve', bufA[:, 8 + h, 0:N], ogb[:, h, 0:N], gn_t[:, 0:1], rstd[:, 0:N], ALU.mult, ALU.mult)
            p.tt('dve', bufA[:, 8 + h, 0:N], bufA[:, 8 + h, 0:N], zb[:, 8 + h, 0:N], ALU.mult)
        for m in range(16):
            wt_ = wdr.get()
            wo = wt_.re("p a (c n) -> p (a c) n", n=128)
            p.dma(p.dq(), wo[:, 0:16, :], w_out[:, :, m * 128:(m + 1) * 128])
            ps = pp.get()
            for kc in range(16):
                p.mm(ps[:, 0:N], wo[:, kc, :], bufA[:, kc, 0:N], kc == 0, kc == 15)
            p.copy('act' if m % 2 == 0 else 'dve', yb[:, m, 0:N], ps[:, 0:N])
        rms_rstd(p, pp, yb, 0, 16, N, ones, bufA, rstd, D)
        for c in range(16):
            p.stt('dve', yb[:, c, 0:N], yb[:, c, 0:N], mod_t[:, 0, bi, c:c + 1], rstd[:, 0:N], ALU.mult, ALU.mult)
            p.tt('pool', xb[:, c, 0:N], xb[:, c, 0:N], yb[:, c, 0:N], ALU.add)
        rms_rstd(p, pp, xb, 0, 16, N, ones, bufA, rstd, D)
        for c in range(16):
            p.stt('dve', bufA[:, c, 0:N], xb[:, c, 0:N], mod_t[:, 1, bi, c:c + 1], rstd[:, 0:N], ALU.mult, ALU.mult)
            p.act(bufA[:, c, 0:N], bufA[:, c, 0:N], AF.Identity, bias=mod_t[:, 2, bi, c:c + 1])
        for ti in range(NTL):
            router(ti)
        for e in range(nexp):
            wgu_e, wd_e = wgu.get(), wdr.get()
            p.dma('sp', wgu_e[:, :, 0:EFF], wg[e, :, :, :])
            p.dma('pool', wgu_e[:, :, EFF:2 * EFF], wu[e, :, :, :])
            p.dma(p.dq(), wd_e[:, :, :], wd[e, :, :, :])
            for f in range(2):
                psg, psu = pp.get(), pp.get()
                for kc in range(16):
                    p.mm(psg[:, 0:N], wgu_e[:, kc, f * 128:(f + 1) * 128], bufA[:, kc, 0:N], kc == 0, kc == 15)
                for kc in range(16):
                    p.mm(psu[:, 0:N], wgu_e[:, kc, EFF + f * 128:EFF + (f + 1) * 128], bufA[:, kc, 0:N],
                         kc == 0, kc == 15)
                tm = tmp.get()
                p.act(tm[:, 0:N], psg[:, 0:N], AF.Silu)
                p.tt('dve', hid[:, f, 0:N], tm[:, 0:N], psu[:, 0:N], ALU.mult)
            for ti in range(NTL):
                for n4 in range(4):
                    ps = pp.get()
                    for f in range(2):
                        p.mm(ps[:, :], hid[:, f, ti * 128:(ti + 1) * 128], wd_e[:, f, n4 * 512:(n4 + 1) * 512],
                             f == 0, f == 1)
                    a = acc[:, ti, n4 * 512:(n4 + 1) * 512]
                    if e == 0:
                        p.ts('dve', a, ps[:, :], gates[:, ti, e:e + 1], None, ALU.mult)
                    else:
                        p.stt('dve', a, ps[:, :], gates[:, ti, e:e + 1], a, ALU.mult, ALU.add)
        for ti in range(NTL):
            for c in range(16):
                ps = pp.get()
                p.transpose(ps[:, 0:128], acc[:, ti, c * 128:(c + 1) * 128], ident[:, :])
                p.copy('act' if c % 2 == 0 else 'dve', bufA[:, c, ti * 128:(ti + 1) * 128], ps[:, 0:128])
        w1 = wgu.get()
        rms_rstd(p, pp, bufA, 0, 16, N, ones, w1, rstd, D)
        for c in range(16):
            p.stt('dve', bufA[:, c, 0:N], bufA[:, c, 0:N], mod_t[:, 3, bi, c:c + 1], rstd[:, 0:N], ALU.mult, ALU.mult)
            p.tt('pool', xb[:, c, 0:N], xb[:, c, 0:N], bufA[:, c, 0:N], ALU.add)
        p.dma('sp', o_x[:, :, t0:t0 + N], xb[:, :, 0:N])

    for bi, (t0, N) in enumerate(BLOCKS4):
        do_block(bi, t0, N)
    p.finish()
    return nc


ACOLS = 6 * D // 2
NJ = ACOLS // 128


def build_a():
    nc = new_nc()
    p = Prog(nc)
    cT = p.dram("dr_cT", [128, 16, 5], IN_)
    aw = p.dram("dr_aw", [D, ACOLS], IN_).re("(c p) n -> p c n", p=128)
    ab = p.dram("dr_ab", [128, NJ], IN_)
    o_m = p.dram("dr_o_m", [128, NJ, 5], OUT_)
    pp = PsumPool(p)
    c_t = p.sb("c_t", [128, 16, 5])
    ab_t = p.sb("ab_t", [128, NJ])
    om = p.sb("om", [128, NJ, 5])
    wt = Ring([p.sb("wt%d" % i, [128, 16, 512]) for i in range(3)])
    p.dma('sp', c_t[:, :, :], cT[:, :, :])
    p.dma('sp', ab_t[:, :], ab[:, :])
    p.act(c_t[:, :, :], c_t[:, :, :], AF.Silu)
    for g in range(NJ // 4):
        w = wt.get()
        p.dma(p.dq(), w[:, :, :], aw[:, :, g * 512:(g + 1) * 512])
        for jj in range(4):
            j = g * 4 + jj
            ps = pp.get()
            for kc in range(16):
                p.mm(ps[:, 0:5], w[:, kc, jj * 128:(jj + 1) * 128], c_t[:, kc, :], kc == 0, kc == 15)
            p.act(om[:, j, :], ps[:, 0:5], AF.Identity, bias=ab_t[:, j:j + 1])
    p.dma('sp', o_m[:, :, :], om[:, :, :])
    p.finish()
    return nc


_NC_CACHE = {}


def _prog(name, builder):
    if name not in _NC_CACHE:
        _NC_CACHE[name] = builder()
    return _NC_CACHE[name]


def _run(name, builder, in_maps):
    nc = _prog(name, builder)
    res = run_bass_kernel_spmd(nc, in_maps, core_ids=list(range(8)))
    return res.results


def _fm(v, n):
    return np.ascontiguousarray(np.asarray(v, np.float32).reshape(n, 128).T)


def _gdn_consts():
    i = np.arange(128)
    same = (i[:, None] // 64) == (i[None, :] // 64)
    PE_f = ((i[:, None] <= i[None, :]) & same).astype(np.float32)
    PS_f = ((i[:, None] > i[None, :]) & same).astype(np.float32)
    BD = same.astype(np.float32)
    CH0 = np.broadcast_to((i < 64)[:, None], (128, 128)).astype(np.float32)
    CH1 = np.broadcast_to((i >= 64)[:, None], (128, 128)).astype(np.float32)
    return np.ascontiguousarray(np.stack([PE_f, PS_f, PE_f.T, PS_f.T, BD, CH0, CH1, np.eye(128, dtype=np.float32)], 0))


def _rope_tables():
    s = np.arange(SEQ)
    r = (s // GRID_W).astype(np.float32)
    col = (s % GRID_W).astype(np.float32)
    half = ROPE // 2
    inv = np.power(np.float32(10000.0), -np.arange(0, half, 2, dtype=np.float32) / np.float32(half)).astype(np.float32)
    ar = r[:, None] * inv
    ac = col[:, None] * inv
    ang = np.concatenate([ar, ar, ac, ac], -1).astype(np.float32)
    cos = np.concatenate([np.ones((CTX, ROPE), np.float32), np.cos(ang).astype(np.float32)], 0)
    sin = np.concatenate([np.zeros((CTX, ROPE), np.float32), np.sin(ang).astype(np.float32)], 0)
    return cos, sin


def _rot_mat():
    R = np.zeros((64, 64), np.float32)
    for i in range(16):
        R[16 + i, i] = -1
        R[i, 16 + i] = 1
        R[48 + i, 32 + i] = -1
        R[32 + i, 48 + i] = 1
    return R


def _blockvec(rows, blocks_n, half, b):
    out = np.empty((blocks_n, D), np.float32)
    for bi in range(blocks_n):
        out[bi] = rows[4] if (half == 0 and bi == 0) else rows[b]
    return np.ascontiguousarray(out.reshape(blocks_n, 16, 128).transpose(2, 0, 1))


def kernel(x, c, ctx, c_ctx, ada_w, ada_b, norm_mix_pre, norm_mix_post, norm_ffn_pre, norm_ffn_post,
           w_in, mla_q_norm, mla_w_q_up, mla_kv_norm, mla_w_kv_up, gdn_conv, gdn_a_log, gdn_dt_bias,
           gdn_norm, w_out, router_w, router_bias, exp_w_gate, exp_w_up, exp_w_down,
           sh_w_gate, sh_w_up, sh_w_down):
    f = np.float32
    A = lambda a: np.asarray(a, dtype=f)
    x, c, ctx, c_ctx = A(x), A(c), A(ctx), A(c_ctx)
    call = np.concatenate([c, c_ctx[None]], 0)
    cT = np.ascontiguousarray(call.T.reshape(16, 128, 5).transpose(1, 0, 2))
    ada_w, ada_b = A(ada_w), A(ada_b)
    ins = []
    for k in range(8):
        l, hf = k // 2, k % 2
        ins.append(dict(dr_cT=cT, dr_aw=np.ascontiguousarray(ada_w[l][:, hf * ACOLS:(hf + 1) * ACOLS]),
                        dr_ab=_fm(ada_b[l][hf * ACOLS:(hf + 1) * ACOLS], NJ)))
    ra = _run("a", build_a, ins)
    mod = np.empty((DEPTH, 5, 6 * D), f)
    for k in range(8):
        l, hf = k // 2, k % 2
        m = ra[k]["dr_o_m"]
        mod[l][:, hf * ACOLS:(hf + 1) * ACOLS] = m.transpose(2, 1, 0).reshape(5, ACOLS)
    xT = []
    for k in range(8):
        b, hf = k // 2, k % 2
        tok = np.concatenate([ctx[b], x[b, :TOK - CTX]], 0) if hf == 0 else x[b, TOK - CTX:]
        xT.append(np.ascontiguousarray(tok.T))
    cos, sin = _rope_tables()
    cosT = [np.ascontiguousarray(cos[hf * TOK:(hf + 1) * TOK].T) for hf in range(2)]
    sinT = [np.ascontiguousarray(sin[hf * TOK:(hf + 1) * TOK].T) for hf in range(2)]
    rmat = _rot_mat()
    gconst = _gdn_consts()
    ident = np.eye(128, dtype=f)
    isctx = np.zeros((2, TOK), f)
    isctx[0, :CTX] = 1
    islat = np.ones(BTOK, f)
    islat[:CTX] = 0
    for l in range(DEPTH):
        sh1, sc1, g1, sh2, sc2, g2 = [mod[l][:, i * D:(i + 1) * D] for i in range(6)]
        wq = A(mla_w_q_up[l]).reshape(QR, H, QK)
        wq_p = np.ascontiguousarray(np.concatenate([wq[:, :, :NOPE].reshape(QR, -1), wq[:, :, NOPE:].reshape(QR, -1)], 1))
        wkv = A(mla_w_kv_up[l]).reshape(KVR, H, NOPE + VD)
        wkv_p = np.ascontiguousarray(np.concatenate([wkv[:, :, :NOPE].reshape(KVR, -1), wkv[:, :, NOPE:].reshape(KVR, -1)], 1))
        w_in_l = A(w_in[l])
        gpre, gq, gkv = _fm(norm_mix_pre[l], 16), _fm(mla_q_norm[l], 4), _fm(mla_kv_norm[l], 2)
        ins = []
        for k in range(8):
            b, hf = k // 2, k % 2
            ins.append(dict(dr_xT=xT[k], dr_gpre=gpre, dr_msc=_blockvec(sc1, NB, hf, b), dr_msh=_blockvec(sh1, NB, hf, b),
                            dr_w_in=w_in_l, dr_gq=gq, dr_wq=wq_p, dr_gkv=gkv, dr_wkv=wkv_p,
                            dr_cosT=cosT[hf], dr_sinT=sinT[hf], dr_rmat=rmat))
        r1 = _run("s1", build_s1, ins)
        ins2, ins3 = [], []
        conv = A(gdn_conv[l])
        alog, dtb = A(gdn_a_log[l]), A(gdn_dt_bias[l])
        for b in range(BATCH):
            r0, r1b = r1[2 * b], r1[2 * b + 1]
            kn = np.ascontiguousarray(np.concatenate([r0["dr_o_kn"], r1b["dr_o_kn"]], 2))
            kp = np.concatenate([r0["dr_o_kp"], r1b["dr_o_kp"]], 1)
            kp = np.ascontiguousarray(np.concatenate([kp, (-MASK_BIG * islat)[None]], 0))
            v = np.ascontiguousarray(np.concatenate([r0["dr_o_v"], r1b["dr_o_v"]], 0))
            qkv = np.concatenate([r0["dr_o_qkv"], r1b["dr_o_qkv"]], 2)
            ba = np.concatenate([r0["dr_o_ba"], r1b["dr_o_ba"]], 0)
            for hf in range(2):
                rr = r1[2 * b + hf]
                qp = np.ascontiguousarray(np.concatenate([rr["dr_o_qp"], np.broadcast_to(isctx[hf], (H, 1, TOK))], 1))
                ins2.append(dict(dr_qn=rr["dr_o_qn"], dr_qp=qp, dr_kn=kn, dr_kp=kp, dr_v=v))
                heads = [hf * 4 + i for i in range(NPAIR)]
                raw = np.ascontiguousarray(np.stack([np.stack([qkv[w * 8 + h] for w in range(3)], 0) for h in heads], 0))
                cw = np.ascontiguousarray(np.stack(
                    [np.stack([conv[:, w * 1024 + h * 128:w * 1024 + (h + 1) * 128].T for w in range(3)], 1) for h in heads], 0))
                hsel = np.zeros((BTOK, NPAIR * 4), f)
                alog_g = np.zeros((128, 16), f)
                dtb_g = np.zeros((128, 16), f)
                for pi, h in enumerate(heads):
                    for d in range(2):
                        hsel[:, pi * 4 + d * 2] = ba[:, d * 8 + h]
                        hsel[:, pi * 4 + d * 2 + 1] = ba[:, 16 + d * 8 + h]
                        alog_g[:, pi * 4 + d * 2 + 1] = alog[d, h]
                        dtb_g[:, pi * 4 + d * 2 + 1] = dtb[d, h]
                hsel_t = np.ascontiguousarray(hsel.reshape(NTB, 128, -1).transpose(1, 0, 2))
                ins3.append(dict(dr_raw=raw, dr_cw=cw, dr_alog=alog_g, dr_dtb=dtb_g, dr_cst=gconst, dr_hsel=hsel_t))
        r2 = _run("s2", build_s2, ins2)
        r3 = _run("s3", build_s3, ins3)
        vecs = np.zeros((128, 4, 16), f)
        vecs[:, 0, :] = _fm(norm_mix_post[l], 16)
        vecs[:, 1, :] = _fm(norm_ffn_pre[l], 16)
        vecs[:, 2, :] = _fm(norm_ffn_post[l], 16)
        wg_all = np.ascontiguousarray(np.concatenate([A(exp_w_gate[l]), A(sh_w_gate[l])[None]], 0))
        wu_all = np.ascontiguousarray(np.concatenate([A(exp_w_up[l]), A(sh_w_up[l])[None]], 0))
        wd_all = np.ascontiguousarray(np.concatenate([A(exp_w_down[l]), A(sh_w_down[l])[None]], 0))
        rb = np.ascontiguousarray(np.broadcast_to(A(router_bias[l]), (128, NE)))
        gn = np.ascontiguousarray(A(gdn_norm[l]).reshape(128, 1))
        w_out_l, rw_l = A(w_out[l]), A(router_w[l])
        ins4 = []
        for k in range(8):
            b, hf = k // 2, k % 2
            ogk = np.ascontiguousarray(np.stack(
                [r3[2 * b + h // NPAIR]["dr_o_g"][h % NPAIR][:, hf * TOK:(hf + 1) * TOK] for h in range(H)], 0))
            mods = np.ascontiguousarray(np.stack([_blockvec(g1, NB4, hf, b), _blockvec(sc2, NB4, hf, b),
                                                  _blockvec(sh2, NB4, hf, b), _blockvec(g2, NB4, hf, b)], 1))
            ins4.append(dict(dr_attn=r2[k]["dr_o_at"], dr_og=ogk, dr_zT=r1[k]["dr_o_z"], dr_xT=xT[k], dr_gn=gn,
                             dr_w_out=w_out_l, dr_vecs=vecs, dr_mods=mods, dr_rw=rw_l, dr_rb=rb,
                             dr_wg=wg_all, dr_wu=wu_all, dr_wd=wd_all, dr_ident=ident))
        r4 = _run("s4", build_s4, ins4)
        xT = [r4[k]["dr_o_x"] for k in range(8)]
    out = np.empty((BATCH, SEQ, D), f)
    for b in range(BATCH):
        out[b, :TOK - CTX] = xT[2 * b][:, CTX:].T
        out[b, TOK - CTX:] = xT[2 * b + 1].T
    return out
```

```python
import contextlib
import numpy as np
import concourse.bass as bass
import concourse.mybir as mybir
from concourse.bass_utils import run_bass_kernel_spmd

F32 = mybir.dt.float32
ALU = mybir.AluOpType
AF = mybir.ActivationFunctionType
AX = mybir.AxisListType

ENGS = ['pe', 'act', 'dve', 'pool', 'sp']

D = 2048
BATCH = 4
SEQ = 4096
DEPTH = 4
GRID_W = 64
CTX = 256
EPS = 1e-6
H = 8
QR = 512
KVR = 256
NOPE = 128
ROPE = 64
VD = 128
QK = NOPE + ROPE
DK = 128
DV = 128
CONV = 5
NE = 64
EFF = 256
IN_W = 4960
O_Q, O_KV, O_KPE, O_QKV, O_Z, O_BA = 0, 512, 768, 832, 3904, 4928
TOK = 2176
NT = 17
BTOK = CTX + SEQ
BLOCKS = [(0, 256), (256, 512), (768, 512), (1280, 512), (1792, 384)]
NB = len(BLOCKS)


class Buf:
    __slots__ = ('name', 'w', 'r', 'excl')

    def __init__(self, name=''):
        self.name = name
        self.w = None
        self.r = {}
        self.excl = False


class V:
    __slots__ = ('tile', 'ap')

    def __init__(self, tile, ap):
        self.tile = tile
        self.ap = ap


class T:
    def __init__(self, t, name, buf=None, track=True):
        self.t = t
        self.name = name
        self.b = buf if buf is not None else Buf(name)
        self.track = track

    def __getitem__(self, idx):
        return V(self, self.t[idx])

    def re(self, pat, **kw):
        base = self.t if hasattr(self.t, 'rearrange') else self.t[:]
        return T(base.rearrange(pat, **kw), self.name, self.b, self.track)


def _tiles(vs):
    out = []
    for v in vs:
        if isinstance(v, V) and v.tile.track and v.tile not in out:
            out.append(v.tile)
    return out


def _ap(x):
    return x.ap if isinstance(x, V) else x


class Prog:
    def __init__(self, nc, same_engine_sync=True):
        self.nc = nc
        self.ops = {e: [] for e in ENGS}
        self.cnt = {}
        self.seen = {e: {} for e in ENGS}
        self.same = same_engine_sync
        self.st = contextlib.ExitStack()
        self.scope = contextlib.ExitStack()
        self.sems = {}
        self.base = {}
        self.stage = 0
        self.rr = 0

    def sb(self, name, shape, dtype=F32):
        return T(self.scope.enter_context(self.nc.sbuf_tensor("%s_s%d" % (name, self.stage), list(shape), dtype)), name)

    def sbp(self, name, shape, dtype=F32):
        return T(self.st.enter_context(self.nc.sbuf_tensor(name, list(shape), dtype)), name)

    def ps(self, name, shape, dtype=F32):
        t = T(self.scope.enter_context(self.nc.psum_tensor("%s_s%d" % (name, self.stage), list(shape), dtype)), name)
        t.b.excl = True
        return t

    def idram(self, name, shape, dtype=F32):
        return T(self.nc.dram_tensor(name, list(shape), dtype).ap(), 'dr_' + name, track=False)

    def dram(self, name, shape, kind, dtype=F32, track=False):
        return T(self.nc.dram_tensor(name, list(shape), dtype, kind=kind).ap(), name, track=track)

    def op(self, eng, fn, reads=(), writes=(), dma=None, inc=None):
        waits = {}
        seen = self.seen[eng]

        def need(dep):
            if dep is None:
                return
            k, v = dep
            if k == eng and (eng == 'pe' or not self.same):
                return
            if seen.get(k, 0) >= v:
                return
            if waits.get(k, 0) < v:
                waits[k] = v

        for t in reads:
            need(t.b.w)
            if t.b.excl:
                for k, v in t.b.r.items():
                    if k != eng:
                        need((k, v))
        for t in writes:
            need(t.b.w)
            for k, v in t.b.r.items():
                need((k, v))
        for k, v in waits.items():
            seen[k] = v
        key = dma if dma else eng
        inc = inc if inc is not None else (16 if dma else 1)
        c = self.cnt.get(key, 0) + inc
        self.cnt[key] = c
        for t in reads:
            t.b.r[key] = c
        for t in writes:
            t.b.w = (key, c)
            t.b.r = {}
        self.ops[eng].append((list(waits.items()), fn, key, inc))

    def dma(self, eng, out, in_):
        sbside = in_.tile if out.tile.name.startswith('dr_') else out.tile
        key = 'd_' + sbside.name + '_' + eng
        o, i = out.ap, in_.ap
        self.op(eng, lambda e: e.dma_start(out=o, in_=i), _tiles([in_]), _tiles([out]), dma=key)

    def dq(self):
        self.rr += 1
        return ('sp', 'pool')[self.rr % 2]

    def mm(self, out, lhsT, rhs, start=True, stop=True):
        o, l, r = out.ap, lhsT.ap, rhs.ap
        self.op('pe', lambda e: e.matmul(o, l, r, start=start, stop=stop), _tiles([lhsT, rhs]), _tiles([out]))

    def transpose(self, out, in_, ident):
        self.mm(out, in_, ident)

    def act(self, out, in_, func, bias=None, scale=1.0, eng='act'):
        o, i, b, s = out.ap, in_.ap, _ap(bias), _ap(scale)
        if b is None:
            self.op('act', lambda e: e.activation(out=o, in_=i, func=func, scale=s), _tiles([in_, scale]), _tiles([out]))
        else:
            self.op('act', lambda e: e.activation(out=o, in_=i, func=func, bias=b, scale=s),
                    _tiles([in_, bias, scale]), _tiles([out]))

    def copy(self, eng, out, in_):
        o, i = out.ap, in_.ap
        if eng == 'act':
            self.op('act', lambda e: e.copy(out=o, in_=i), _tiles([in_]), _tiles([out]))
        else:
            self.op(eng, lambda e: e.tensor_copy(out=o, in_=i), _tiles([in_]), _tiles([out]))

    def tt(self, eng, out, in0, in1, op):
        o, a, b = out.ap, in0.ap, in1.ap
        self.op(eng, lambda e: e.tensor_tensor(out=o, in0=a, in1=b, op=op), _tiles([in0, in1]), _tiles([out]))

    def ts(self, eng, out, in0, s1, s2, op0, op1=None):
        o, a, x1, x2 = out.ap, in0.ap, _ap(s1), _ap(s2)
        if op1 is None:
            self.op(eng, lambda e: e.tensor_scalar(out=o, in0=a, scalar1=x1, scalar2=None, op0=op0),
                    _tiles([in0, s1]), _tiles([out]))
        else:
            self.op(eng, lambda e: e.tensor_scalar(out=o, in0=a, scalar1=x1, scalar2=x2, op0=op0, op1=op1),
                    _tiles([in0, s1, s2]), _tiles([out]))

    def stt(self, eng, out, in0, scalar, in1, op0, op1):
        o, a, s, b = out.ap, in0.ap, _ap(scalar), in1.ap
        self.op(eng, lambda e: e.scalar_tensor_tensor(out=o, in0=a, scalar=s, in1=b, op0=op0, op1=op1),
                _tiles([in0, scalar, in1]), _tiles([out]))

    def memset(self, eng, out, val):
        o = out.ap
        self.op(eng, lambda e: e.memset(o, val), [], _tiles([out]))

    def recip(self, out, in_):
        o, i = out.ap, in_.ap
        self.op('dve', lambda e: e.reciprocal(out=o, in_=i), _tiles([in_]), _tiles([out]))

    def max8(self, out, in_):
        o, i = out.ap, in_.ap
        self.op('dve', lambda e: e.max(out=o, in_=i), _tiles([in_]), _tiles([out]))

    def reduce(self, eng, out, in_, op, axis=AX.X):
        o, i = out.ap, in_.ap
        self.op(eng, lambda e: e.tensor_reduce(out=o, in_=i, axis=axis, op=op), _tiles([in_]), _tiles([out]))

    def flush(self, final=False):
        nc = self.nc
        for k in self.cnt:
            if k not in self.sems:
                self.sems[k] = self.st.enter_context(nc.semaphore("s_" + k))
        sems = self.sems
        base = list(self.base.items())
        if final:
            waits = [(k, v) for k, v in self.cnt.items() if k.startswith('d_')]
            self.ops['sp'].append((waits, None, None, 0))
        ops = self.ops
        with nc.Block() as block:
            def run(e):
                def f(engobj):
                    for k, v in base:
                        engobj.wait_ge(sems[k], v)
                    for waits, fn, key, inc in ops[e]:
                        for k, v in waits:
                            engobj.wait_ge(sems[k], v)
                        if fn is not None:
                            fn(engobj).then_inc(sems[key], inc)
                return f

            block.tensor(run('pe'))
            block.scalar(run('act'))
            block.vector(run('dve'))
            block.gpsimd(run('pool'))
            block.sync(run('sp'))
        self.ops = {e: [] for e in ENGS}
        self.base = dict(self.cnt)
        self.seen = {e: dict(self.base) for e in ENGS}
        self.scope.close()
        self.scope = contextlib.ExitStack()
        self.stage += 1

    def close(self):
        self.st.close()


def new_nc():
    return bass.Bass("TRN2", target_bir_lowering=False)


class PsumPool:
    def __init__(self, p, n=8):
        self.tiles = [p.ps("psb%d" % i, [128, 512]) for i in range(n)]
        self.i = 0

    def get(self):
        t = self.tiles[self.i % len(self.tiles)]
        self.i += 1
        return t


class Ring:
    def __init__(self, tiles):
        self.tiles = tiles
        self.i = 0

    def get(self):
        t = self.tiles[self.i % len(self.tiles)]
        self.i += 1
        return t


def rstd_from_ps(p, ps, N, rstd, dim):
    p.ts('dve', rstd[:, 0:N], ps[:, 0:N], 1.0 / dim, EPS, ALU.mult, ALU.add)
    p.act(rstd[:, 0:N], rstd[:, 0:N], AF.Sqrt)
    p.recip(rstd[:, 0:N], rstd[:, 0:N])


def rms_rstd(p, pp, src, c0, nch, N, ones, sq, rstd, dim):
    p.act(sq[:, 0:nch, 0:N], src[:, c0:c0 + nch, 0:N], AF.Square)
    ps = pp.get()
    for c in range(nch):
        p.mm(ps[:, 0:N], ones[:, :], sq[:, c, 0:N], c == 0, c == nch - 1)
    rstd_from_ps(p, ps, N, rstd, dim)


IN_, OUT_ = "ExternalInput", "ExternalOutput"


IN_, OUT_ = "ExternalInput", "ExternalOutput"
G1R = H * 128 + 128 + 3072
G2C = H * 128 + 32
R_KP = H * 128
R_QKV = H * 128 + 128
NG1 = G1R // 128
BLOCKS4 = [(0, 256)] + [(256 + i * 384, 384) for i in range(5)]
NB4 = len(BLOCKS4)
NEX = NE + 1
NEXD = NEX
ROUTED_SCALE = 2.5
MASK_BIG = 1.0e5
NPAIR = 4
NTB = BTOK // 128
SEGS = [(0, CTX), (CTX, BTOK)]
FWD_ORDER = list(range(NTB))
BWD_ORDER = [1, 0] + list(range(NTB - 1, 1, -1))
PAIR_GROUPS = [[0, 1], [2, 3], [4, 5], [6, 7]]


def sub(t, idx):
    return T(t.t[idx], t.name, t.b, t.track)


class IO:
    pass


def declare_io(p, depth):
    io = IO()

    def d(name, shape):
        return p.dram("dr_" + name, shape, IN_)

    io.x0T = d("x0T", [D, TOK]).re("(c p) n -> p c n", p=128)
    io.cT2 = d("cT2", [128, 16, 2])
    io.bsel1 = d("bsel1", [128, NB, 2])
    io.bsel4 = d("bsel4", [128, NB4, 2])
    io.par = d("par", [128, 2])
    io.cosT, io.sinT = d("cosT", [64, TOK]), d("sinT", [64, TOK])
    io.rmat = d("rmat", [64, 64])
    io.qmask, io.kmask = d("qmask", [1, TOK]), d("kmask", [1, BTOK])
    io.cst = d("cst", [8, 128, 128])
    io.ada_w = d("ada_w", [depth, D, 6 * D])
    io.ada_b = d("ada_b", [depth, 128, 96])
    io.gvec = d("gvec", [depth, 128, 4, 16])
    io.w_in = d("w_in", [depth, D, IN_W])
    io.gq = d("gq", [depth, 128, 4])
    io.wq = d("wq", [depth, QR, H * QK])
    io.gkv = d("gkv", [depth, 128, 2])
    io.wkv = d("wkv", [depth, KVR, 2 * H * 128])
    io.cw = d("cw", [depth, NPAIR, 128, 3, 5])
    io.alog = d("alog", [depth, 128, 16])
    io.dtb = d("dtb", [depth, 128, 16])
    io.gn = d("gn", [depth, 128, 1])
    io.w_out = d("w_out", [depth, D, D])
    io.rw = d("rw", [depth, D, NE])
    io.rb = d("rb", [depth, 128, NE])
    io.wg = d("wg", [depth, NEXD, D, EFF])
    io.wu = d("wu", [depth, NEXD, D, EFF])
    io.wd = d("wd", [depth, NEXD, EFF, D])
    io.o_x = p.dram("dr_o_x", [D, TOK], OUT_).re("(c p) n -> p c n", p=128)
    io.xs = p.idram("xs", [D, TOK]).re("(c p) n -> p c n", p=128)
    io.qn_s = p.idram("qn_s", [H, 128, TOK])
    io.qp_s = p.idram("qp_s", [H, 64, TOK])
    io.z_s = p.idram("z_s", [H, 128, TOK])
    io.attn_s = p.idram("attn_s", [H, 128, TOK])
    io.g1_s = p.idram("g1_s", [G1R, TOK])
    io.g1_g = p.idram("g1_g", [2 * G1R, TOK])
    io.g2_s = p.idram("g2_s", [TOK, G2C])
    io.g2_g = p.idram("g2_g", [BTOK, G2C])
    io.og_s = p.idram("og_s", [NPAIR * 128, BTOK])
    io.og_g = p.idram("og_g", [2 * NPAIR * 128, BTOK])
    return io


_CCBUF = T(None, 'ccbuf')


def all_gather(p, snd, rcv):
    s_ap, r_ap = snd.t.opt(), rcv.t.opt()
    p.op('pool', lambda e: e.collective_compute("AllGather", ALU.bypass, replica_groups=PAIR_GROUPS,
                                                ins=[s_ap], outs=[r_ap]), writes=[_CCBUF], dma='cc', inc=1)


def gather_chunks(p, snd, rcv, rows, nch):
    for c in range(nch):
        all_gather(p, sub(snd, slice(c * rows, (c + 1) * rows)), sub(rcv, slice(c * 2 * rows, (c + 1) * 2 * rows)))


def blend(p, out, a, b, par):
    p.ts('dve', out, a, par[:, 0:1], None, ALU.mult)
    p.stt('dve', out, b, par[:, 1:2], out, ALU.mult, ALU.add)


def blockvecs(p, dst, modp, l, i, bsel, nblk, tmp):
    own, cx = modp[:, l, i * 16:(i + 1) * 16, 0], modp[:, l, i * 16:(i + 1) * 16, 1]
    for bi in range(nblk):
        p.ts('dve', tmp[:, 0:16], own, bsel[:, bi, 1:2], None, ALU.mult)
        p.stt('dve', dst[:, bi, :], cx, bsel[:, bi, 0:1], tmp[:, 0:16], ALU.mult, ALU.add)


def stage_a(p, io, depth, modp):
    pp = PsumPool(p, 4)
    c_t = p.sb("c_t", [128, 16, 2])
    ab_t = p.sb("ab_t", [128, depth, 96])
    wt = Ring([p.sb("awt%d" % i, [128, 16, 512]) for i in range(3)])
    p.dma('sp', c_t[:, :, :], io.cT2[:, :, :])
    p.dma('sp', ab_t[:, :, :], io.ada_b.re("l p j -> p l j")[:, :, :])
    p.act(c_t[:, :, :], c_t[:, :, :], AF.Silu)
    for l in range(depth):
        aw = sub(io.ada_w, l).re("(c p) n -> p c n", p=128)
        for g in range(24):
            w = wt.get()
            p.dma(p.dq(), w[:, :, :], aw[:, :, g * 512:(g + 1) * 512])
            for jj in range(4):
                j = g * 4 + jj
                ps = pp.get()
                for kc in range(16):
                    p.mm(ps[:, 0:2], w[:, kc, jj * 128:(jj + 1) * 128], c_t[:, kc, :], kc == 0, kc == 15)
                p.act(modp[:, l, j, :], ps[:, 0:2], AF.Identity, bias=ab_t[:, l, j:j + 1])


def stage_s1(p, io, l, modp):
    xT = io.x0T if l == 0 else io.xs
    w_in = sub(io.w_in, l).re("(c p) n -> p c n", p=128)
    wq = sub(io.wq, l).re("(c p) n -> p c n", p=128)
    wkv = sub(io.wkv, l).re("(c p) n -> p c n", p=128)
    pp = PsumPool(p)
    ones = p.sb("ones", [128, 128])
    xb = p.sb("xb", [128, 16, 512])
    hb = p.sb("hb", [128, 16, 512])
    wt = Ring([p.sb("wt%d" % i, [128, 16, 320]) for i in range(2)])
    rstd = p.sb("rstd", [128, 512])
    A_t = p.sb("A_t", [128, NB, 16])
    S_t = p.sb("S_t", [128, NB, 16])
    bs_t = p.sb("bs_t", [128, NB, 2])
    tmpv = p.sb("tmpv", [128, 16])
    gv_t = p.sb("gv_t", [128, 4, 16])
    gq_t = p.sb("gq_t", [128, 4])
    gkv_t = p.sb("gkv_t", [128, 2])
    wq_t = p.sb("wq_t", [128, 4, H * QK])
    wkv_t = p.sb("wkv_t", [128, 2, 2 * H * 128])
    cos_t = p.sb("cos_t", [64, 512])
    sin_t = p.sb("sin_t", [64, 512])
    rm_t = p.sb("rm_t", [64, 64])
    lat = p.sb("lat", [128, 7, 512])
    latn = p.sb("latn", [128, 6, 512])
    lsq = xb
    rq = p.sb("rq", [128, 512])
    stg = Ring([p.sb("stg%d" % i, [128, 512]) for i in range(4)])
    pe_a = p.sb("pe_a", [64, 512])
    pe_b = p.sb("pe_b", [64, 512])
    pe_c = Ring([p.sb("pe_c%d" % i, [64, 512]) for i in range(2)])
    vst = Ring([p.sb("vst%d" % i, [128, 512]) for i in range(2)])
    bast = Ring([p.sb("bast%d" % i, [128, 32]) for i in range(2)])

    p.memset('pool', ones[:, :], 1.0)
    p.dma('sp', gv_t[:, :, :], sub(io.gvec, l)[:, :, :])
    p.dma('sp', bs_t[:, :, :], io.bsel1[:, :, :])
    p.dma('sp', gq_t[:, :], sub(io.gq, l)[:, :])
    p.dma('sp', gkv_t[:, :], sub(io.gkv, l)[:, :])
    p.dma('sp', rm_t[:, :], io.rmat[:, :])
    p.dma('pool', wq_t[:, :, :], wq[:, :, :])
    p.dma('pool', wkv_t[:, :, :], wkv[:, :, :])
    blockvecs(p, A_t, modp, l, 1, bs_t, NB, tmpv)
    blockvecs(p, S_t, modp, l, 0, bs_t, NB, tmpv)
    for b in range(NB):
        p.stt('dve', A_t[:, b, :], A_t[:, b, :], 1.0, gv_t[:, 0, :], ALU.add, ALU.mult)

    groups = [(0, 256), (256, 256), (512, 320)] + [(O_QKV + i * 256, 256) for i in range(12)] + \
             [(O_Z + i * 256, 256) for i in range(4)] + [(O_BA, 32)]

    def rope(src, N, dst):
        p.copy('act', pe_a[:, 0:N], src)
        ps2 = pp.get()
        p.mm(ps2[0:64, 0:N], rm_t[:, :], pe_a[:, 0:N])
        p.tt('dve', pe_b[:, 0:N], ps2[0:64, 0:N], sin_t[:, 0:N], ALU.mult)
        pc = pe_c.get()
        p.tt('pool', pc[:, 0:N], pe_a[:, 0:N], cos_t[:, 0:N], ALU.mult)
        p.tt('dve', pc[:, 0:N], pc[:, 0:N], pe_b[:, 0:N], ALU.add)
        p.dma(p.dq(), dst, pc[:, 0:N])

    def mla_up(t0, N):
        rms_rstd(p, pp, lat, 0, 4, N, ones, lsq, rq, QR)
        for c in range(4):
            p.stt('dve', latn[:, c, 0:N], lat[:, c, 0:N], gq_t[:, c:c + 1], rq[:, 0:N], ALU.mult, ALU.mult)
        rms_rstd(p, pp, lat, 4, 2, N, ones, lsq, rq, KVR)
        for c in range(2):
            p.stt('dve', latn[:, 4 + c, 0:N], lat[:, 4 + c, 0:N], gkv_t[:, c:c + 1], rq[:, 0:N], ALU.mult, ALU.mult)
        rope(lat[0:64, 6, 0:N], N, io.g1_s[R_KP:R_KP + 64, t0:t0 + N])
        for h in range(H):
            ps = pp.get()
            for kc in range(4):
                p.mm(ps[:, 0:N], wq_t[:, kc, h * 128:(h + 1) * 128], latn[:, kc, 0:N], kc == 0, kc == 3)
            s = stg.get()
            p.copy('act', s[:, 0:N], ps[:, 0:N])
            p.dma(p.dq(), io.qn_s[h, :, t0:t0 + N], s[:, 0:N])
            ps = pp.get()
            for kc in range(4):
                p.mm(ps[0:64, 0:N], wq_t[:, kc, H * 128 + h * 64:H * 128 + (h + 1) * 64], latn[:, kc, 0:N],
                     kc == 0, kc == 3)
            rope(ps[0:64, 0:N], N, io.qp_s[h, :, t0:t0 + N])
        for h in range(H):
            ps = pp.get()
            for kc in range(2):
                p.mm(ps[:, 0:N], wkv_t[:, kc, h * 128:(h + 1) * 128], latn[:, 4 + kc, 0:N], kc == 0, kc == 1)
            s = stg.get()
            p.copy('dve', s[:, 0:N], ps[:, 0:N])
            p.dma(p.dq(), io.g1_s[h * 128:(h + 1) * 128, t0:t0 + N], s[:, 0:N])
        for ti in range(N // 128):
            for half in range(2):
                vs = vst.get()
                ps = pp.get()
                for kc in range(2):
                    p.mm(ps[:, :], latn[:, 4 + kc, ti * 128:(ti + 1) * 128],
                         wkv_t[:, kc, H * 128 + half * 512:H * 128 + (half + 1) * 512], kc == 0, kc == 1)
                p.copy('act', vs[:, :], ps[:, :])
                p.dma(p.dq(), io.g2_s[t0 + ti * 128:t0 + (ti + 1) * 128, half * 512:(half + 1) * 512], vs[:, :])

    def do_block(bi, t0, N):
        p.dma('sp', xb[:, :, 0:N], xT[:, :, t0:t0 + N])
        p.dma('pool', cos_t[:, 0:N], io.cosT[:, t0:t0 + N])
        p.dma('pool', sin_t[:, 0:N], io.sinT[:, t0:t0 + N])
        rms_rstd(p, pp, xb, 0, 16, N, ones, hb, rstd, D)
        for c in range(16):
            p.stt('dve', hb[:, c, 0:N], xb[:, c, 0:N], A_t[:, bi, c:c + 1], rstd[:, 0:N], ALU.mult, ALU.mult)
            p.act(hb[:, c, 0:N], hb[:, c, 0:N], AF.Identity, bias=S_t[:, bi, c:c + 1])
        for (c0, cw) in groups:
            w = wt.get()
            p.dma(p.dq(), w[:, :, 0:cw], w_in[:, :, c0:c0 + cw])
            if c0 == O_BA:
                for ti in range(N // 128):
                    ps = pp.get()
                    for kc in range(16):
                        p.mm(ps[:, 0:32], hb[:, kc, ti * 128:(ti + 1) * 128], w[:, kc, 0:32], kc == 0, kc == 15)
                    bs = bast.get()
                    p.copy('act', bs[:, :], ps[:, 0:32])
                    p.dma('sp', io.g2_s[t0 + ti * 128:t0 + (ti + 1) * 128, H * 128:H * 128 + 32], bs[:, :])
                continue
            for m in range((cw + 127) // 128):
                mw = min(128, cw - m * 128)
                ps = pp.get()
                for kc in range(16):
                    p.mm(ps[0:mw, 0:N], w[:, kc, m * 128:m * 128 + mw], hb[:, kc, 0:N], kc == 0, kc == 15)
                col = c0 + m * 128
                if col < O_QKV:
                    p.copy('act', lat[0:mw, col // 128, 0:N], ps[0:mw, 0:N])
                else:
                    s = stg.get()
                    p.copy('act' if m % 2 == 0 else 'dve', s[:, 0:N], ps[:, 0:N])
                    if col < O_Z:
                        r0 = R_QKV + ((col - O_QKV) // 128) * 128
                        p.dma(p.dq(), io.g1_s[r0:r0 + 128, t0:t0 + N], s[:, 0:N])
                    else:
                        p.dma(p.dq(), io.z_s[(col - O_Z) // 128, :, t0:t0 + N], s[:, 0:N])
            if c0 == 512:
                mla_up(t0, N)

    for bi, (t0, N) in enumerate(BLOCKS):
        do_block(bi, t0, N)


def stage_s2(p, io):
    v = io.g2_g.re("(c r p) n -> p r c n", r=2, p=128)
    NKT = BTOK // 128
    st_ps = Ring([p.ps("st_ps%d" % i, [128, 512]) for i in range(4)])
    ot_ps = Ring([p.ps("ot_ps%d" % i, [128, 512]) for i in range(2)])
    su_ps = Ring([p.ps("su_ps%d" % i, [128, 512]) for i in range(2)])
    ones = p.sb("ones", [128, 128])
    kn_t = Ring([p.sb("kn_t%d" % i, [128, BTOK]) for i in range(2)])
    kp_t = p.sb("kp_t", [65, BTOK])
    v_t = Ring([p.sb("v_t%d" % i, [128, NKT, 128]) for i in range(2)])
    qn_t = Ring([p.sb("qn_t%d" % i, [128, TOK]) for i in range(2)])
    qp_t = Ring([p.sb("qp_t%d" % i, [65, TOK]) for i in range(2)])
    pt = Ring([p.sb("pt%d" % i, [128, 512]) for i in range(4)])
    rs = p.sb("rs", [128, 512])
    og = Ring([p.sb("og%d" % i, [128, 512]) for i in range(2)])
    p.memset('pool', ones[:, :], 1.0)
    for r in range(2):
        p.dma('sp', kp_t[0:64, r * TOK:(r + 1) * TOK], io.g1_g[H * 256 + r * 128:H * 256 + r * 128 + 64, :])
    p.dma('sp', kp_t[64:65, :], io.kmask[:, :])
    scale = float(QK) ** -0.5
    for h in range(H):
        knh, vh, qnh, qph = kn_t.get(), v_t.get(), qn_t.get(), qp_t.get()
        for r in range(2):
            p.dma('sp', knh[:, r * TOK:(r + 1) * TOK], io.g1_g[h * 256 + r * 128:h * 256 + (r + 1) * 128, :])
        vh4 = vh.re("p (r c) n -> p r c n", r=2)
        for r in range(2):
            p.dma('pool', vh4[:, r, :, :], v[:, r, :, h * 128:(h + 1) * 128])
        p.dma('sp', qnh[:, :], io.qn_s[h, :, :])
        p.dma('pool', qph[0:64, :], io.qp_s[h, :, :])
        p.dma('pool', qph[64:65, :], io.qmask[:, :])
        for (t0, N) in BLOCKS:
            ot, su = ot_ps.get(), su_ps.get()
            for kt in range(NKT):
                st = st_ps.get()
                p.mm(st[:, 0:N], knh[:, kt * 128:(kt + 1) * 128], qnh[:, t0:t0 + N], True, False)
                p.mm(st[:, 0:N], kp_t[:, kt * 128:(kt + 1) * 128], qph[:, t0:t0 + N], False, True)
                pk = pt.get()
                p.act(pk[:, 0:N], st[:, 0:N], AF.Exp, scale=scale)
                p.mm(ot[:, 0:N], vh[:, kt, :], pk[:, 0:N], kt == 0, kt == NKT - 1)
                p.mm(su[:, 0:N], ones[:, :], pk[:, 0:N], kt == 0, kt == NKT - 1)
            p.recip(rs[:, 0:N], su[:, 0:N])
            o = og.get()
            p.tt('dve', o[:, 0:N], ot[:, 0:N], rs[:, 0:N], ALU.mult)
            p.dma(p.dq(), io.attn_s[h, :, t0:t0 + N], o[:, 0:N])


class QPsum:
    def __init__(self, p, nbanks=8):
        self.q = []
        for i in range(nbanks):
            bank = p.ps("qb%d" % i, [128, 512])
            for j in range(4):
                self.q.append(T(bank.t[:, j * 128:(j + 1) * 128], "qb%d_%d" % (i, j), buf=bank.b))
        self.i = 0

    def get(self):
        t = self.q[self.i % len(self.q)]
        self.i += 1
        return t


def stage_s3(p, io, l):
    dbg = 0
    cw = sub(io.cw, l)
    qp_ = QPsum(p, 7)
    big = PsumPool(p, 1)
    C = p.sb("C", [128, 8, 128])
    ones = p.sb("ones", [128, 128])
    par = p.sb("par", [128, 2])
    p.dma('sp', C[:, :, :], io.cst.re("c p n -> p c n")[:, :, :])
    p.dma('sp', par[:, :], io.par[:, :])
    p.memset('pool', ones[:, :], 1.0)
    PEm = [C[:, 0, :], C[:, 2, :]]
    PSm = [C[:, 1, :], C[:, 3, :]]
    BD, CH, ident = C[:, 4, :], [C[:, 5, :], C[:, 6, :]], C[:, 7, :]
    bat = p.sb("bat", [128, NTB, NPAIR * 4])
    baf = p.sb("baf", [128, NTB, 32])
    baf4 = baf.re("p (r c) n -> p r c n", r=2)
    for rk in range(2):
        p.dma('sp', baf4[:, rk, :, :], io.g2_g.re("(c r p) n -> p r c n", r=2, p=128)[:, rk, :, H * 128:H * 128 + 32])
    for pi in range(NPAIR):
        for d in range(2):
            for k2 in range(2):
                ca = k2 * 16 + d * 8 + pi
                blend(p, bat[:, :, pi * 4 + d * 2 + k2], baf[:, :, ca], baf[:, :, ca + 4], par)
    alog_t = p.sb("alog_t", [128, 16])
    dtb_t = p.sb("dtb_t", [128, 16])
    p.dma('sp', alog_t[:, :], sub(io.alog, l)[:, :])
    p.dma('sp', dtb_t[:, :], sub(io.dtb, l)[:, :])
    nea = p.sb("nea", [128, 16])
    p.act(nea[:, :], alog_t[:, :], AF.Exp)
    p.ts('dve', nea[:, :], nea[:, :], -1.0, None, ALU.mult)
    gb = p.sb("gb", [128, NTB, NPAIR * 4])
    for t in range(NTB):
        p.tt('pool', gb[:, t, :], bat[:, t, :], dtb_t[:, :], ALU.add)
    p.act(gb[:, :, :], gb[:, :, :], AF.Exp)
    p.act(gb[:, :, :], gb[:, :, :], AF.Ln, bias=1.0)
    for t in range(NTB):
        p.tt('pool', gb[:, t, :], gb[:, t, :], nea[:, :], ALU.mult)
    sig = p.sb("sig", [128, NTB, NPAIR * 4])
    p.act(sig[:, :, :], bat[:, :, :], AF.Sigmoid)

    rawt = Ring([p.sb("rawt%d" % i, [128, BTOK]) for i in range(2)])
    cv = p.sb("cv", [128, 3, BTOK])
    cwt = p.sb("cwt", [128, 3, 5])
    sq = p.sb("sq", [128, 512])
    rn = p.sb("rn", [128, 512])
    oT = p.sb("oT", [128, BTOK])
    vec = {}
    for d in range(2):
        for nm in ("gc", "bg", "kd", "nb", "be", "g"):
            vec[(nm, d)] = p.sb("v_%s%d" % (nm, d), [128, NTB])
        vec[("egl", d)] = p.sb("v_egl%d" % d, [128, NTB, 2])
    S = [p.sb("S%d" % d, [128, 128]) for d in range(2)]

    def ring(nm, n=2):
        return [Ring([p.sb("%s%d_%d" % (nm, d, i), [128, 128]) for i in range(n)]) for d in range(2)]

    r_gpe, r_dm, r_dmt, r_egcb, r_t1, r_t2 = ring("gpe"), ring("dm"), ring("dmt"), ring("egcb"), ring("t1"), ring("t2")
    r_M, r_Mt, r_Pt = ring("M", 3), ring("Mt", 3), ring("Pt", 3)
    r_qkdt, r_kbg, r_kdec, r_vb, r_wT, r_u, r_qgT, r_vnew = (ring("qkdt"), ring("kbg"), ring("kdec"), ring("vb"),
                                                            ring("wT"), ring("u"), ring("qgT"), ring("vnew"))

    def prep_pair(pi):
        p.dma('sp', cwt[:, :, :], cw[pi, :, :, :])
        for w3 in range(3):
            r, r2 = rawt.get(), rawt.get()
            for rk in range(2):
                ra = (R_QKV // 128 + w3 * 8 + pi) * 256 + rk * 128
                p.dma('sp', r[:, rk * TOK:(rk + 1) * TOK], io.g1_g[ra:ra + 128, :])
                p.dma('pool', r2[:, rk * TOK:(rk + 1) * TOK], io.g1_g[ra + 1024:ra + 1152, :])
            blend(p, r[:, :], r[:, :], r2[:, :], par)
            eng = 'dve'
            p.ts(eng, cv[:, w3, :], r[:, :], cwt[:, w3, 2:3], None, ALU.mult)
            for s in (-2, -1, 1, 2):
                for (a, b) in SEGS:
                    lo, hi = max(a, a - s), min(b, b - s)
                    p.stt(eng, cv[:, w3, lo:hi], r[:, lo + s:hi + s], cwt[:, w3, s + 2:s + 3], cv[:, w3, lo:hi],
                          ALU.mult, ALU.add)
            p.act(cv[:, w3, :], cv[:, w3, :], AF.Silu)
        for w3 in range(2):
            for c0 in range(0, BTOK, 512):
                n = min(512, BTOK - c0)
                p.act(sq[:, 0:n], cv[:, w3, c0:c0 + n], AF.Square)
                ps = big.get()
                p.mm(ps[:, 0:n], ones[:, :], sq[:, 0:n])
                if w3 == 0:
                    p.ts('dve', rn[:, 0:n], ps[:, 0:n], EPS, float(DK), ALU.add, ALU.mult)
                else:
                    p.ts('dve', rn[:, 0:n], ps[:, 0:n], EPS, None, ALU.add)
                p.act(rn[:, 0:n], rn[:, 0:n], AF.Sqrt)
                p.recip(rn[:, 0:n], rn[:, 0:n])
                p.tt('dve', cv[:, w3, c0:c0 + n], cv[:, w3, c0:c0 + n], rn[:, 0:n], ALU.mult)
        p.memset('pool', oT[:, :], 0.0)
        for d in range(2):
            cb = pi * 4 + d * 2
            g, be = vec[("g", d)], vec[("be", d)]
            p.copy('pool', g[:, :], gb[:, :, cb + 1])
            p.copy('pool', be[:, :], sig[:, :, cb])
            ps = qp_.get()
            p.mm(ps[:, 0:NTB], PEm[d], g[:, :])
            gc = vec[("gc", d)]
            p.copy('act', gc[:, :], ps[:, 0:NTB])
            ps2 = qp_.get()
            p.mm(ps2[:, 0:NTB], BD, g[:, :])
            kd = vec[("kd", d)]
            p.tt('dve', kd[:, :], ps2[:, 0:NTB], gc[:, :], ALU.subtract)
            p.act(kd[:, :], kd[:, :], AF.Exp)
            bg = vec[("bg", d)]
            p.act(bg[:, :], gc[:, :], AF.Exp)
            p.tt('dve', bg[:, :], bg[:, :], be[:, :], ALU.mult)
            p.ts('dve', vec[("nb", d)][:, :], be[:, :], -1.0, None, ALU.mult)
            egl = vec[("egl", d)]
            for c in range(2):
                ps3 = qp_.get()
                p.mm(ps3[:, 0:NTB], CH[c], g[:, :])
                p.act(egl[:, :, c], ps3[:, 0:NTB], AF.Exp)
            p.memset('pool', S[d][:, :], 0.0)

    def tile_dir(d, t):
        c0 = t * 128
        kT, qT, vT = cv[:, 1, c0:c0 + 128], cv[:, 0, c0:c0 + 128], cv[:, 2, c0:c0 + 128]
        g, be, nb = vec[("g", d)], vec[("be", d)], vec[("nb", d)]
        bg, kd, egl = vec[("bg", d)], vec[("kd", d)], vec[("egl", d)]
        gpe = r_gpe[d].get()
        p.ts('pool', gpe[:, :], PEm[d], g[:, t:t + 1], None, ALU.mult)
        ps_d, ps_dt, ps_gb = qp_.get(), qp_.get(), qp_.get()
        p.mm(ps_d[:, :], gpe[:, :], PSm[d])
        p.mm(ps_dt[:, :], PSm[d], gpe[:, :])
        p.mm(ps_gb[:, :], ones[:, :], gpe[:, :])
        dm, dmt, egcb = r_dm[d].get(), r_dmt[d].get(), r_egcb[d].get()
        p.act(dm[:, :], ps_d[:, :], AF.Exp)
        p.act(dmt[:, :], ps_dt[:, :], AF.Exp)
        p.act(egcb[:, :], ps_gb[:, :], AF.Exp)
        if dbg == 1:
            return
        ps_G, ps_KQ = qp_.get(), qp_.get()
        p.mm(ps_G[:, :], kT, kT)
        p.mm(ps_KQ[:, :], kT, qT)
        if dbg == 12:
            return
        t1, t2 = r_t1[d].get(), r_t2[d].get()
        p.tt('dve', t1[:, :], dm[:, :], PSm[d], ALU.mult)
        p.tt('dve', t2[:, :], dmt[:, :], PEm[d], ALU.mult)
        if dbg == 13:
            return
        M = r_M[d].get()
        p.stt('dve', M[:, :], ps_G[:, :], nb[:, t:t + 1], t1[:, :], ALU.mult, ALU.mult)
        if dbg == 14:
            return
        qkdt = r_qkdt[d].get()
        p.tt('dve', qkdt[:, :], ps_KQ[:, :], t2[:, :], ALU.mult)
        if dbg == 2:
            return
        ps_t = qp_.get()
        p.transpose(ps_t[:, :], M[:, :], ident)
        Mt = r_Mt[d].get()
        p.copy('act', Mt[:, :], ps_t[:, :])
        Pt = r_Pt[d].get()
        p.tt('dve', Pt[:, :], Mt[:, :], ident, ALU.add)
        if dbg == 21:
            return
        for lv in range(5 if dbg != 22 else 1):
            last = lv == 4
            ps_a = qp_.get()
            p.mm(ps_a[:, :], Mt[:, :], M[:, :])
            if dbg == 23:
                return
            if not last:
                ps_b = qp_.get()
                p.mm(ps_b[:, :], M[:, :], Mt[:, :])
            if dbg == 24:
                return
            M2 = r_M[d].get()
            p.copy('act', M2[:, :], ps_a[:, :])
            if dbg == 25:
                return
            if not last:
                Mt2 = r_Mt[d].get()
                p.copy('act' if dbg in (26, 27) else 'dve', Mt2[:, :], ps_b[:, :])
            if dbg in (26, 28):
                return
            ps_c = qp_.get()
            p.mm(ps_c[:, :], M2[:, :], Pt[:, :])
            Pt2 = r_Pt[d].get()
            p.tt('dve', Pt2[:, :], ps_c[:, :], Pt[:, :], ALU.add)
            M, Pt = M2, Pt2
            if not last:
                Mt = Mt2
        Xt = Pt
        if dbg == 3:
            return
        ps_k, ps_v = qp_.get(), qp_.get()
        p.transpose(ps_k[:, :], kT, ident)
        p.transpose(ps_v[:, :], vT, ident)
        kbg, kdec, vb = r_kbg[d].get(), r_kdec[d].get(), r_vb[d].get()
        p.ts('dve', kbg[:, :], ps_k[:, :], bg[:, t:t + 1], None, ALU.mult)
        p.act(kdec[:, :], ps_k[:, :], AF.Copy, scale=kd[:, t:t + 1])
        p.act(vb[:, :], ps_v[:, :], AF.Copy, scale=be[:, t:t + 1])
        ps_w, ps_u = qp_.get(), qp_.get()
        p.mm(ps_w[:, :], kbg[:, :], Xt[:, :])
        p.mm(ps_u[:, :], Xt[:, :], vb[:, :])
        wT, u, qgT = r_wT[d].get(), r_u[d].get(), r_qgT[d].get()
        p.copy('act', wT[:, :], ps_w[:, :])
        p.copy('dve', u[:, :], ps_u[:, :])
        p.tt('dve', qgT[:, :], cv[:, 0, c0:c0 + 128], egcb[:, :], ALU.mult)
        if dbg == 4:
            return
        vnew = r_vnew[d].get()
        for c in ((0, 1) if d == 0 else (1, 0)):
            r0, r1 = c * 64, c * 64 + 64
            ps_ws = qp_.get()
            p.mm(ps_ws[:, :], wT[:, :], S[d][:, :])
            p.tt('dve', vnew[r0:r1, :], u[r0:r1, :], ps_ws[r0:r1, :], ALU.subtract)
            ps_o = qp_.get()
            p.mm(ps_o[:, 0:64], S[d][:, :], qgT[:, r0:r1], True, False)
            p.mm(ps_o[:, 0:64], vnew[r0:r1, :], qkdt[r0:r1, r0:r1], False, True)
            p.tt('dve', oT[:, c0 + r0:c0 + r1], oT[:, c0 + r0:c0 + r1], ps_o[:, 0:64], ALU.add)
            ps_s = qp_.get()
            p.mm(ps_s[:, :], kdec[r0:r1, :], vnew[r0:r1, :])
            p.stt('dve', S[d][:, :], S[d][:, :], egl[:, t, c:c + 1], ps_s[:, :], ALU.mult, ALU.add)

    for pi in range(NPAIR):
        prep_pair(pi)
        for s in range(NTB):
            tile_dir(0, FWD_ORDER[s])
            tile_dir(1, BWD_ORDER[s])
        p.dma('sp', io.og_s[pi * 128:(pi + 1) * 128, :], oT[:, :])


def stage_s4(p, io, l, modp, last):
    nexp = NEX
    attn = io.attn_s.re("h p n -> p h n")
    ogg = io.og_g.re("(pi q r i) n -> q i r pi n", q=2, r=2, i=64)
    zT = io.z_s.re("h p n -> p h n")
    xT = io.x0T if l == 0 else io.xs
    o_x = io.o_x if last else io.xs
    w_out = sub(io.w_out, l).re("(c p) n -> p c n", p=128)
    rw = sub(io.rw, l).re("(c p) n -> p c n", p=128)
    wg = sub(io.wg, l).re("e (c p) n -> e p c n", p=128)
    wu = sub(io.wu, l).re("e (c p) n -> e p c n", p=128)
    wd = sub(io.wd, l).re("e (c p) n -> e p c n", p=128)
    pp = PsumPool(p)
    NM = 384
    ones = p.sb("ones", [128, 128])
    ident = p.sb("ident", [128, 128])
    bufA = p.sb("bufA", [128, 16, NM])
    bufB = p.sb("bufB", [128, 16 * NM])
    xb = p.sb("xb", [128, 16, NM])
    wgu = Ring([p.sb("wgu%d" % i, [128, 16, 512]) for i in range(2)])
    wdr = Ring([p.sb("wdr%d" % i, [128, 2, D]) for i in range(2)])
    hid = p.sb("hid", [128, 2, NM])
    tmp = Ring([p.sb("tmp%d" % i, [128, NM]) for i in range(2)])
    rstd = p.sb("rstd", [128, NM])
    gn_t = p.sb("gn_t", [128, 1])
    vec_t = p.sb("vec_t", [128, 4, 16])
    mod_t = p.sb("mod_t", [128, 4, NB4, 16])
    rw_t = p.sb("rw_t", [128, 16, NE])
    rb_t = p.sb("rb_t", [128, NE])
    gates = p.sb("gates", [128, 3, NEX])
    sc = p.sb("sc", [128, NE])
    sel = p.sb("sel", [128, NE])
    selm = p.sb("selm", [128, NE])
    m8 = p.sb("m8", [128, 8, 8])
    grp = p.sb("grp", [128, 8])
    g8 = p.sb("g8", [128, 8])
    gmask = p.sb("gmask", [128, 8])
    nm = p.sb("nm", [128, 8])
    t8 = p.sb("t8", [128, 8])
    den = p.sb("den", [128, 1])
    yb = bufB.re("p (c n) -> p c n", n=NM)
    acc = bufB.re("p (t d) -> p t d", d=D)

    p.memset('pool', ones[:, :], 1.0)
    p.memset('pool', gates[:, :, :], 1.0)
    par = p.sb("par", [128, 2])
    bs_t = p.sb("bs_t", [128, NB4, 2])
    tmpv = p.sb("tmpv", [128, 16])
    p.dma('sp', par[:, :], io.par[:, :])
    p.dma('sp', bs_t[:, :, :], io.bsel4[:, :, :])
    p.dma('sp', ident[:, :], io.cst[7, :, :])
    p.dma('sp', gn_t[:, :], sub(io.gn, l)[:, :])
    p.dma('sp', vec_t[:, :, :], sub(io.gvec, l)[:, :, :])
    p.dma('sp', rw_t[:, :, :], rw[:, :, :])
    p.dma('sp', rb_t[:, :], sub(io.rb, l)[:, :])
    for slot, vi in enumerate((2, 4, 3, 5)):
        blockvecs(p, sub(mod_t, (slice(None), slot)), modp, l, vi, bs_t, NB4, tmpv)
    for b in range(NB4):
        p.tt('dve', mod_t[:, 0, b, :], mod_t[:, 0, b, :], vec_t[:, 1, :], ALU.mult)
        p.stt('dve', mod_t[:, 1, b, :], mod_t[:, 1, b, :], 1.0, vec_t[:, 2, :], ALU.add, ALU.mult)
        p.tt('dve', mod_t[:, 3, b, :], mod_t[:, 3, b, :], vec_t[:, 3, :], ALU.mult)

    def router(ti):
        ps = pp.get()
        for kc in range(16):
            p.mm(ps[:, 0:NE], bufA[:, kc, ti * 128:(ti + 1) * 128], rw_t[:, kc, :], kc == 0, kc == 15)
        p.act(sc[:, :], ps[:, 0:NE], AF.Sigmoid)
        p.tt('dve', sel[:, :], sc[:, :], rb_t[:, :], ALU.add)
        for g in range(8):
            p.max8(m8[:, g, :], sel[:, g * 8:(g + 1) * 8])
        p.tt('dve', grp[:, :], m8[:, :, 0], m8[:, :, 1], ALU.add)
        p.max8(g8[:, :], grp[:, :])
        p.ts('dve', gmask[:, :], grp[:, :], g8[:, 3:4], None, ALU.is_ge)
        p.ts('dve', nm[:, :], gmask[:, :], -1.0, 1.0e30, ALU.add, ALU.mult)
        for g in range(8):
            p.ts('dve', selm[:, g * 8:(g + 1) * 8], sel[:, g * 8:(g + 1) * 8], gmask[:, g:g + 1], nm[:, g:g + 1],
                 ALU.mult, ALU.add)
        p.max8(t8[:, :], selm[:, :])
        p.ts('dve', selm[:, :], selm[:, :], t8[:, 7:8], None, ALU.is_ge)
        p.tt('dve', selm[:, :], selm[:, :], sc[:, :], ALU.mult)
        p.reduce('dve', den[:, :], selm[:, :], ALU.add)
        p.recip(den[:, :], den[:, :])
        p.ts('dve', gates[:, ti, 0:NE], selm[:, :], den[:, 0:1], ROUTED_SCALE, ALU.mult, ALU.mult)

    def do_block(bi, t0, N):
        NTL = N // 128
        w0 = wgu.get()
        w1 = wgu.get()
        ogb, zb, sq = w0, w0, w1
        p.dma('sp', bufA[:, 0:8, 0:N], attn[:, :, t0:t0 + N])
        for q in range(2):
            for r in range(2):
                p.dma('pool', ogb[q * 64:(q + 1) * 64, r * 4:(r + 1) * 4, 0:N], ogg[q, :, r, :, t0:t0 + N])
                p.dma('pool', w1[q * 64:(q + 1) * 64, 8 + r * 4:8 + (r + 1) * 4, 0:N], ogg[q, :, r, :, TOK + t0:TOK + t0 + N])
        blend(p, ogb[:, 0:8, 0:N], ogb[:, 0:8, 0:N], w1[:, 8:16, 0:N], par)
        p.dma('sp', zb[:, 8:16, 0:N], zT[:, :, t0:t0 + N])
        p.dma('pool', xb[:, :, 0:N], xT[:, :, t0:t0 + N])
        p.act(sq[:, 0:8, 0:N], ogb[:, 0:8, 0:N], AF.Square)
        p.act(zb[:, 8:16, 0:N], zb[:, 8:16, 0:N], AF.Silu)
        for h in range(H):
            ps = pp.get()
            p.mm(ps[:, 0:N], ones[:, :], sq[:, h, 0:N])
            rstd_from_ps(p, ps, N, rstd, DV)
            p.stt('dve', bufA[:, 8 + h, 0:N], ogb[:, h, 0:N], gn_t[:, 0:1], rstd[:, 0:N], ALU.mult, ALU.mult)
            p.tt('dve', bufA[:, 8 + h, 0:N], bufA[:, 8 + h, 0:N], zb[:, 8 + h, 0:N], ALU.mult)
        for m in range(16):
            wt_ = wdr.get()
            wo = wt_.re("p a (c n) -> p (a c) n", n=128)
            p.dma(p.dq(), wo[:, 0:16, :], w_out[:, :, m * 128:(m + 1) * 128])
            ps = pp.get()
            for kc in range(16):
                p.mm(ps[:, 0:N], wo[:, kc, :], bufA[:, kc, 0:N], kc == 0, kc == 15)
            p.copy('act' if m % 2 == 0 else 'dve', yb[:, m, 0:N], ps[:, 0:N])
        rms_rstd(p, pp, yb, 0, 16, N, ones, bufA, rstd, D)
        for c in range(16):
            p.stt('dve', yb[:, c, 0:N], yb[:, c, 0:N], mod_t[:, 0, bi, c:c + 1], rstd[:, 0:N], ALU.mult, ALU.mult)
            p.tt('pool', xb[:, c, 0:N], xb[:, c, 0:N], yb[:, c, 0:N], ALU.add)
        rms_rstd(p, pp, xb, 0, 16, N, ones, bufA, rstd, D)
        for c in range(16):
            p.stt('dve', bufA[:, c, 0:N], xb[:, c, 0:N], mod_t[:, 1, bi, c:c + 1], rstd[:, 0:N], ALU.mult, ALU.mult)
            p.act(bufA[:, c, 0:N], bufA[:, c, 0:N], AF.Identity, bias=mod_t[:, 2, bi, c:c + 1])
        for ti in range(NTL):
            router(ti)
        for e in range(nexp):
            wgu_e, wd_e = wgu.get(), wdr.get()
            p.dma('sp', wgu_e[:, :, 0:EFF], wg[e, :, :, :])
            p.dma('pool', wgu_e[:, :, EFF:2 * EFF], wu[e, :, :, :])
            p.dma(p.dq(), wd_e[:, :, :], wd[e, :, :, :])
            for f in range(2):
                psg, psu = pp.get(), pp.get()
                for kc in range(16):
                    p.mm(psg[:, 0:N], wgu_e[:, kc, f * 128:(f + 1) * 128], bufA[:, kc, 0:N], kc == 0, kc == 15)
                for kc in range(16):
                    p.mm(psu[:, 0:N], wgu_e[:, kc, EFF + f * 128:EFF + (f + 1) * 128], bufA[:, kc, 0:N],
                         kc == 0, kc == 15)
                tm = tmp.get()
                p.act(tm[:, 0:N], psg[:, 0:N], AF.Silu)
                p.tt('dve', hid[:, f, 0:N], tm[:, 0:N], psu[:, 0:N], ALU.mult)
            for ti in range(NTL):
                for n4 in range(4):
                    ps = pp.get()
                    for f in range(2):
                        p.mm(ps[:, :], hid[:, f, ti * 128:(ti + 1) * 128], wd_e[:, f, n4 * 512:(n4 + 1) * 512],
                             f == 0, f == 1)
                    a = acc[:, ti, n4 * 512:(n4 + 1) * 512]
                    if e == 0:
                        p.ts('dve', a, ps[:, :], gates[:, ti, e:e + 1], None, ALU.mult)
                    else:
                        p.stt('dve', a, ps[:, :], gates[:, ti, e:e + 1], a, ALU.mult, ALU.add)
        for ti in range(NTL):
            for c in range(16):
                ps = pp.get()
                p.transpose(ps[:, 0:128], acc[:, ti, c * 128:(c + 1) * 128], ident[:, :])
                p.copy('act' if c % 2 == 0 else 'dve', bufA[:, c, ti * 128:(ti + 1) * 128], ps[:, 0:128])
        w1 = wgu.get()
        rms_rstd(p, pp, bufA, 0, 16, N, ones, w1, rstd, D)
        for c in range(16):
            p.stt('dve', bufA[:, c, 0:N], bufA[:, c, 0:N], mod_t[:, 3, bi, c:c + 1], rstd[:, 0:N], ALU.mult, ALU.mult)
            p.tt('pool', xb[:, c, 0:N], xb[:, c, 0:N], bufA[:, c, 0:N], ALU.add)
        p.dma('sp', o_x[:, :, t0:t0 + N], xb[:, :, 0:N])

    for bi, (t0, N) in enumerate(BLOCKS4):
        do_block(bi, t0, N)


def build_fused(depth=DEPTH, upto=9):
    nc = new_nc()
    p = Prog(nc)
    _CCBUF.b.w = None
    _CCBUF.b.r = {}
    io = declare_io(p, depth)
    modp = p.sbp("modp", [128, depth, 96, 2])
    stage_a(p, io, depth, modp)
    p.flush(final=(upto == 1))
    for l in range(depth):
        if upto < 2:
            break
        stage_s1(p, io, l, modp)
        p.flush(final=(upto == 2))
        if upto < 3:
            break
        gather_chunks(p, io.g1_s, io.g1_g, 128, NG1)
        gather_chunks(p, io.g2_s, io.g2_g, 128, NT)
        p.flush(final=(upto == 3))
        if upto < 4:
            break
        stage_s2(p, io)
        p.flush(final=(upto == 4))
        if upto < 5:
            break
        stage_s3(p, io, l)
        p.flush()
        gather_chunks(p, io.og_s, io.og_g, 64, NPAIR * 2)
        p.flush(final=(upto == 5))
        if upto < 6:
            break
        stage_s4(p, io, l, modp, l == depth - 1)
        p.flush(final=(l == depth - 1))
    p.close()
    return nc


def _fm(v, n):
    return np.ascontiguousarray(np.asarray(v, np.float32).reshape(n, 128).T)


def _gdn_consts():
    i = np.arange(128)
    same = (i[:, None] // 64) == (i[None, :] // 64)
    PE_f = ((i[:, None] <= i[None, :]) & same).astype(np.float32)
    PS_f = ((i[:, None] > i[None, :]) & same).astype(np.float32)
    BD = same.astype(np.float32)
    CH0 = np.broadcast_to((i < 64)[:, None], (128, 128)).astype(np.float32)
    CH1 = np.broadcast_to((i >= 64)[:, None], (128, 128)).astype(np.float32)
    return np.ascontiguousarray(np.stack([PE_f, PS_f, PE_f.T, PS_f.T, BD, CH0, CH1, np.eye(128, dtype=np.float32)], 0))


def _rope_tables():
    s = np.arange(SEQ)
    r = (s // GRID_W).astype(np.float32)
    col = (s % GRID_W).astype(np.float32)
    half = ROPE // 2
    inv = np.power(np.float32(10000.0), -np.arange(0, half, 2, dtype=np.float32) / np.float32(half)).astype(np.float32)
    ar = r[:, None] * inv
    ac = col[:, None] * inv
    ang = np.concatenate([ar, ar, ac, ac], -1).astype(np.float32)
    cos = np.concatenate([np.ones((CTX, ROPE), np.float32), np.cos(ang).astype(np.float32)], 0)
    sin = np.concatenate([np.zeros((CTX, ROPE), np.float32), np.sin(ang).astype(np.float32)], 0)
    return cos, sin


def _rot_mat():
    R = np.zeros((64, 64), np.float32)
    for i in range(16):
        R[16 + i, i] = -1
        R[i, 16 + i] = 1
        R[48 + i, 32 + i] = -1
        R[32 + i, 48 + i] = 1
    return R


_NC = {}


def kernel(x, c, ctx, c_ctx, ada_w, ada_b, norm_mix_pre, norm_mix_post, norm_ffn_pre, norm_ffn_post,
           w_in, mla_q_norm, mla_w_q_up, mla_kv_norm, mla_w_kv_up, gdn_conv, gdn_a_log, gdn_dt_bias,
           gdn_norm, w_out, router_w, router_bias, exp_w_gate, exp_w_up, exp_w_down,
           sh_w_gate, sh_w_up, sh_w_down, depth=DEPTH):
    f = np.float32
    A = lambda a: np.ascontiguousarray(np.asarray(a, dtype=f)[:depth])
    x, c, ctx, c_ctx = [np.asarray(a, dtype=f) for a in (x, c, ctx, c_ctx)]
    L = depth
    cos, sin = _rope_tables()
    islat = np.ones(BTOK, f)
    islat[:CTX] = 0
    shared = dict(
        dr_rmat=_rot_mat(), dr_kmask=np.ascontiguousarray((-MASK_BIG * islat)[None]), dr_cst=_gdn_consts(),
        dr_ada_w=A(ada_w), dr_ada_b=np.ascontiguousarray(A(ada_b).reshape(L, 96, 128).transpose(0, 2, 1)),
        dr_gvec=np.ascontiguousarray(np.stack([A(v).reshape(L, 16, 128).transpose(0, 2, 1) for v in
                                               (norm_mix_pre, norm_mix_post, norm_ffn_pre, norm_ffn_post)], 2)),
        dr_w_in=A(w_in),
        dr_gq=np.ascontiguousarray(A(mla_q_norm).reshape(L, 4, 128).transpose(0, 2, 1)),
        dr_gkv=np.ascontiguousarray(A(mla_kv_norm).reshape(L, 2, 128).transpose(0, 2, 1)),
        dr_gn=np.ascontiguousarray(A(gdn_norm).reshape(L, 128, 1)),
        dr_w_out=A(w_out), dr_rw=A(router_w),
        dr_rb=np.ascontiguousarray(np.broadcast_to(A(router_bias)[:, None, :], (L, 128, NE))),
        dr_wg=np.ascontiguousarray(np.concatenate([A(exp_w_gate), A(sh_w_gate)[:, None]], 1)),
        dr_wu=np.ascontiguousarray(np.concatenate([A(exp_w_up), A(sh_w_up)[:, None]], 1)),
        dr_wd=np.ascontiguousarray(np.concatenate([A(exp_w_down), A(sh_w_down)[:, None]], 1)),
    )
    wq = A(mla_w_q_up).reshape(L, QR, H, QK)
    shared["dr_wq"] = np.ascontiguousarray(np.concatenate([wq[..., :NOPE].reshape(L, QR, -1), wq[..., NOPE:].reshape(L, QR, -1)], 2))
    wkv = A(mla_w_kv_up).reshape(L, KVR, H, NOPE + VD)
    shared["dr_wkv"] = np.ascontiguousarray(np.concatenate([wkv[..., :NOPE].reshape(L, KVR, -1), wkv[..., NOPE:].reshape(L, KVR, -1)], 2))
    conv, alog, dtb = A(gdn_conv), A(gdn_a_log), A(gdn_dt_bias)
    per_par = []
    for hf in range(2):
        heads = [hf * NPAIR + i for i in range(NPAIR)]
        cw = np.stack([np.stack([np.stack([conv[l][:, w * 1024 + h * 128:w * 1024 + (h + 1) * 128].T for w in range(3)], 1)
                                 for h in heads], 0) for l in range(L)], 0)
        alog_g = np.zeros((L, 128, 16), f)
        dtb_g = np.zeros((L, 128, 16), f)
        for pi, h in enumerate(heads):
            for d in range(2):
                alog_g[:, :, pi * 4 + d * 2 + 1] = alog[:, d, h][:, None]
                dtb_g[:, :, pi * 4 + d * 2 + 1] = dtb[:, d, h][:, None]
        bsel1 = np.zeros((128, NB, 2), f)
        bsel4 = np.zeros((128, NB4, 2), f)
        bsel1[:, :, 1] = 1
        bsel4[:, :, 1] = 1
        if hf == 0:
            bsel1[:, 0, :] = (1, 0)
            bsel4[:, 0, :] = (1, 0)
        par = np.zeros((128, 2), f)
        par[:, hf] = 1
        qmask = np.zeros((1, TOK), f)
        if hf == 0:
            qmask[0, :CTX] = 1
        per_par.append(dict(dr_cw=np.ascontiguousarray(cw), dr_alog=alog_g, dr_dtb=dtb_g, dr_bsel1=bsel1, dr_bsel4=bsel4,
                            dr_par=par, dr_qmask=qmask,
                            dr_cosT=np.ascontiguousarray(cos[hf * TOK:(hf + 1) * TOK].T),
                            dr_sinT=np.ascontiguousarray(sin[hf * TOK:(hf + 1) * TOK].T)))
    in_maps = []
    for k in range(8):
        b, hf = k // 2, k % 2
        tok = np.concatenate([ctx[b], x[b, :TOK - CTX]], 0) if hf == 0 else x[b, TOK - CTX:]
        c2 = np.stack([c[b], c_ctx], 1)
        m = dict(shared)
        m.update(per_par[hf])
        m["dr_x0T"] = np.ascontiguousarray(tok.T)
        m["dr_cT2"] = np.ascontiguousarray(c2.reshape(16, 128, 2).transpose(1, 0, 2))
        in_maps.append(m)
    if depth not in _NC:
        _NC[depth] = build_fused(depth)
    res = run_bass_kernel_spmd(_NC[depth], in_maps, core_ids=list(range(8))).results
    out = np.empty((BATCH, SEQ, D), f)
    for b in range(BATCH):
        out[b, :TOK - CTX] = res[2 * b]["dr_o_x"][:, CTX:].T
        out[b, TOK - CTX:] = res[2 * b + 1]["dr_o_x"].T
    return out
```

```python
import contextlib
import numpy as np
import concourse.bass as bass
import concourse.mybir as mybir
from concourse.bass_utils import run_bass_kernel_spmd

F32 = mybir.dt.float32
F32R = mybir.dt.float32r
ALU = mybir.AluOpType
AF = mybir.ActivationFunctionType
AX = mybir.AxisListType

ENGS = ['pe', 'act', 'dve', 'pool', 'sp']

D = 2048
BATCH = 4
SEQ = 4096
DEPTH = 4
GRID_W = 64
CTX = 256
EPS = 1e-6
H = 8
QR = 512
KVR = 256
NOPE = 128
ROPE = 64
VD = 128
QK = NOPE + ROPE
DK = 128
DV = 128
CONV = 5
NE = 64
EFF = 256
IN_W = 4960
O_Q, O_KV, O_KPE, O_QKV, O_Z, O_BA = 0, 512, 768, 832, 3904, 4928
TOK = 2176
NT = 17
BTOK = CTX + SEQ
BLOCKS = [(0, 256), (256, 512), (768, 512), (1280, 512), (1792, 384)]
NB = len(BLOCKS)


class Buf:
    __slots__ = ('name', 'w', 'r', 'excl')

    def __init__(self, name=''):
        self.name = name
        self.w = None
        self.r = {}
        self.excl = False


class V:
    __slots__ = ('tile', 'ap')

    def __init__(self, tile, ap):
        self.tile = tile
        self.ap = ap


class T:
    def __init__(self, t, name, buf=None, track=True):
        self.t = t
        self.name = name
        self.b = buf if buf is not None else Buf(name)
        self.track = track

    def __getitem__(self, idx):
        return V(self, self.t[idx])

    def re(self, pat, **kw):
        base = self.t if hasattr(self.t, 'rearrange') else self.t[:]
        return T(base.rearrange(pat, **kw), self.name, self.b, self.track)


def _tiles(vs):
    out = []
    for v in vs:
        if isinstance(v, V) and v.tile.track and v.tile not in out:
            out.append(v.tile)
    return out


def _ap(x):
    return x.ap if isinstance(x, V) else x


class Prog:
    def __init__(self, nc, same_engine_sync=True):
        self.nc = nc
        self.ops = {e: [] for e in ENGS}
        self.cnt = {}
        self.seen = {e: {} for e in ENGS}
        self.same = same_engine_sync
        self.st = contextlib.ExitStack()
        self.scope = contextlib.ExitStack()
        self.sems = {}
        self.base = {}
        self.stage = 0
        self.rr = 0

    def sb(self, name, shape, dtype=F32):
        return T(self.scope.enter_context(self.nc.sbuf_tensor("%s_s%d" % (name, self.stage), list(shape), dtype)), name)

    def sbp(self, name, shape, dtype=F32):
        return T(self.st.enter_context(self.nc.sbuf_tensor(name, list(shape), dtype)), name)

    def ps(self, name, shape, dtype=F32):
        t = T(self.scope.enter_context(self.nc.psum_tensor("%s_s%d" % (name, self.stage), list(shape), dtype)), name)
        t.b.excl = True
        return t

    def idram(self, name, shape, dtype=F32):
        return T(self.nc.dram_tensor(name, list(shape), dtype).ap(), 'dr_' + name, track=False)

    def dram(self, name, shape, kind, dtype=F32, track=False):
        return T(self.nc.dram_tensor(name, list(shape), dtype, kind=kind).ap(), name, track=track)

    def op(self, eng, fn, reads=(), writes=(), dma=None, inc=None):
        waits = {}
        seen = self.seen[eng]

        def need(dep):
            if dep is None:
                return
            k, v = dep
            if k == eng and (eng == 'pe' or not self.same):
                return
            if seen.get(k, 0) >= v:
                return
            if waits.get(k, 0) < v:
                waits[k] = v

        for t in reads:
            need(t.b.w)
            if t.b.excl:
                for k, v in t.b.r.items():
                    if k != eng:
                        need((k, v))
        for t in writes:
            need(t.b.w)
            for k, v in t.b.r.items():
                need((k, v))
        for k, v in waits.items():
            seen[k] = v
        key = dma if dma else eng
        inc = inc if inc is not None else (16 if dma else 1)
        c = self.cnt.get(key, 0) + inc
        self.cnt[key] = c
        for t in reads:
            t.b.r[key] = c
        for t in writes:
            t.b.w = (key, c)
            t.b.r = {}
        self.ops[eng].append((list(waits.items()), fn, key, inc))

    def dma(self, eng, out, in_):
        sbside = in_.tile if out.tile.name.startswith('dr_') else out.tile
        key = 'd_' + sbside.name + '_' + eng
        o, i = out.ap, in_.ap
        self.op(eng, lambda e: e.dma_start(out=o, in_=i), _tiles([in_]), _tiles([out]), dma=key)

    def dq(self):
        self.rr += 1
        return ('sp', 'pool')[self.rr % 2]

    def mm(self, out, lhsT, rhs, start=True, stop=True):
        o, l, r = out.ap, lhsT.ap, rhs.ap
        self.op('pe', lambda e: e.matmul(o, l, r, start=start, stop=stop), _tiles([lhsT, rhs]), _tiles([out]))

    def transpose(self, out, in_, ident):
        self.mm(out, in_, ident)

    def act(self, out, in_, func, bias=None, scale=1.0, eng='act'):
        o, i, b, s = out.ap, in_.ap, _ap(bias), _ap(scale)
        if b is None:
            self.op('act', lambda e: e.activation(out=o, in_=i, func=func, scale=s), _tiles([in_, scale]), _tiles([out]))
        else:
            self.op('act', lambda e: e.activation(out=o, in_=i, func=func, bias=b, scale=s),
                    _tiles([in_, bias, scale]), _tiles([out]))

    def copy(self, eng, out, in_):
        o, i = out.ap, in_.ap
        if eng == 'act':
            self.op('act', lambda e: e.copy(out=o, in_=i), _tiles([in_]), _tiles([out]))
        else:
            self.op(eng, lambda e: e.tensor_copy(out=o, in_=i), _tiles([in_]), _tiles([out]))

    def tt(self, eng, out, in0, in1, op):
        o, a, b = out.ap, in0.ap, in1.ap
        self.op(eng, lambda e: e.tensor_tensor(out=o, in0=a, in1=b, op=op), _tiles([in0, in1]), _tiles([out]))

    def ts(self, eng, out, in0, s1, s2, op0, op1=None):
        o, a, x1, x2 = out.ap, in0.ap, _ap(s1), _ap(s2)
        if op1 is None:
            self.op(eng, lambda e: e.tensor_scalar(out=o, in0=a, scalar1=x1, scalar2=None, op0=op0),
                    _tiles([in0, s1]), _tiles([out]))
        else:
            self.op(eng, lambda e: e.tensor_scalar(out=o, in0=a, scalar1=x1, scalar2=x2, op0=op0, op1=op1),
                    _tiles([in0, s1, s2]), _tiles([out]))

    def stt(self, eng, out, in0, scalar, in1, op0, op1):
        o, a, s, b = out.ap, in0.ap, _ap(scalar), in1.ap
        self.op(eng, lambda e: e.scalar_tensor_tensor(out=o, in0=a, scalar=s, in1=b, op0=op0, op1=op1),
                _tiles([in0, scalar, in1]), _tiles([out]))

    def memset(self, eng, out, val):
        o = out.ap
        self.op(eng, lambda e: e.memset(o, val), [], _tiles([out]))

    def recip(self, out, in_):
        o, i = out.ap, in_.ap
        self.op('dve', lambda e: e.reciprocal(out=o, in_=i), _tiles([in_]), _tiles([out]))

    def max8(self, out, in_):
        o, i = out.ap, in_.ap
        self.op('dve', lambda e: e.max(out=o, in_=i), _tiles([in_]), _tiles([out]))

    def reduce(self, eng, out, in_, op, axis=AX.X):
        o, i = out.ap, in_.ap
        self.op(eng, lambda e: e.tensor_reduce(out=o, in_=i, axis=axis, op=op), _tiles([in_]), _tiles([out]))

    def flush(self, final=False):
        nc = self.nc
        for k in self.cnt:
            if k not in self.sems:
                self.sems[k] = self.st.enter_context(nc.semaphore("s_" + k))
        sems = self.sems
        base = list(self.base.items())
        if final:
            waits = [(k, v) for k, v in self.cnt.items() if k.startswith('d_')]
            self.ops['sp'].append((waits, None, None, 0))
        ops = self.ops
        with nc.Block() as block:
            def run(e):
                def f(engobj):
                    for k, v in base:
                        engobj.wait_ge(sems[k], v)
                    for waits, fn, key, inc in ops[e]:
                        for k, v in waits:
                            engobj.wait_ge(sems[k], v)
                        if fn is not None:
                            fn(engobj).then_inc(sems[key], inc)
                return f

            block.tensor(run('pe'))
            block.scalar(run('act'))
            block.vector(run('dve'))
            block.gpsimd(run('pool'))
            block.sync(run('sp'))
        self.ops = {e: [] for e in ENGS}
        self.base = dict(self.cnt)
        self.seen = {e: dict(self.base) for e in ENGS}
        self.scope.close()
        self.scope = contextlib.ExitStack()
        self.stage += 1

    def close(self):
        self.st.close()


def new_nc():
    return bass.Bass("TRN2", target_bir_lowering=False)


class PsumPool:
    def __init__(self, p, n=8):
        self.tiles = [p.ps("psb%d" % i, [128, 512]) for i in range(n)]
        self.i = 0

    def get(self):
        t = self.tiles[self.i % len(self.tiles)]
        self.i += 1
        return t


class Ring:
    def __init__(self, tiles):
        self.tiles = tiles
        self.i = 0

    def get(self):
        t = self.tiles[self.i % len(self.tiles)]
        self.i += 1
        return t


def rstd_from_ps(p, ps, N, rstd, dim):
    p.ts('dve', rstd[:, 0:N], ps[:, 0:N], 1.0 / dim, EPS, ALU.mult, ALU.add)
    p.act(rstd[:, 0:N], rstd[:, 0:N], AF.Sqrt)
    p.recip(rstd[:, 0:N], rstd[:, 0:N])


def rms_rstd(p, pp, src, c0, nch, N, ones, sq, rstd, dim):
    p.act(sq[:, 0:nch, 0:N], src[:, c0:c0 + nch, 0:N], AF.Square)
    ps = pp.get()
    for c in range(nch):
        p.mm(ps[:, 0:N], ones[:, :], sq[:, c, 0:N], c == 0, c == nch - 1)
    rstd_from_ps(p, ps, N, rstd, dim)


IN_, OUT_ = "ExternalInput", "ExternalOutput"


IN_, OUT_ = "ExternalInput", "ExternalOutput"
G1R = H * 128 + 128 + 3072
G2C = H * 128 + 32
R_KP = H * 128
R_QKV = H * 128 + 128
NG1 = G1R // 128
BLOCKS4 = [(0, 256)] + [(256 + i * 384, 384) for i in range(5)]
NB4 = len(BLOCKS4)
NEX = NE + 1
NEXD = NEX
ROUTED_SCALE = 2.5
MASK_BIG = 1.0e5
NPAIR = 4
NTB = BTOK // 128
SEGS = [(0, CTX), (CTX, BTOK)]
FWD_ORDER = list(range(NTB))
BWD_ORDER = [1, 0] + list(range(NTB - 1, 1, -1))
PAIR_GROUPS = [[0, 1], [2, 3], [4, 5], [6, 7]]


def sub(t, idx):
    return T(t.t[idx], t.name, t.b, t.track)


class IO:
    pass


def declare_io(p, depth):
    io = IO()

    def d(name, shape):
        return p.dram("dr_" + name, shape, IN_)

    io.x0T = d("x0T", [D, TOK]).re("(c p) n -> p c n", p=128)
    io.cT2 = d("cT2", [128, 16, 2])
    io.bsel1 = d("bsel1", [128, NB, 2])
    io.bsel4 = d("bsel4", [128, NB4, 2])
    io.par = d("par", [128, 2])
    io.cosT, io.sinT = d("cosT", [64, TOK]), d("sinT", [64, TOK])
    io.rmat = d("rmat", [64, 64])
    io.qmask, io.kmask = d("qmask", [1, TOK]), d("kmask", [1, BTOK])
    io.cst = d("cst", [8, 128, 128])
    io.ada_w = d("ada_w", [depth, D, 6 * D])
    io.ada_b = d("ada_b", [depth, 128, 96])
    io.gvec = d("gvec", [depth, 128, 4, 16])
    io.w_in = d("w_in", [depth, D, IN_W])
    io.gq = d("gq", [depth, 128, 4])
    io.wq = d("wq", [depth, QR, H * QK])
    io.gkv = d("gkv", [depth, 128, 2])
    io.wkv = d("wkv", [depth, KVR, 2 * H * 128])
    io.cw = d("cw", [depth, NPAIR, 128, 3, 5])
    io.alog = d("alog", [depth, 128, 16])
    io.dtb = d("dtb", [depth, 128, 16])
    io.gn = d("gn", [depth, 128, 1])
    io.w_out = d("w_out", [depth, D, D])
    io.rw = d("rw", [depth, D, NE])
    io.rb = d("rb", [depth, 128, NE])
    io.wg = d("wg", [depth, NEXD, D, EFF])
    io.wu = d("wu", [depth, NEXD, D, EFF])
    io.wd = d("wd", [depth, NEXD, EFF, D])
    io.o_x = p.dram("dr_o_x", [D, TOK], OUT_).re("(c p) n -> p c n", p=128)
    io.xs = p.idram("xs", [D, TOK]).re("(c p) n -> p c n", p=128)
    io.qn_s = p.idram("qn_s", [H, 128, TOK])
    io.qp_s = p.idram("qp_s", [H, 64, TOK])
    io.z_s = p.idram("z_s", [H, 128, TOK])
    io.attn_s = p.idram("attn_s", [H, 128, TOK])
    io.g1_s = p.idram("g1_s", [G1R, TOK])
    io.g1_g = p.idram("g1_g", [2 * G1R, TOK])
    io.g2_s = p.idram("g2_s", [TOK, G2C])
    io.g2_g = p.idram("g2_g", [BTOK, G2C])
    io.og_s = p.idram("og_s", [NPAIR * 128, BTOK])
    io.og_g = p.idram("og_g", [2 * NPAIR * 128, BTOK])
    return io


_CCBUF = T(None, 'ccbuf')


def all_gather(p, snd, rcv):
    s_ap, r_ap = snd.t.opt(), rcv.t.opt()
    p.op('pool', lambda e: e.collective_compute("AllGather", ALU.bypass, replica_groups=PAIR_GROUPS,
                                                ins=[s_ap], outs=[r_ap]), writes=[_CCBUF], dma='cc', inc=1)


def gather_chunks(p, snd, rcv, rows, nch):
    for c in range(nch):
        all_gather(p, sub(snd, slice(c * rows, (c + 1) * rows)), sub(rcv, slice(c * 2 * rows, (c + 1) * 2 * rows)))


def blend(p, out, a, b, par):
    p.ts('dve', out, a, par[:, 0:1], None, ALU.mult)
    p.stt('dve', out, b, par[:, 1:2], out, ALU.mult, ALU.add)


def blockvecs(p, dst, modp, l, i, bsel, nblk, tmp):
    own, cx = modp[:, l, i * 16:(i + 1) * 16, 0], modp[:, l, i * 16:(i + 1) * 16, 1]
    for bi in range(nblk):
        p.ts('dve', tmp[:, 0:16], own, bsel[:, bi, 1:2], None, ALU.mult)
        p.stt('dve', dst[:, bi, :], cx, bsel[:, bi, 0:1], tmp[:, 0:16], ALU.mult, ALU.add)


def stage_a(p, io, depth, modp):
    pp = PsumPool(p, 4)
    c_t = p.sb("c_t", [128, 16, 2])
    ab_t = p.sb("ab_t", [128, depth, 96])
    wt = Ring([p.sb("awt%d" % i, [128, 16, 512]) for i in range(3)])
    p.dma('sp', c_t[:, :, :], io.cT2[:, :, :])
    p.dma('sp', ab_t[:, :, :], io.ada_b.re("l p j -> p l j")[:, :, :])
    p.act(c_t[:, :, :], c_t[:, :, :], AF.Silu)
    for l in range(depth):
        aw = sub(io.ada_w, l).re("(c p) n -> p c n", p=128)
        for g in range(24):
            w = wt.get()
            p.dma(p.dq(), w[:, :, :], aw[:, :, g * 512:(g + 1) * 512])
            for jj in range(4):
                j = g * 4 + jj
                ps = pp.get()
                for kc in range(16):
                    p.mm(ps[:, 0:2], w[:, kc, jj * 128:(jj + 1) * 128], c_t[:, kc, :], kc == 0, kc == 15)
                p.act(modp[:, l, j, :], ps[:, 0:2], AF.Identity, bias=ab_t[:, l, j:j + 1])


def stage_s1(p, io, l, modp):
    xT = io.x0T if l == 0 else io.xs
    w_in = sub(io.w_in, l).re("(c p) n -> p c n", p=128)
    wq = sub(io.wq, l).re("(c p) n -> p c n", p=128)
    wkv = sub(io.wkv, l).re("(c p) n -> p c n", p=128)
    pp = PsumPool(p)
    ones = p.sb("ones", [128, 128])
    xb = p.sb("xb", [128, 16, 512])
    hb = p.sb("hb", [128, 16, 512])
    wt = Ring([p.sb("wt%d" % i, [128, 16, 320]) for i in range(2)])
    rstd = p.sb("rstd", [128, 512])
    A_t = p.sb("A_t", [128, NB, 16])
    S_t = p.sb("S_t", [128, NB, 16])
    bs_t = p.sb("bs_t", [128, NB, 2])
    tmpv = p.sb("tmpv", [128, 16])
    gv_t = p.sb("gv_t", [128, 4, 16])
    gq_t = p.sb("gq_t", [128, 4])
    gkv_t = p.sb("gkv_t", [128, 2])
    wq_t = p.sb("wq_t", [128, 4, H * QK])
    wkv_t = p.sb("wkv_t", [128, 2, 2 * H * 128])
    cos_t = p.sb("cos_t", [64, 512])
    sin_t = p.sb("sin_t", [64, 512])
    rm_t = p.sb("rm_t", [64, 64])
    lat = p.sb("lat", [128, 7, 512])
    latn = p.sb("latn", [128, 6, 512])
    lsq = xb
    rq = p.sb("rq", [128, 512])
    stg = Ring([p.sb("stg%d" % i, [128, 512]) for i in range(4)])
    pe_a = p.sb("pe_a", [64, 512])
    pe_b = p.sb("pe_b", [64, 512])
    pe_c = Ring([p.sb("pe_c%d" % i, [64, 512]) for i in range(2)])
    vst = Ring([p.sb("vst%d" % i, [128, 512]) for i in range(2)])
    bast = Ring([p.sb("bast%d" % i, [128, 32]) for i in range(2)])

    p.memset('pool', ones[:, :], 1.0)
    p.dma('sp', gv_t[:, :, :], sub(io.gvec, l)[:, :, :])
    p.dma('sp', bs_t[:, :, :], io.bsel1[:, :, :])
    p.dma('sp', gq_t[:, :], sub(io.gq, l)[:, :])
    p.dma('sp', gkv_t[:, :], sub(io.gkv, l)[:, :])
    p.dma('sp', rm_t[:, :], io.rmat[:, :])
    p.dma('pool', wq_t[:, :, :], wq[:, :, :])
    p.dma('pool', wkv_t[:, :, :], wkv[:, :, :])
    blockvecs(p, A_t, modp, l, 1, bs_t, NB, tmpv)
    blockvecs(p, S_t, modp, l, 0, bs_t, NB, tmpv)
    for b in range(NB):
        p.stt('dve', A_t[:, b, :], A_t[:, b, :], 1.0, gv_t[:, 0, :], ALU.add, ALU.mult)

    groups = [(0, 256), (256, 256), (512, 320)] + [(O_QKV + i * 256, 256) for i in range(12)] + \
             [(O_Z + i * 256, 256) for i in range(4)] + [(O_BA, 32)]

    def rope(src, N, dst):
        p.copy('act', pe_a[:, 0:N], src)
        ps2 = pp.get()
        p.mm(ps2[0:64, 0:N], rm_t[:, :], pe_a[:, 0:N])
        p.tt('dve', pe_b[:, 0:N], ps2[0:64, 0:N], sin_t[:, 0:N], ALU.mult)
        pc = pe_c.get()
        p.tt('pool', pc[:, 0:N], pe_a[:, 0:N], cos_t[:, 0:N], ALU.mult)
        p.tt('dve', pc[:, 0:N], pc[:, 0:N], pe_b[:, 0:N], ALU.add)
        p.dma(p.dq(), dst, pc[:, 0:N])

    def mla_up(t0, N):
        rms_rstd(p, pp, lat, 0, 4, N, ones, lsq, rq, QR)
        for c in range(4):
            p.stt('dve', latn[:, c, 0:N], lat[:, c, 0:N], gq_t[:, c:c + 1], rq[:, 0:N], ALU.mult, ALU.mult)
        rms_rstd(p, pp, lat, 4, 2, N, ones, lsq, rq, KVR)
        for c in range(2):
            p.stt('dve', latn[:, 4 + c, 0:N], lat[:, 4 + c, 0:N], gkv_t[:, c:c + 1], rq[:, 0:N], ALU.mult, ALU.mult)
        rope(lat[0:64, 6, 0:N], N, io.g1_s[R_KP:R_KP + 64, t0:t0 + N])
        for h in range(H):
            ps = pp.get()
            for kc in range(4):
                p.mm(ps[:, 0:N], wq_t[:, kc, h * 128:(h + 1) * 128], latn[:, kc, 0:N], kc == 0, kc == 3)
            s = stg.get()
            p.copy('act', s[:, 0:N], ps[:, 0:N])
            p.dma(p.dq(), io.qn_s[h, :, t0:t0 + N], s[:, 0:N])
            ps = pp.get()
            for kc in range(4):
                p.mm(ps[0:64, 0:N], wq_t[:, kc, H * 128 + h * 64:H * 128 + (h + 1) * 64], latn[:, kc, 0:N],
                     kc == 0, kc == 3)
            rope(ps[0:64, 0:N], N, io.qp_s[h, :, t0:t0 + N])
        for h in range(H):
            ps = pp.get()
            for kc in range(2):
                p.mm(ps[:, 0:N], wkv_t[:, kc, h * 128:(h + 1) * 128], latn[:, 4 + kc, 0:N], kc == 0, kc == 1)
            s = stg.get()
            p.copy('dve', s[:, 0:N], ps[:, 0:N])
            p.dma(p.dq(), io.g1_s[h * 128:(h + 1) * 128, t0:t0 + N], s[:, 0:N])
        for ti in range(N // 128):
            for half in range(2):
                vs = vst.get()
                ps = pp.get()
                for kc in range(2):
                    p.mm(ps[:, :], latn[:, 4 + kc, ti * 128:(ti + 1) * 128],
                         wkv_t[:, kc, H * 128 + half * 512:H * 128 + (half + 1) * 512], kc == 0, kc == 1)
                p.copy('act', vs[:, :], ps[:, :])
                p.dma(p.dq(), io.g2_s[t0 + ti * 128:t0 + (ti + 1) * 128, half * 512:(half + 1) * 512], vs[:, :])

    def do_block(bi, t0, N):
        p.dma('sp', xb[:, :, 0:N], xT[:, :, t0:t0 + N])
        p.dma('pool', cos_t[:, 0:N], io.cosT[:, t0:t0 + N])
        p.dma('pool', sin_t[:, 0:N], io.sinT[:, t0:t0 + N])
        rms_rstd(p, pp, xb, 0, 16, N, ones, hb, rstd, D)
        for c in range(16):
            p.stt('dve', hb[:, c, 0:N], xb[:, c, 0:N], A_t[:, bi, c:c + 1], rstd[:, 0:N], ALU.mult, ALU.mult)
            p.act(hb[:, c, 0:N], hb[:, c, 0:N], AF.Identity, bias=S_t[:, bi, c:c + 1])
        for (c0, cw) in groups:
            w = wt.get()
            p.dma(p.dq(), w[:, :, 0:cw], w_in[:, :, c0:c0 + cw])
            if c0 == O_BA:
                for ti in range(N // 128):
                    ps = pp.get()
                    for kc in range(16):
                        p.mm(ps[:, 0:32], hb[:, kc, ti * 128:(ti + 1) * 128], w[:, kc, 0:32], kc == 0, kc == 15)
                    bs = bast.get()
                    p.copy('act', bs[:, :], ps[:, 0:32])
                    p.dma('sp', io.g2_s[t0 + ti * 128:t0 + (ti + 1) * 128, H * 128:H * 128 + 32], bs[:, :])
                continue
            for m in range((cw + 127) // 128):
                mw = min(128, cw - m * 128)
                ps = pp.get()
                for kc in range(16):
                    p.mm(ps[0:mw, 0:N], w[:, kc, m * 128:m * 128 + mw], hb[:, kc, 0:N], kc == 0, kc == 15)
                col = c0 + m * 128
                if col < O_QKV:
                    p.copy('act', lat[0:mw, col // 128, 0:N], ps[0:mw, 0:N])
                else:
                    s = stg.get()
                    p.copy('act' if m % 2 == 0 else 'dve', s[:, 0:N], ps[:, 0:N])
                    if col < O_Z:
                        r0 = R_QKV + ((col - O_QKV) // 128) * 128
                        p.dma(p.dq(), io.g1_s[r0:r0 + 128, t0:t0 + N], s[:, 0:N])
                    else:
                        p.dma(p.dq(), io.z_s[(col - O_Z) // 128, :, t0:t0 + N], s[:, 0:N])
            if c0 == 512:
                mla_up(t0, N)

    for bi, (t0, N) in enumerate(BLOCKS):
        do_block(bi, t0, N)


def stage_s2(p, io):
    v = io.g2_g.re("(c r p) n -> p r c n", r=2, p=128)
    NKT = BTOK // 128
    st_ps = Ring([p.ps("st_ps%d" % i, [128, 512]) for i in range(4)])
    ot_ps = Ring([p.ps("ot_ps%d" % i, [128, 512]) for i in range(2)])
    su_ps = Ring([p.ps("su_ps%d" % i, [128, 512]) for i in range(2)])
    ones = p.sb("ones", [128, 128])
    kn_t = Ring([p.sb("kn_t%d" % i, [128, BTOK]) for i in range(2)])
    kp_t = p.sb("kp_t", [65, BTOK])
    v_t = Ring([p.sb("v_t%d" % i, [128, NKT, 128]) for i in range(2)])
    qn_t = Ring([p.sb("qn_t%d" % i, [128, TOK]) for i in range(2)])
    qp_t = Ring([p.sb("qp_t%d" % i, [65, TOK]) for i in range(2)])
    pt = Ring([p.sb("pt%d" % i, [128, 512]) for i in range(4)])
    rs = p.sb("rs", [128, 512])
    og = Ring([p.sb("og%d" % i, [128, 512]) for i in range(2)])
    p.memset('pool', ones[:, :], 1.0)
    for r in range(2):
        p.dma('sp', kp_t[0:64, r * TOK:(r + 1) * TOK], io.g1_g[H * 256 + r * 128:H * 256 + r * 128 + 64, :])
    p.dma('sp', kp_t[64:65, :], io.kmask[:, :])
    scale = float(QK) ** -0.5
    for h in range(H):
        knh, vh, qnh, qph = kn_t.get(), v_t.get(), qn_t.get(), qp_t.get()
        for r in range(2):
            p.dma('sp', knh[:, r * TOK:(r + 1) * TOK], io.g1_g[h * 256 + r * 128:h * 256 + (r + 1) * 128, :])
        vh4 = vh.re("p (r c) n -> p r c n", r=2)
        for r in range(2):
            p.dma('pool', vh4[:, r, :, :], v[:, r, :, h * 128:(h + 1) * 128])
        p.dma('sp', qnh[:, :], io.qn_s[h, :, :])
        p.dma('pool', qph[0:64, :], io.qp_s[h, :, :])
        p.dma('pool', qph[64:65, :], io.qmask[:, :])
        for (t0, N) in BLOCKS:
            ot, su = ot_ps.get(), su_ps.get()
            for kt in range(NKT):
                st = st_ps.get()
                p.mm(st[:, 0:N], knh[:, kt * 128:(kt + 1) * 128], qnh[:, t0:t0 + N], True, False)
                p.mm(st[:, 0:N], kp_t[:, kt * 128:(kt + 1) * 128], qph[:, t0:t0 + N], False, True)
                pk = pt.get()
                p.act(pk[:, 0:N], st[:, 0:N], AF.Exp, scale=scale)
                p.mm(ot[:, 0:N], vh[:, kt, :], pk[:, 0:N], kt == 0, kt == NKT - 1)
                p.mm(su[:, 0:N], ones[:, :], pk[:, 0:N], kt == 0, kt == NKT - 1)
            p.recip(rs[:, 0:N], su[:, 0:N])
            o = og.get()
            p.tt('dve', o[:, 0:N], ot[:, 0:N], rs[:, 0:N], ALU.mult)
            p.dma(p.dq(), io.attn_s[h, :, t0:t0 + N], o[:, 0:N])


class QPsum:
    def __init__(self, p, nbanks=8):
        self.q = []
        for i in range(nbanks):
            bank = p.ps("qb%d" % i, [128, 512])
            for j in range(4):
                self.q.append(T(bank.t[:, j * 128:(j + 1) * 128], "qb%d_%d" % (i, j), buf=bank.b))
        self.i = 0

    def get(self):
        t = self.q[self.i % len(self.q)]
        self.i += 1
        return t


def stage_s3(p, io, l):
    dbg = 0
    cw = sub(io.cw, l)
    qp_ = QPsum(p, 7)
    big = PsumPool(p, 1)
    C = p.sb("C", [128, 8, 128])
    ones = p.sb("ones", [128, 128])
    par = p.sb("par", [128, 2])
    p.dma('sp', C[:, :, :], io.cst.re("c p n -> p c n")[:, :, :])
    p.dma('sp', par[:, :], io.par[:, :])
    p.memset('pool', ones[:, :], 1.0)
    PEm = [C[:, 0, :], C[:, 2, :]]
    PSm = [C[:, 1, :], C[:, 3, :]]
    BD, CH, ident = C[:, 4, :], [C[:, 5, :], C[:, 6, :]], C[:, 7, :]
    bat = p.sb("bat", [128, NTB, NPAIR * 4])
    baf = p.sb("baf", [128, NTB, 32])
    baf4 = baf.re("p (r c) n -> p r c n", r=2)
    for rk in range(2):
        p.dma('sp', baf4[:, rk, :, :], io.g2_g.re("(c r p) n -> p r c n", r=2, p=128)[:, rk, :, H * 128:H * 128 + 32])
    for pi in range(NPAIR):
        for d in range(2):
            for k2 in range(2):
                ca = k2 * 16 + d * 8 + pi
                blend(p, bat[:, :, pi * 4 + d * 2 + k2], baf[:, :, ca], baf[:, :, ca + 4], par)
    alog_t = p.sb("alog_t", [128, 16])
    dtb_t = p.sb("dtb_t", [128, 16])
    p.dma('sp', alog_t[:, :], sub(io.alog, l)[:, :])
    p.dma('sp', dtb_t[:, :], sub(io.dtb, l)[:, :])
    nea = p.sb("nea", [128, 16])
    p.act(nea[:, :], alog_t[:, :], AF.Exp)
    p.ts('dve', nea[:, :], nea[:, :], -1.0, None, ALU.mult)
    gb = p.sb("gb", [128, NTB, NPAIR * 4])
    for t in range(NTB):
        p.tt('pool', gb[:, t, :], bat[:, t, :], dtb_t[:, :], ALU.add)
    p.act(gb[:, :, :], gb[:, :, :], AF.Exp)
    p.act(gb[:, :, :], gb[:, :, :], AF.Ln, bias=1.0)
    for t in range(NTB):
        p.tt('pool', gb[:, t, :], gb[:, t, :], nea[:, :], ALU.mult)
    sig = p.sb("sig", [128, NTB, NPAIR * 4])
    p.act(sig[:, :, :], bat[:, :, :], AF.Sigmoid)

    rawt = Ring([p.sb("rawt%d" % i, [128, BTOK]) for i in range(2)])
    cv = p.sb("cv", [128, 3, BTOK])
    cwt = p.sb("cwt", [128, 3, 5])
    sq = p.sb("sq", [128, 512])
    rn = p.sb("rn", [128, 512])
    oT = p.sb("oT", [128, BTOK])
    vec = {}
    for d in range(2):
        for nm in ("gc", "bg", "kd", "nb", "be", "g"):
            vec[(nm, d)] = p.sb("v_%s%d" % (nm, d), [128, NTB])
        vec[("egl", d)] = p.sb("v_egl%d" % d, [128, NTB, 2])
    S = [p.sb("S%d" % d, [128, 128]) for d in range(2)]

    def ring(nm, n=2):
        return [Ring([p.sb("%s%d_%d" % (nm, d, i), [128, 128]) for i in range(n)]) for d in range(2)]

    r_gpe, r_dm, r_dmt, r_egcb, r_t1, r_t2 = ring("gpe"), ring("dm"), ring("dmt"), ring("egcb"), ring("t1"), ring("t2")
    r_M, r_Mt, r_Pt = ring("M", 3), ring("Mt", 3), ring("Pt", 3)
    r_qkdt, r_kbg, r_kdec, r_vb, r_wT, r_u, r_qgT, r_vnew = (ring("qkdt"), ring("kbg"), ring("kdec"), ring("vb"),
                                                            ring("wT"), ring("u"), ring("qgT"), ring("vnew"))

    def prep_pair(pi):
        p.dma('sp', cwt[:, :, :], cw[pi, :, :, :])
        for w3 in range(3):
            r, r2 = rawt.get(), rawt.get()
            for rk in range(2):
                ra = (R_QKV // 128 + w3 * 8 + pi) * 256 + rk * 128
                p.dma('sp', r[:, rk * TOK:(rk + 1) * TOK], io.g1_g[ra:ra + 128, :])
                p.dma('pool', r2[:, rk * TOK:(rk + 1) * TOK], io.g1_g[ra + 1024:ra + 1152, :])
            blend(p, r[:, :], r[:, :], r2[:, :], par)
            eng = 'dve'
            p.ts(eng, cv[:, w3, :], r[:, :], cwt[:, w3, 2:3], None, ALU.mult)
            for s in (-2, -1, 1, 2):
                for (a, b) in SEGS:
                    lo, hi = max(a, a - s), min(b, b - s)
                    p.stt(eng, cv[:, w3, lo:hi], r[:, lo + s:hi + s], cwt[:, w3, s + 2:s + 3], cv[:, w3, lo:hi],
                          ALU.mult, ALU.add)
            p.act(cv[:, w3, :], cv[:, w3, :], AF.Silu)
        for w3 in range(2):
            for c0 in range(0, BTOK, 512):
                n = min(512, BTOK - c0)
                p.act(sq[:, 0:n], cv[:, w3, c0:c0 + n], AF.Square)
                ps = big.get()
                p.mm(ps[:, 0:n], ones[:, :], sq[:, 0:n])
                if w3 == 0:
                    p.ts('dve', rn[:, 0:n], ps[:, 0:n], EPS, float(DK), ALU.add, ALU.mult)
                else:
                    p.ts('dve', rn[:, 0:n], ps[:, 0:n], EPS, None, ALU.add)
                p.act(rn[:, 0:n], rn[:, 0:n], AF.Sqrt)
                p.recip(rn[:, 0:n], rn[:, 0:n])
                p.tt('dve', cv[:, w3, c0:c0 + n], cv[:, w3, c0:c0 + n], rn[:, 0:n], ALU.mult)
        p.memset('pool', oT[:, :], 0.0)
        for d in range(2):
            cb = pi * 4 + d * 2
            g, be = vec[("g", d)], vec[("be", d)]
            p.copy('pool', g[:, :], gb[:, :, cb + 1])
            p.copy('pool', be[:, :], sig[:, :, cb])
            ps = qp_.get()
            p.mm(ps[:, 0:NTB], PEm[d], g[:, :])
            gc = vec[("gc", d)]
            p.copy('act', gc[:, :], ps[:, 0:NTB])
            ps2 = qp_.get()
            p.mm(ps2[:, 0:NTB], BD, g[:, :])
            kd = vec[("kd", d)]
            p.tt('dve', kd[:, :], ps2[:, 0:NTB], gc[:, :], ALU.subtract)
            p.act(kd[:, :], kd[:, :], AF.Exp)
            bg = vec[("bg", d)]
            p.act(bg[:, :], gc[:, :], AF.Exp)
            p.tt('dve', bg[:, :], bg[:, :], be[:, :], ALU.mult)
            p.ts('dve', vec[("nb", d)][:, :], be[:, :], -1.0, None, ALU.mult)
            egl = vec[("egl", d)]
            for c in range(2):
                ps3 = qp_.get()
                p.mm(ps3[:, 0:NTB], CH[c], g[:, :])
                p.act(egl[:, :, c], ps3[:, 0:NTB], AF.Exp)
            p.memset('pool', S[d][:, :], 0.0)

    def tile_dir(d, t):
        c0 = t * 128
        kT, qT, vT = cv[:, 1, c0:c0 + 128], cv[:, 0, c0:c0 + 128], cv[:, 2, c0:c0 + 128]
        g, be, nb = vec[("g", d)], vec[("be", d)], vec[("nb", d)]
        bg, kd, egl = vec[("bg", d)], vec[("kd", d)], vec[("egl", d)]
        gpe = r_gpe[d].get()
        p.ts('pool', gpe[:, :], PEm[d], g[:, t:t + 1], None, ALU.mult)
        ps_d, ps_dt, ps_gb = qp_.get(), qp_.get(), qp_.get()
        p.mm(ps_d[:, :], gpe[:, :], PSm[d])
        p.mm(ps_dt[:, :], PSm[d], gpe[:, :])
        p.mm(ps_gb[:, :], ones[:, :], gpe[:, :])
        dm, dmt, egcb = r_dm[d].get(), r_dmt[d].get(), r_egcb[d].get()
        p.act(dm[:, :], ps_d[:, :], AF.Exp)
        p.act(dmt[:, :], ps_dt[:, :], AF.Exp)
        p.act(egcb[:, :], ps_gb[:, :], AF.Exp)
        if dbg == 1:
            return
        ps_G, ps_KQ = qp_.get(), qp_.get()
        p.mm(ps_G[:, :], kT, kT)
        p.mm(ps_KQ[:, :], kT, qT)
        if dbg == 12:
            return
        t1, t2 = r_t1[d].get(), r_t2[d].get()
        p.tt('dve', t1[:, :], dm[:, :], PSm[d], ALU.mult)
        p.tt('dve', t2[:, :], dmt[:, :], PEm[d], ALU.mult)
        if dbg == 13:
            return
        M = r_M[d].get()
        p.stt('dve', M[:, :], ps_G[:, :], nb[:, t:t + 1], t1[:, :], ALU.mult, ALU.mult)
        if dbg == 14:
            return
        qkdt = r_qkdt[d].get()
        p.tt('dve', qkdt[:, :], ps_KQ[:, :], t2[:, :], ALU.mult)
        if dbg == 2:
            return
        ps_t = qp_.get()
        p.transpose(ps_t[:, :], M[:, :], ident)
        Mt = r_Mt[d].get()
        p.copy('act', Mt[:, :], ps_t[:, :])
        Pt = r_Pt[d].get()
        p.tt('dve', Pt[:, :], Mt[:, :], ident, ALU.add)
        if dbg == 21:
            return
        for lv in range(5 if dbg != 22 else 1):
            last = lv == 4
            ps_a = qp_.get()
            p.mm(ps_a[:, :], Mt[:, :], M[:, :])
            if dbg == 23:
                return
            if not last:
                ps_b = qp_.get()
                p.mm(ps_b[:, :], M[:, :], Mt[:, :])
            if dbg == 24:
                return
            M2 = r_M[d].get()
            p.copy('act', M2[:, :], ps_a[:, :])
            if dbg == 25:
                return
            if not last:
                Mt2 = r_Mt[d].get()
                p.copy('act' if dbg in (26, 27) else 'dve', Mt2[:, :], ps_b[:, :])
            if dbg in (26, 28):
                return
            ps_c = qp_.get()
            p.mm(ps_c[:, :], M2[:, :], Pt[:, :])
            Pt2 = r_Pt[d].get()
            p.tt('dve', Pt2[:, :], ps_c[:, :], Pt[:, :], ALU.add)
            M, Pt = M2, Pt2
            if not last:
                Mt = Mt2
        Xt = Pt
        if dbg == 3:
            return
        ps_k, ps_v = qp_.get(), qp_.get()
        p.transpose(ps_k[:, :], kT, ident)
        p.transpose(ps_v[:, :], vT, ident)
        kbg, kdec, vb = r_kbg[d].get(), r_kdec[d].get(), r_vb[d].get()
        p.ts('dve', kbg[:, :], ps_k[:, :], bg[:, t:t + 1], None, ALU.mult)
        p.act(kdec[:, :], ps_k[:, :], AF.Copy, scale=kd[:, t:t + 1])
        p.act(vb[:, :], ps_v[:, :], AF.Copy, scale=be[:, t:t + 1])
        ps_w, ps_u = qp_.get(), qp_.get()
        p.mm(ps_w[:, :], kbg[:, :], Xt[:, :])
        p.mm(ps_u[:, :], Xt[:, :], vb[:, :])
        wT, u, qgT = r_wT[d].get(), r_u[d].get(), r_qgT[d].get()
        p.copy('act', wT[:, :], ps_w[:, :])
        p.copy('dve', u[:, :], ps_u[:, :])
        p.tt('dve', qgT[:, :], cv[:, 0, c0:c0 + 128], egcb[:, :], ALU.mult)
        if dbg == 4:
            return
        vnew = r_vnew[d].get()
        for c in ((0, 1) if d == 0 else (1, 0)):
            r0, r1 = c * 64, c * 64 + 64
            ps_ws = qp_.get()
            p.mm(ps_ws[:, :], wT[:, :], S[d][:, :])
            p.tt('dve', vnew[r0:r1, :], u[r0:r1, :], ps_ws[r0:r1, :], ALU.subtract)
            ps_o = qp_.get()
            p.mm(ps_o[:, 0:64], S[d][:, :], qgT[:, r0:r1], True, False)
            p.mm(ps_o[:, 0:64], vnew[r0:r1, :], qkdt[r0:r1, r0:r1], False, True)
            p.tt('dve', oT[:, c0 + r0:c0 + r1], oT[:, c0 + r0:c0 + r1], ps_o[:, 0:64], ALU.add)
            ps_s = qp_.get()
            p.mm(ps_s[:, :], kdec[r0:r1, :], vnew[r0:r1, :])
            p.stt('dve', S[d][:, :], S[d][:, :], egl[:, t, c:c + 1], ps_s[:, :], ALU.mult, ALU.add)

    for pi in range(NPAIR):
        prep_pair(pi)
        for s in range(NTB):
            tile_dir(0, FWD_ORDER[s])
            tile_dir(1, BWD_ORDER[s])
        p.dma('sp', io.og_s[pi * 128:(pi + 1) * 128, :], oT[:, :])


def stage_s4(p, io, l, modp, last):
    nexp = NEXD
    attn = io.attn_s.re("h p n -> p h n")
    ogg = io.og_g.re("(pi q r i) n -> q i r pi n", q=2, r=2, i=64)
    zT = io.z_s.re("h p n -> p h n")
    xT = io.x0T if l == 0 else io.xs
    o_x = io.o_x if last else io.xs
    w_out = sub(io.w_out, l).re("(c p) n -> p c n", p=128)
    rw = sub(io.rw, l).re("(c p) n -> p c n", p=128)
    wg = sub(io.wg, l).re("e (c p) n -> e p c n", p=128)
    wu = sub(io.wu, l).re("e (c p) n -> e p c n", p=128)
    wd = sub(io.wd, l).re("e (c p) n -> e p c n", p=128)
    pp = PsumPool(p)
    NM = 384
    ones = p.sb("ones", [128, 128])
    ident = p.sb("ident", [128, 128])
    bufA = p.sb("bufA", [128, 16, NM])
    bufB = p.sb("bufB", [128, 16 * NM])
    xb = p.sb("xb", [128, 16, NM])
    wgs = Ring([p.sb("wgs%d" % i, [128, 16, 256]) for i in range(2)])
    wgr = Ring([p.sb("wgr%d" % i, [128, 16, 256], F32R) for i in range(2)])
    wds = Ring([p.sb("wds%d" % i, [128, D]) for i in range(1)])
    wdr = Ring([p.sb("wdr%d" % i, [128, D], F32R) for i in range(2)])
    h2r = p.sb("h2r", [128, 16, NM], F32R)
    hid = p.sb("hid", [128, 2, NM], F32R)
    tmp = Ring([p.sb("tmp%d" % i, [128, NM]) for i in range(1)])
    rstd = p.sb("rstd", [128, NM])
    gn_t = p.sb("gn_t", [128, 1])
    vec_t = p.sb("vec_t", [128, 4, 16])
    mod_t = p.sb("mod_t", [128, 4, NB4, 16])
    rw_t = p.sb("rw_t", [128, 16, NE])
    rb_t = p.sb("rb_t", [128, NE])
    gates = p.sb("gates", [128, 3, NEX])
    sc = p.sb("sc", [128, NE])
    sel = p.sb("sel", [128, NE])
    selm = p.sb("selm", [128, NE])
    m8 = p.sb("m8", [128, 8, 8])
    grp = p.sb("grp", [128, 8])
    g8 = p.sb("g8", [128, 8])
    gmask = p.sb("gmask", [128, 8])
    nm = p.sb("nm", [128, 8])
    t8 = p.sb("t8", [128, 8])
    den = p.sb("den", [128, 1])
    yb = bufB.re("p (c n) -> p c n", n=NM)
    acc = bufB.re("p (t d) -> p t d", d=D)

    p.memset('pool', ones[:, :], 1.0)
    p.memset('pool', gates[:, :, :], 1.0)
    par = p.sb("par", [128, 2])
    bs_t = p.sb("bs_t", [128, NB4, 2])
    tmpv = p.sb("tmpv", [128, 16])
    p.dma('sp', par[:, :], io.par[:, :])
    p.dma('sp', bs_t[:, :, :], io.bsel4[:, :, :])
    p.dma('sp', ident[:, :], io.cst[7, :, :])
    p.dma('sp', gn_t[:, :], sub(io.gn, l)[:, :])
    p.dma('sp', vec_t[:, :, :], sub(io.gvec, l)[:, :, :])
    p.dma('sp', rw_t[:, :, :], rw[:, :, :])
    p.dma('sp', rb_t[:, :], sub(io.rb, l)[:, :])
    for slot, vi in enumerate((2, 4, 3, 5)):
        blockvecs(p, sub(mod_t, (slice(None), slot)), modp, l, vi, bs_t, NB4, tmpv)
    for b in range(NB4):
        p.tt('dve', mod_t[:, 0, b, :], mod_t[:, 0, b, :], vec_t[:, 1, :], ALU.mult)
        p.stt('dve', mod_t[:, 1, b, :], mod_t[:, 1, b, :], 1.0, vec_t[:, 2, :], ALU.add, ALU.mult)
        p.tt('dve', mod_t[:, 3, b, :], mod_t[:, 3, b, :], vec_t[:, 3, :], ALU.mult)

    def router(ti):
        ps = pp.get()
        for kc in range(16):
            p.mm(ps[:, 0:NE], bufA[:, kc, ti * 128:(ti + 1) * 128], rw_t[:, kc, :], kc == 0, kc == 15)
        p.act(sc[:, :], ps[:, 0:NE], AF.Sigmoid)
        p.tt('dve', sel[:, :], sc[:, :], rb_t[:, :], ALU.add)
        for g in range(8):
            p.max8(m8[:, g, :], sel[:, g * 8:(g + 1) * 8])
        p.tt('dve', grp[:, :], m8[:, :, 0], m8[:, :, 1], ALU.add)
        p.max8(g8[:, :], grp[:, :])
        p.ts('dve', gmask[:, :], grp[:, :], g8[:, 3:4], None, ALU.is_ge)
        p.ts('dve', nm[:, :], gmask[:, :], -1.0, 1.0e30, ALU.add, ALU.mult)
        for g in range(8):
            p.ts('dve', selm[:, g * 8:(g + 1) * 8], sel[:, g * 8:(g + 1) * 8], gmask[:, g:g + 1], nm[:, g:g + 1],
                 ALU.mult, ALU.add)
        p.max8(t8[:, :], selm[:, :])
        p.ts('dve', selm[:, :], selm[:, :], t8[:, 7:8], None, ALU.is_ge)
        p.tt('dve', selm[:, :], selm[:, :], sc[:, :], ALU.mult)
        p.reduce('dve', den[:, :], selm[:, :], ALU.add)
        p.recip(den[:, :], den[:, :])
        p.ts('dve', gates[:, ti, 0:NE], selm[:, :], den[:, 0:1], ROUTED_SCALE, ALU.mult, ALU.mult)

    def do_block(bi, t0, N):
        NTL = N // 128
        ogb = wgs.get().re("p c n -> p (c n)").re("p (c n) -> p c n", n=512)
        zb8 = wgs.get().re("p c n -> p (c n)").re("p (c n) -> p c n", n=512)
        sq = yb
        w1 = yb
        p.dma('sp', bufA[:, 0:8, 0:N], attn[:, :, t0:t0 + N])
        for q in range(2):
            for r in range(2):
                p.dma('pool', ogb[q * 64:(q + 1) * 64, r * 4:(r + 1) * 4, 0:N], ogg[q, :, r, :, t0:t0 + N])
                p.dma('pool', w1[q * 64:(q + 1) * 64, 8 + r * 4:8 + (r + 1) * 4, 0:N], ogg[q, :, r, :, TOK + t0:TOK + t0 + N])
        blend(p, ogb[:, 0:8, 0:N], ogb[:, 0:8, 0:N], w1[:, 8:16, 0:N], par)
        p.dma('sp', zb8[:, 0:8, 0:N], zT[:, :, t0:t0 + N])
        p.dma('pool', xb[:, :, 0:N], xT[:, :, t0:t0 + N])
        p.act(sq[:, 0:8, 0:N], ogb[:, 0:8, 0:N], AF.Square)
        p.act(zb8[:, 0:8, 0:N], zb8[:, 0:8, 0:N], AF.Silu)
        for h in range(H):
            ps = pp.get()
            p.mm(ps[:, 0:N], ones[:, :], sq[:, h, 0:N])
            rstd_from_ps(p, ps, N, rstd, DV)
            p.stt('dve', bufA[:, 8 + h, 0:N], ogb[:, h, 0:N], gn_t[:, 0:1], rstd[:, 0:N], ALU.mult, ALU.mult)
            p.tt('dve', bufA[:, 8 + h, 0:N], bufA[:, 8 + h, 0:N], zb8[:, h, 0:N], ALU.mult)
        for m in range(16):
            wt_ = wds.get()
            wo = wt_.re("p (c n) -> p c n", n=128)
            p.dma(p.dq(), wo[:, 0:16, :], w_out[:, :, m * 128:(m + 1) * 128])
            ps = pp.get()
            for kc in range(16):
                p.mm(ps[:, 0:N], wo[:, kc, :], bufA[:, kc, 0:N], kc == 0, kc == 15)
            p.copy('act' if m % 2 == 0 else 'dve', yb[:, m, 0:N], ps[:, 0:N])
        rms_rstd(p, pp, yb, 0, 16, N, ones, bufA, rstd, D)
        for c in range(16):
            p.stt('dve', yb[:, c, 0:N], yb[:, c, 0:N], mod_t[:, 0, bi, c:c + 1], rstd[:, 0:N], ALU.mult, ALU.mult)
            p.tt('pool', xb[:, c, 0:N], xb[:, c, 0:N], yb[:, c, 0:N], ALU.add)
        rms_rstd(p, pp, xb, 0, 16, N, ones, bufA, rstd, D)
        for c in range(16):
            p.stt('dve', bufA[:, c, 0:N], xb[:, c, 0:N], mod_t[:, 1, bi, c:c + 1], rstd[:, 0:N], ALU.mult, ALU.mult)
            p.act(bufA[:, c, 0:N], bufA[:, c, 0:N], AF.Identity, bias=mod_t[:, 2, bi, c:c + 1])
        for ti in range(NTL):
            router(ti)
        p.copy('act', h2r[:, :, 0:N], bufA[:, :, 0:N])
        for e in range(nexp):
            for f in range(2):
                ws, wr = wgs.get(), wgr.get()
                p.dma('sp', ws[:, :, 0:128], wg[e, :, :, f * 128:(f + 1) * 128])
                p.dma('pool', ws[:, :, 128:256], wu[e, :, :, f * 128:(f + 1) * 128])
                p.copy('act', wr[:, :, :], ws[:, :, :])
                psg, psu = pp.get(), pp.get()
                for kc in range(16):
                    p.mm(psg[:, 0:N], wr[:, kc, 0:128], h2r[:, kc, 0:N], kc == 0, kc == 15)
                for kc in range(16):
                    p.mm(psu[:, 0:N], wr[:, kc, 128:256], h2r[:, kc, 0:N], kc == 0, kc == 15)
                tm = tmp.get()
                p.act(tm[:, 0:N], psg[:, 0:N], AF.Silu)
                p.tt('dve', hid[:, f, 0:N], tm[:, 0:N], psu[:, 0:N], ALU.mult)
            wdf = []
            for f in range(2):
                ds, dr = wds.get(), wdr.get()
                p.dma(p.dq(), ds[:, :], wd[e, :, f, :])
                p.copy('pool', dr[:, :], ds[:, :])
                wdf.append(dr)
            for ti in range(NTL):
                for n4 in range(4):
                    ps = pp.get()
                    for f in range(2):
                        p.mm(ps[:, :], hid[:, f, ti * 128:(ti + 1) * 128], wdf[f][:, n4 * 512:(n4 + 1) * 512],
                             f == 0, f == 1)
                    a = acc[:, ti, n4 * 512:(n4 + 1) * 512]
                    if e == 0:
                        p.ts('dve', a, ps[:, :], gates[:, ti, e:e + 1], None, ALU.mult)
                    else:
                        p.stt('dve', a, ps[:, :], gates[:, ti, e:e + 1], a, ALU.mult, ALU.add)
        for ti in range(NTL):
            for c in range(16):
                ps = pp.get()
                p.transpose(ps[:, 0:128], acc[:, ti, c * 128:(c + 1) * 128], ident[:, :])
                p.copy('act' if c % 2 == 0 else 'dve', bufA[:, c, ti * 128:(ti + 1) * 128], ps[:, 0:128])
        rms_rstd(p, pp, bufA, 0, 16, N, ones, yb, rstd, D)
        for c in range(16):
            p.stt('dve', bufA[:, c, 0:N], bufA[:, c, 0:N], mod_t[:, 3, bi, c:c + 1], rstd[:, 0:N], ALU.mult, ALU.mult)
            p.tt('pool', xb[:, c, 0:N], xb[:, c, 0:N], bufA[:, c, 0:N], ALU.add)
        p.dma('sp', o_x[:, :, t0:t0 + N], xb[:, :, 0:N])

    for bi, (t0, N) in enumerate(BLOCKS4):
        do_block(bi, t0, N)


def build_fused(depth=DEPTH, upto=9):
    nc = new_nc()
    p = Prog(nc)
    _CCBUF.b.w = None
    _CCBUF.b.r = {}
    io = declare_io(p, depth)
    modp = p.sbp("modp", [128, depth, 96, 2])
    stage_a(p, io, depth, modp)
    p.flush(final=(upto == 1))
    for l in range(depth):
        if upto < 2:
            break
        stage_s1(p, io, l, modp)
        p.flush(final=(upto == 2))
        if upto < 3:
            break
        gather_chunks(p, io.g1_s, io.g1_g, 128, NG1)
        gather_chunks(p, io.g2_s, io.g2_g, 128, NT)
        p.flush(final=(upto == 3))
        if upto < 4:
            break
        stage_s2(p, io)
        p.flush(final=(upto == 4))
        if upto < 5:
            break
        stage_s3(p, io, l)
        p.flush()
        gather_chunks(p, io.og_s, io.og_g, 64, NPAIR * 2)
        p.flush(final=(upto == 5))
        if upto < 6:
            break
        stage_s4(p, io, l, modp, l == depth - 1)
        p.flush(final=(l == depth - 1))
    p.close()
    return nc


def _fm(v, n):
    return np.ascontiguousarray(np.asarray(v, np.float32).reshape(n, 128).T)


def _gdn_consts():
    i = np.arange(128)
    same = (i[:, None] // 64) == (i[None, :] // 64)
    PE_f = ((i[:, None] <= i[None, :]) & same).astype(np.float32)
    PS_f = ((i[:, None] > i[None, :]) & same).astype(np.float32)
    BD = same.astype(np.float32)
    CH0 = np.broadcast_to((i < 64)[:, None], (128, 128)).astype(np.float32)
    CH1 = np.broadcast_to((i >= 64)[:, None], (128, 128)).astype(np.float32)
    return np.ascontiguousarray(np.stack([PE_f, PS_f, PE_f.T, PS_f.T, BD, CH0, CH1, np.eye(128, dtype=np.float32)], 0))


def _rope_tables():
    s = np.arange(SEQ)
    r = (s // GRID_W).astype(np.float32)
    col = (s % GRID_W).astype(np.float32)
    half = ROPE // 2
    inv = np.power(np.float32(10000.0), -np.arange(0, half, 2, dtype=np.float32) / np.float32(half)).astype(np.float32)
    ar = r[:, None] * inv
    ac = col[:, None] * inv
    ang = np.concatenate([ar, ar, ac, ac], -1).astype(np.float32)
    cos = np.concatenate([np.ones((CTX, ROPE), np.float32), np.cos(ang).astype(np.float32)], 0)
    sin = np.concatenate([np.zeros((CTX, ROPE), np.float32), np.sin(ang).astype(np.float32)], 0)
    return cos, sin


def _rot_mat():
    R = np.zeros((64, 64), np.float32)
    for i in range(16):
        R[16 + i, i] = -1
        R[i, 16 + i] = 1
        R[48 + i, 32 + i] = -1
        R[32 + i, 48 + i] = 1
    return R


_NC = {}


def kernel(x, c, ctx, c_ctx, ada_w, ada_b, norm_mix_pre, norm_mix_post, norm_ffn_pre, norm_ffn_post,
           w_in, mla_q_norm, mla_w_q_up, mla_kv_norm, mla_w_kv_up, gdn_conv, gdn_a_log, gdn_dt_bias,
           gdn_norm, w_out, router_w, router_bias, exp_w_gate, exp_w_up, exp_w_down,
           sh_w_gate, sh_w_up, sh_w_down, depth=DEPTH):
    f = np.float32
    A = lambda a: np.ascontiguousarray(np.asarray(a, dtype=f)[:depth])
    x, c, ctx, c_ctx = [np.asarray(a, dtype=f) for a in (x, c, ctx, c_ctx)]
    L = depth
    cos, sin = _rope_tables()
    islat = np.ones(BTOK, f)
    islat[:CTX] = 0
    shared = dict(
        dr_rmat=_rot_mat(), dr_kmask=np.ascontiguousarray((-MASK_BIG * islat)[None]), dr_cst=_gdn_consts(),
        dr_ada_w=A(ada_w), dr_ada_b=np.ascontiguousarray(A(ada_b).reshape(L, 96, 128).transpose(0, 2, 1)),
        dr_gvec=np.ascontiguousarray(np.stack([A(v).reshape(L, 16, 128).transpose(0, 2, 1) for v in
                                               (norm_mix_pre, norm_mix_post, norm_ffn_pre, norm_ffn_post)], 2)),
        dr_w_in=A(w_in),
        dr_gq=np.ascontiguousarray(A(mla_q_norm).reshape(L, 4, 128).transpose(0, 2, 1)),
        dr_gkv=np.ascontiguousarray(A(mla_kv_norm).reshape(L, 2, 128).transpose(0, 2, 1)),
        dr_gn=np.ascontiguousarray(A(gdn_norm).reshape(L, 128, 1)),
        dr_w_out=A(w_out), dr_rw=A(router_w),
        dr_rb=np.ascontiguousarray(np.broadcast_to(A(router_bias)[:, None, :], (L, 128, NE))),
        dr_wg=np.ascontiguousarray(np.concatenate([A(exp_w_gate), A(sh_w_gate)[:, None]], 1)),
        dr_wu=np.ascontiguousarray(np.concatenate([A(exp_w_up), A(sh_w_up)[:, None]], 1)),
        dr_wd=np.ascontiguousarray(np.concatenate([A(exp_w_down), A(sh_w_down)[:, None]], 1)),
    )
    wq = A(mla_w_q_up).reshape(L, QR, H, QK)
    shared["dr_wq"] = np.ascontiguousarray(np.concatenate([wq[..., :NOPE].reshape(L, QR, -1), wq[..., NOPE:].reshape(L, QR, -1)], 2))
    wkv = A(mla_w_kv_up).reshape(L, KVR, H, NOPE + VD)
    shared["dr_wkv"] = np.ascontiguousarray(np.concatenate([wkv[..., :NOPE].reshape(L, KVR, -1), wkv[..., NOPE:].reshape(L, KVR, -1)], 2))
    conv, alog, dtb = A(gdn_conv), A(gdn_a_log), A(gdn_dt_bias)
    per_par = []
    for hf in range(2):
        heads = [hf * NPAIR + i for i in range(NPAIR)]
        cw = np.stack([np.stack([np.stack([conv[l][:, w * 1024 + h * 128:w * 1024 + (h + 1) * 128].T for w in range(3)], 1)
                                 for h in heads], 0) for l in range(L)], 0)
        alog_g = np.zeros((L, 128, 16), f)
        dtb_g = np.zeros((L, 128, 16), f)
        for pi, h in enumerate(heads):
            for d in range(2):
                alog_g[:, :, pi * 4 + d * 2 + 1] = alog[:, d, h][:, None]
                dtb_g[:, :, pi * 4 + d * 2 + 1] = dtb[:, d, h][:, None]
        bsel1 = np.zeros((128, NB, 2), f)
        bsel4 = np.zeros((128, NB4, 2), f)
        bsel1[:, :, 1] = 1
        bsel4[:, :, 1] = 1
        if hf == 0:
            bsel1[:, 0, :] = (1, 0)
            bsel4[:, 0, :] = (1, 0)
        par = np.zeros((128, 2), f)
        par[:, hf] = 1
        qmask = np.zeros((1, TOK), f)
        if hf == 0:
            qmask[0, :CTX] = 1
        per_par.append(dict(dr_cw=np.ascontiguousarray(cw), dr_alog=alog_g, dr_dtb=dtb_g, dr_bsel1=bsel1, dr_bsel4=bsel4,
                            dr_par=par, dr_qmask=qmask,
                            dr_cosT=np.ascontiguousarray(cos[hf * TOK:(hf + 1) * TOK].T),
                            dr_sinT=np.ascontiguousarray(sin[hf * TOK:(hf + 1) * TOK].T)))
    in_maps = []
    for k in range(8):
        b, hf = k // 2, k % 2
        tok = np.concatenate([ctx[b], x[b, :TOK - CTX]], 0) if hf == 0 else x[b, TOK - CTX:]
        c2 = np.stack([c[b], c_ctx], 1)
        m = dict(shared)
        m.update(per_par[hf])
        m["dr_x0T"] = np.ascontiguousarray(tok.T)
        m["dr_cT2"] = np.ascontiguousarray(c2.reshape(16, 128, 2).transpose(1, 0, 2))
        in_maps.append(m)
    if depth not in _NC:
        _NC[depth] = build_fused(depth)
    res = run_bass_kernel_spmd(_NC[depth], in_maps, core_ids=list(range(8))).results
    out = np.empty((BATCH, SEQ, D), f)
    for b in range(BATCH):
        out[b, :TOK - CTX] = res[2 * b]["dr_o_x"][:, CTX:].T
        out[b, TOK - CTX:] = res[2 * b + 1]["dr_o_x"].T
    return out
```

```python
import contextlib
import numpy as np
import concourse.bass as bass
import concourse.mybir as mybir
from concourse.bass_utils import run_bass_kernel_spmd

F32 = mybir.dt.float32
F32R = mybir.dt.float32r
ALU = mybir.AluOpType
AF = mybir.ActivationFunctionType
AX = mybir.AxisListType

ENGS = ['pe', 'act', 'dve', 'pool', 'sp']

D = 2048
BATCH = 4
SEQ = 4096
DEPTH = 4
GRID_W = 64
CTX = 256
EPS = 1e-6
H = 8
QR = 512
KVR = 256
NOPE = 128
ROPE = 64
VD = 128
QK = NOPE + ROPE
DK = 128
DV = 128
CONV = 5
NE = 64
EFF = 256
IN_W = 4960
O_Q, O_KV, O_KPE, O_QKV, O_Z, O_BA = 0, 512, 768, 832, 3904, 4928
TOK = 2176
NT = 17
BTOK = CTX + SEQ
BLOCKS = [(0, 256), (256, 512), (768, 512), (1280, 512), (1792, 384)]
NB = len(BLOCKS)


class Buf:
    __slots__ = ('name', 'w', 'r', 'excl')

    def __init__(self, name=''):
        self.name = name
        self.w = None
        self.r = {}
        self.excl = False


class V:
    __slots__ = ('tile', 'ap')

    def __init__(self, tile, ap):
        self.tile = tile
        self.ap = ap


class T:
    def __init__(self, t, name, buf=None, track=True):
        self.t = t
        self.name = name
        self.b = buf if buf is not None else Buf(name)
        self.track = track

    def __getitem__(self, idx):
        return V(self, self.t[idx])

    def re(self, pat, **kw):
        base = self.t if hasattr(self.t, 'rearrange') else self.t[:]
        return T(base.rearrange(pat, **kw), self.name, self.b, self.track)


def _tiles(vs):
    out = []
    for v in vs:
        if isinstance(v, V) and v.tile.track and v.tile not in out:
            out.append(v.tile)
    return out


def _ap(x):
    return x.ap if isinstance(x, V) else x


class Prog:
    def __init__(self, nc, same_engine_sync=True):
        self.nc = nc
        self.ops = {e: [] for e in ENGS}
        self.cnt = {}
        self.seen = {e: {} for e in ENGS}
        self.same = same_engine_sync
        self.st = contextlib.ExitStack()
        self.scope = contextlib.ExitStack()
        self.sems = {}
        self.base = {}
        self.stage = 0
        self.rr = 0

    def sb(self, name, shape, dtype=F32):
        return T(self.scope.enter_context(self.nc.sbuf_tensor("%s_s%d" % (name, self.stage), list(shape), dtype)), name)

    def sbp(self, name, shape, dtype=F32):
        return T(self.st.enter_context(self.nc.sbuf_tensor(name, list(shape), dtype)), name)

    def ps(self, name, shape, dtype=F32):
        t = T(self.scope.enter_context(self.nc.psum_tensor("%s_s%d" % (name, self.stage), list(shape), dtype)), name)
        t.b.excl = True
        return t

    def idram(self, name, shape, dtype=F32):
        return T(self.nc.dram_tensor(name, list(shape), dtype).ap(), 'dr_' + name, track=False)

    def dram(self, name, shape, kind, dtype=F32, track=False):
        return T(self.nc.dram_tensor(name, list(shape), dtype, kind=kind).ap(), name, track=track)

    def op(self, eng, fn, reads=(), writes=(), dma=None, inc=None):
        waits = {}
        seen = self.seen[eng]

        def need(dep):
            if dep is None:
                return
            k, v = dep
            if k == eng and (eng == 'pe' or not self.same):
                return
            if seen.get(k, 0) >= v:
                return
            if waits.get(k, 0) < v:
                waits[k] = v

        for t in reads:
            need(t.b.w)
            if t.b.excl:
                for k, v in t.b.r.items():
                    if k != eng:
                        need((k, v))
        for t in writes:
            need(t.b.w)
            for k, v in t.b.r.items():
                need((k, v))
        for k, v in waits.items():
            seen[k] = v
        key = dma if dma else eng
        inc = inc if inc is not None else (16 if dma else 1)
        c = self.cnt.get(key, 0) + inc
        self.cnt[key] = c
        for t in reads:
            t.b.r[key] = c
        for t in writes:
            t.b.w = (key, c)
            t.b.r = {}
        self.ops[eng].append((list(waits.items()), fn, key, inc))

    def dma(self, eng, out, in_):
        sbside = in_.tile if out.tile.name.startswith('dr_') else out.tile
        key = 'd_' + sbside.name + '_' + eng
        o, i = out.ap, in_.ap
        self.op(eng, lambda e: e.dma_start(out=o, in_=i), _tiles([in_]), _tiles([out]), dma=key)

    def dq(self):
        self.rr += 1
        return ('sp', 'pool')[self.rr % 2]

    def mm(self, out, lhsT, rhs, start=True, stop=True):
        o, l, r = out.ap, lhsT.ap, rhs.ap
        self.op('pe', lambda e: e.matmul(o, l, r, start=start, stop=stop), _tiles([lhsT, rhs]), _tiles([out]))

    def transpose(self, out, in_, ident):
        self.mm(out, in_, ident)

    def act(self, out, in_, func, bias=None, scale=1.0, eng='act'):
        o, i, b, s = out.ap, in_.ap, _ap(bias), _ap(scale)
        if b is None:
            self.op('act', lambda e: e.activation(out=o, in_=i, func=func, scale=s), _tiles([in_, scale]), _tiles([out]))
        else:
            self.op('act', lambda e: e.activation(out=o, in_=i, func=func, bias=b, scale=s),
                    _tiles([in_, bias, scale]), _tiles([out]))

    def copy(self, eng, out, in_):
        o, i = out.ap, in_.ap
        if eng == 'act':
            self.op('act', lambda e: e.copy(out=o, in_=i), _tiles([in_]), _tiles([out]))
        else:
            self.op(eng, lambda e: e.tensor_copy(out=o, in_=i), _tiles([in_]), _tiles([out]))

    def tt(self, eng, out, in0, in1, op):
        o, a, b = out.ap, in0.ap, in1.ap
        self.op(eng, lambda e: e.tensor_tensor(out=o, in0=a, in1=b, op=op), _tiles([in0, in1]), _tiles([out]))

    def ts(self, eng, out, in0, s1, s2, op0, op1=None):
        o, a, x1, x2 = out.ap, in0.ap, _ap(s1), _ap(s2)
        if op1 is None:
            self.op(eng, lambda e: e.tensor_scalar(out=o, in0=a, scalar1=x1, scalar2=None, op0=op0),
                    _tiles([in0, s1]), _tiles([out]))
        else:
            self.op(eng, lambda e: e.tensor_scalar(out=o, in0=a, scalar1=x1, scalar2=x2, op0=op0, op1=op1),
                    _tiles([in0, s1, s2]), _tiles([out]))

    def stt(self, eng, out, in0, scalar, in1, op0, op1):
        o, a, s, b = out.ap, in0.ap, _ap(scalar), in1.ap
        self.op(eng, lambda e: e.scalar_tensor_tensor(out=o, in0=a, scalar=s, in1=b, op0=op0, op1=op1),
                _tiles([in0, scalar, in1]), _tiles([out]))

    def memset(self, eng, out, val):
        o = out.ap
        self.op(eng, lambda e: e.memset(o, val), [], _tiles([out]))

    def recip(self, out, in_):
        o, i = out.ap, in_.ap
        self.op('dve', lambda e: e.reciprocal(out=o, in_=i), _tiles([in_]), _tiles([out]))

    def max8(self, out, in_):
        o, i = out.ap, in_.ap
        self.op('dve', lambda e: e.max(out=o, in_=i), _tiles([in_]), _tiles([out]))

    def reduce(self, eng, out, in_, op, axis=AX.X):
        o, i = out.ap, in_.ap
        self.op(eng, lambda e: e.tensor_reduce(out=o, in_=i, axis=axis, op=op), _tiles([in_]), _tiles([out]))

    def flush(self, final=False):
        nc = self.nc
        for k in self.cnt:
            if k not in self.sems:
                self.sems[k] = self.st.enter_context(nc.semaphore("s_" + k))
        sems = self.sems
        base = list(self.base.items())
        if final:
            waits = [(k, v) for k, v in self.cnt.items() if k.startswith('d_')]
            self.ops['sp'].append((waits, None, None, 0))
        ops = self.ops
        with nc.Block() as block:
            def run(e):
                def f(engobj):
                    for k, v in base:
                        engobj.wait_ge(sems[k], v)
                    for waits, fn, key, inc in ops[e]:
                        for k, v in waits:
                            engobj.wait_ge(sems[k], v)
                        if fn is not None:
                            fn(engobj).then_inc(sems[key], inc)
                return f

            block.tensor(run('pe'))
            block.scalar(run('act'))
            block.vector(run('dve'))
            block.gpsimd(run('pool'))
            block.sync(run('sp'))
        self.ops = {e: [] for e in ENGS}
        self.base = dict(self.cnt)
        self.seen = {e: dict(self.base) for e in ENGS}
        self.scope.close()
        self.scope = contextlib.ExitStack()
        self.stage += 1

    def close(self):
        self.st.close()


def new_nc():
    return bass.Bass("TRN2", target_bir_lowering=False)


class PsumPool:
    def __init__(self, p, n=8):
        self.tiles = [p.ps("psb%d" % i, [128, 512]) for i in range(n)]
        self.i = 0

    def get(self):
        t = self.tiles[self.i % len(self.tiles)]
        self.i += 1
        return t


class Ring:
    def __init__(self, tiles):
        self.tiles = tiles
        self.i = 0

    def get(self):
        t = self.tiles[self.i % len(self.tiles)]
        self.i += 1
        return t


def rstd_from_ps(p, ps, N, rstd, dim):
    p.ts('dve', rstd[:, 0:N], ps[:, 0:N], 1.0 / dim, EPS, ALU.mult, ALU.add)
    p.act(rstd[:, 0:N], rstd[:, 0:N], AF.Sqrt)
    p.recip(rstd[:, 0:N], rstd[:, 0:N])


def rms_rstd(p, pp, src, c0, nch, N, ones, sq, rstd, dim):
    p.act(sq[:, 0:nch, 0:N], src[:, c0:c0 + nch, 0:N], AF.Square)
    ps = pp.get()
    for c in range(nch):
        p.mm(ps[:, 0:N], ones[:, :], sq[:, c, 0:N], c == 0, c == nch - 1)
    rstd_from_ps(p, ps, N, rstd, dim)


IN_, OUT_ = "ExternalInput", "ExternalOutput"


IN_, OUT_ = "ExternalInput", "ExternalOutput"
G1R = H * 128 + 128 + 3072
G2C = H * 128 + 32
R_KP = H * 128
R_QKV = H * 128 + 128
NG1 = G1R // 128
BLOCKS4 = [(0, 256)] + [(256 + i * 384, 384) for i in range(5)]
NB4 = len(BLOCKS4)
NEX = NE + 1
NEXD = NEX
ROUTED_SCALE = 2.5
MASK_BIG = 1.0e5
NPAIR = 4
NTB = BTOK // 128
SEGS = [(0, CTX), (CTX, BTOK)]
FWD_ORDER = list(range(NTB))
BWD_ORDER = [1, 0] + list(range(NTB - 1, 1, -1))
PAIR_GROUPS = [[0, 1], [2, 3], [4, 5], [6, 7]]


def sub(t, idx):
    return T(t.t[idx], t.name, t.b, t.track)


class IO:
    pass


def declare_io(p, depth):
    io = IO()

    def d(name, shape):
        return p.dram("dr_" + name, shape, IN_)

    io.x0T = d("x0T", [D, TOK]).re("(c p) n -> p c n", p=128)
    io.cT2 = d("cT2", [128, 16, 2])
    io.bsel1 = d("bsel1", [128, NB, 2])
    io.bsel4 = d("bsel4", [128, NB4, 2])
    io.par = d("par", [128, 2])
    io.cosT, io.sinT = d("cosT", [64, TOK]), d("sinT", [64, TOK])
    io.rmat = d("rmat", [64, 64])
    io.qmask, io.kmask = d("qmask", [1, TOK]), d("kmask", [1, BTOK])
    io.cst = d("cst", [8, 128, 128])
    io.ada_w = d("ada_w", [depth, D, 6 * D])
    io.ada_b = d("ada_b", [depth, 128, 96])
    io.gvec = d("gvec", [depth, 128, 4, 16])
    io.w_in = d("w_in", [depth, D, IN_W])
    io.gq = d("gq", [depth, 128, 4])
    io.wq = d("wq", [depth, QR, H * QK])
    io.gkv = d("gkv", [depth, 128, 2])
    io.wkv = d("wkv", [depth, KVR, 2 * H * 128])
    io.cw = d("cw", [depth, NPAIR, 128, 3, 5])
    io.alog = d("alog", [depth, 128, 16])
    io.dtb = d("dtb", [depth, 128, 16])
    io.gn = d("gn", [depth, 128, 1])
    io.w_out = d("w_out", [depth, D, D])
    io.rw = d("rw", [depth, D, NE])
    io.rb = d("rb", [depth, 128, NE])
    io.wg = d("wg", [depth, NEXD, D, EFF])
    io.wu = d("wu", [depth, NEXD, D, EFF])
    io.wd = d("wd", [depth, NEXD, EFF, D])
    io.o_x = p.dram("dr_o_x", [D, TOK], OUT_).re("(c p) n -> p c n", p=128)
    io.xs = p.idram("xs", [D, TOK]).re("(c p) n -> p c n", p=128)
    io.qn_s = p.idram("qn_s", [H, 128, TOK])
    io.qp_s = p.idram("qp_s", [H, 64, TOK])
    io.z_s = p.idram("z_s", [H, 128, TOK])
    io.attn_s = p.idram("attn_s", [H, 128, TOK])
    io.g1_s = p.idram("g1_s", [G1R, TOK])
    io.g1_g = p.idram("g1_g", [2 * G1R, TOK])
    io.g2_s = p.idram("g2_s", [TOK, G2C])
    io.g2_g = p.idram("g2_g", [BTOK, G2C])
    io.og_s = p.idram("og_s", [NPAIR * 128, BTOK])
    io.og_g = p.idram("og_g", [2 * NPAIR * 128, BTOK])
    return io


_CCBUF = T(None, 'ccbuf')


def all_gather(p, snd, rcv):
    s_ap, r_ap = snd.t.opt(), rcv.t.opt()
    p.op('pool', lambda e: e.collective_compute("AllGather", ALU.bypass, replica_groups=PAIR_GROUPS,
                                                ins=[s_ap], outs=[r_ap]), writes=[_CCBUF], dma='cc', inc=1)


def gather_chunks(p, snd, rcv, rows, nch):
    for c in range(nch):
        all_gather(p, sub(snd, slice(c * rows, (c + 1) * rows)), sub(rcv, slice(c * 2 * rows, (c + 1) * 2 * rows)))


def blend(p, out, a, b, par):
    p.ts('dve', out, a, par[:, 0:1], None, ALU.mult)
    p.stt('dve', out, b, par[:, 1:2], out, ALU.mult, ALU.add)


def blockvecs(p, dst, modp, l, i, bsel, nblk, tmp):
    own, cx = modp[:, l, i * 16:(i + 1) * 16, 0], modp[:, l, i * 16:(i + 1) * 16, 1]
    for bi in range(nblk):
        p.ts('dve', tmp[:, 0:16], own, bsel[:, bi, 1:2], None, ALU.mult)
        p.stt('dve', dst[:, bi, :], cx, bsel[:, bi, 0:1], tmp[:, 0:16], ALU.mult, ALU.add)


def stage_a(p, io, depth, modp):
    pp = PsumPool(p, 4)
    c_t = p.sb("c_t", [128, 16, 2])
    ab_t = p.sb("ab_t", [128, depth, 96])
    wt = Ring([p.sb("awt%d" % i, [128, 16, 512]) for i in range(3)])
    p.dma('sp', c_t[:, :, :], io.cT2[:, :, :])
    p.dma('sp', ab_t[:, :, :], io.ada_b.re("l p j -> p l j")[:, :, :])
    p.act(c_t[:, :, :], c_t[:, :, :], AF.Silu)
    for l in range(depth):
        aw = sub(io.ada_w, l).re("(c p) n -> p c n", p=128)
        for g in range(24):
            w = wt.get()
            p.dma(p.dq(), w[:, :, :], aw[:, :, g * 512:(g + 1) * 512])
            for jj in range(4):
                j = g * 4 + jj
                ps = pp.get()
                for kc in range(16):
                    p.mm(ps[:, 0:2], w[:, kc, jj * 128:(jj + 1) * 128], c_t[:, kc, :], kc == 0, kc == 15)
                p.act(modp[:, l, j, :], ps[:, 0:2], AF.Identity, bias=ab_t[:, l, j:j + 1])


def stage_s1(p, io, l, modp):
    xT = io.x0T if l == 0 else io.xs
    w_in = sub(io.w_in, l).re("(c p) n -> p c n", p=128)
    wq = sub(io.wq, l).re("(c p) n -> p c n", p=128)
    wkv = sub(io.wkv, l).re("(c p) n -> p c n", p=128)
    pp = PsumPool(p)
    ones = p.sb("ones", [128, 128])
    xb = p.sb("xb", [128, 16, 512])
    hb = p.sb("hb", [128, 16, 512])
    wt = Ring([p.sb("wt%d" % i, [128, 16, 320]) for i in range(2)])
    rstd = p.sb("rstd", [128, 512])
    A_t = p.sb("A_t", [128, NB, 16])
    S_t = p.sb("S_t", [128, NB, 16])
    bs_t = p.sb("bs_t", [128, NB, 2])
    tmpv = p.sb("tmpv", [128, 16])
    gv_t = p.sb("gv_t", [128, 4, 16])
    gq_t = p.sb("gq_t", [128, 4])
    gkv_t = p.sb("gkv_t", [128, 2])
    wq_t = p.sb("wq_t", [128, 4, H * QK])
    wkv_t = p.sb("wkv_t", [128, 2, 2 * H * 128])
    cos_t = p.sb("cos_t", [64, 512])
    sin_t = p.sb("sin_t", [64, 512])
    rm_t = p.sb("rm_t", [64, 64])
    lat = p.sb("lat", [128, 7, 512])
    latn = p.sb("latn", [128, 6, 512])
    lsq = xb
    rq = p.sb("rq", [128, 512])
    stg = Ring([p.sb("stg%d" % i, [128, 512]) for i in range(4)])
    pe_a = p.sb("pe_a", [64, 512])
    pe_b = p.sb("pe_b", [64, 512])
    pe_c = Ring([p.sb("pe_c%d" % i, [64, 512]) for i in range(2)])
    vst = Ring([p.sb("vst%d" % i, [128, 512]) for i in range(2)])
    bast = Ring([p.sb("bast%d" % i, [128, 32]) for i in range(2)])

    p.memset('pool', ones[:, :], 1.0)
    p.dma('sp', gv_t[:, :, :], sub(io.gvec, l)[:, :, :])
    p.dma('sp', bs_t[:, :, :], io.bsel1[:, :, :])
    p.dma('sp', gq_t[:, :], sub(io.gq, l)[:, :])
    p.dma('sp', gkv_t[:, :], sub(io.gkv, l)[:, :])
    p.dma('sp', rm_t[:, :], io.rmat[:, :])
    p.dma('pool', wq_t[:, :, :], wq[:, :, :])
    p.dma('pool', wkv_t[:, :, :], wkv[:, :, :])
    blockvecs(p, A_t, modp, l, 1, bs_t, NB, tmpv)
    blockvecs(p, S_t, modp, l, 0, bs_t, NB, tmpv)
    for b in range(NB):
        p.stt('dve', A_t[:, b, :], A_t[:, b, :], 1.0, gv_t[:, 0, :], ALU.add, ALU.mult)

    groups = [(0, 256), (256, 256), (512, 320)] + [(O_QKV + i * 256, 256) for i in range(12)] + \
             [(O_Z + i * 256, 256) for i in range(4)] + [(O_BA, 32)]

    def rope(src, N, dst):
        p.copy('act', pe_a[:, 0:N], src)
        ps2 = pp.get()
        p.mm(ps2[0:64, 0:N], rm_t[:, :], pe_a[:, 0:N])
        p.tt('dve', pe_b[:, 0:N], ps2[0:64, 0:N], sin_t[:, 0:N], ALU.mult)
        pc = pe_c.get()
        p.tt('pool', pc[:, 0:N], pe_a[:, 0:N], cos_t[:, 0:N], ALU.mult)
        p.tt('dve', pc[:, 0:N], pc[:, 0:N], pe_b[:, 0:N], ALU.add)
        p.dma(p.dq(), dst, pc[:, 0:N])

    def mla_up(t0, N):
        rms_rstd(p, pp, lat, 0, 4, N, ones, lsq, rq, QR)
        for c in range(4):
            p.stt('dve', latn[:, c, 0:N], lat[:, c, 0:N], gq_t[:, c:c + 1], rq[:, 0:N], ALU.mult, ALU.mult)
        rms_rstd(p, pp, lat, 4, 2, N, ones, lsq, rq, KVR)
        for c in range(2):
            p.stt('dve', latn[:, 4 + c, 0:N], lat[:, 4 + c, 0:N], gkv_t[:, c:c + 1], rq[:, 0:N], ALU.mult, ALU.mult)
        rope(lat[0:64, 6, 0:N], N, io.g1_s[R_KP:R_KP + 64, t0:t0 + N])
        for h in range(H):
            ps = pp.get()
            for kc in range(4):
                p.mm(ps[:, 0:N], wq_t[:, kc, h * 128:(h + 1) * 128], latn[:, kc, 0:N], kc == 0, kc == 3)
            s = stg.get()
            p.copy('act', s[:, 0:N], ps[:, 0:N])
            p.dma(p.dq(), io.qn_s[h, :, t0:t0 + N], s[:, 0:N])
            ps = pp.get()
            for kc in range(4):
                p.mm(ps[0:64, 0:N], wq_t[:, kc, H * 128 + h * 64:H * 128 + (h + 1) * 64], latn[:, kc, 0:N],
                     kc == 0, kc == 3)
            rope(ps[0:64, 0:N], N, io.qp_s[h, :, t0:t0 + N])
        for h in range(H):
            ps = pp.get()
            for kc in range(2):
                p.mm(ps[:, 0:N], wkv_t[:, kc, h * 128:(h + 1) * 128], latn[:, 4 + kc, 0:N], kc == 0, kc == 1)
            s = stg.get()
            p.copy('dve', s[:, 0:N], ps[:, 0:N])
            p.dma(p.dq(), io.g1_s[h * 128:(h + 1) * 128, t0:t0 + N], s[:, 0:N])
        for ti in range(N // 128):
            for half in range(2):
                vs = vst.get()
                ps = pp.get()
                for kc in range(2):
                    p.mm(ps[:, :], latn[:, 4 + kc, ti * 128:(ti + 1) * 128],
                         wkv_t[:, kc, H * 128 + half * 512:H * 128 + (half + 1) * 512], kc == 0, kc == 1)
                p.copy('act', vs[:, :], ps[:, :])
                p.dma(p.dq(), io.g2_s[t0 + ti * 128:t0 + (ti + 1) * 128, half * 512:(half + 1) * 512], vs[:, :])

    def do_block(bi, t0, N):
        p.dma('sp', xb[:, :, 0:N], xT[:, :, t0:t0 + N])
        p.dma('pool', cos_t[:, 0:N], io.cosT[:, t0:t0 + N])
        p.dma('pool', sin_t[:, 0:N], io.sinT[:, t0:t0 + N])
        rms_rstd(p, pp, xb, 0, 16, N, ones, hb, rstd, D)
        for c in range(16):
            p.stt('dve', hb[:, c, 0:N], xb[:, c, 0:N], A_t[:, bi, c:c + 1], rstd[:, 0:N], ALU.mult, ALU.mult)
            p.act(hb[:, c, 0:N], hb[:, c, 0:N], AF.Identity, bias=S_t[:, bi, c:c + 1])
        for (c0, cw) in groups:
            w = wt.get()
            p.dma(p.dq(), w[:, :, 0:cw], w_in[:, :, c0:c0 + cw])
            if c0 == O_BA:
                for ti in range(N // 128):
                    ps = pp.get()
                    for kc in range(16):
                        p.mm(ps[:, 0:32], hb[:, kc, ti * 128:(ti + 1) * 128], w[:, kc, 0:32], kc == 0, kc == 15)
                    bs = bast.get()
                    p.copy('act', bs[:, :], ps[:, 0:32])
                    p.dma('sp', io.g2_s[t0 + ti * 128:t0 + (ti + 1) * 128, H * 128:H * 128 + 32], bs[:, :])
                continue
            for m in range((cw + 127) // 128):
                mw = min(128, cw - m * 128)
                ps = pp.get()
                for kc in range(16):
                    p.mm(ps[0:mw, 0:N], w[:, kc, m * 128:m * 128 + mw], hb[:, kc, 0:N], kc == 0, kc == 15)
                col = c0 + m * 128
                if col < O_QKV:
                    p.copy('act', lat[0:mw, col // 128, 0:N], ps[0:mw, 0:N])
                else:
                    s = stg.get()
                    p.copy('act' if m % 2 == 0 else 'dve', s[:, 0:N], ps[:, 0:N])
                    if col < O_Z:
                        r0 = R_QKV + ((col - O_QKV) // 128) * 128
                        p.dma(p.dq(), io.g1_s[r0:r0 + 128, t0:t0 + N], s[:, 0:N])
                    else:
                        p.dma(p.dq(), io.z_s[(col - O_Z) // 128, :, t0:t0 + N], s[:, 0:N])
            if c0 == 512:
                mla_up(t0, N)

    for bi, (t0, N) in enumerate(BLOCKS):
        do_block(bi, t0, N)


def stage_s2(p, io):
    v = io.g2_g.re("(c r p) n -> p r c n", r=2, p=128)
    NKT = BTOK // 128
    st_ps = Ring([p.ps("st_ps%d" % i, [128, 512]) for i in range(4)])
    ot_ps = Ring([p.ps("ot_ps%d" % i, [128, 512]) for i in range(2)])
    su_ps = Ring([p.ps("su_ps%d" % i, [128, 512]) for i in range(2)])
    ones_f = p.sb("ones_f", [128, 128])
    ones = p.sb("ones", [128, 128], F32R)
    knr = p.sb("knr", [128, BTOK], F32R)
    kpr = p.sb("kpr", [65, BTOK], F32R)
    vr = p.sb("vr", [128, NKT, 128], F32R)
    qnr = p.sb("qnr", [128, TOK], F32R)
    qpr = p.sb("qpr", [65, TOK], F32R)
    kn_t = Ring([p.sb("kn_t%d" % i, [128, BTOK]) for i in range(2)])
    kp_t = p.sb("kp_t", [65, BTOK])
    v_t = Ring([p.sb("v_t%d" % i, [128, NKT, 128]) for i in range(2)])
    qn_t = Ring([p.sb("qn_t%d" % i, [128, TOK]) for i in range(2)])
    qp_t = Ring([p.sb("qp_t%d" % i, [65, TOK]) for i in range(2)])
    pt = Ring([p.sb("pt%d" % i, [128, 512], F32R) for i in range(3)])
    rs = p.sb("rs", [128, 512])
    og = Ring([p.sb("og%d" % i, [128, 512]) for i in range(2)])
    p.memset('pool', ones_f[:, :], 1.0)
    p.copy('act', ones[:, :], ones_f[:, :])
    for r in range(2):
        p.dma('sp', kp_t[0:64, r * TOK:(r + 1) * TOK], io.g1_g[H * 256 + r * 128:H * 256 + r * 128 + 64, :])
    p.dma('sp', kp_t[64:65, :], io.kmask[:, :])
    p.copy('pool', kpr[:, :], kp_t[:, :])
    scale = float(QK) ** -0.5
    for h in range(H):
        knh, vh, qnh, qph = kn_t.get(), v_t.get(), qn_t.get(), qp_t.get()
        for r in range(2):
            p.dma('sp', knh[:, r * TOK:(r + 1) * TOK], io.g1_g[h * 256 + r * 128:h * 256 + (r + 1) * 128, :])
        vh4 = vh.re("p (r c) n -> p r c n", r=2)
        for r in range(2):
            p.dma('pool', vh4[:, r, :, :], v[:, r, :, h * 128:(h + 1) * 128])
        p.dma('sp', qnh[:, :], io.qn_s[h, :, :])
        p.dma('pool', qph[0:64, :], io.qp_s[h, :, :])
        p.dma('pool', qph[64:65, :], io.qmask[:, :])
        p.copy('pool', knr[:, :], knh[:, :])
        p.copy('pool', vr[:, :, :], vh[:, :, :])
        p.copy('act', qnr[:, :], qnh[:, :])
        p.copy('act', qpr[:, :], qph[:, :])
        for (t0, N) in BLOCKS:
            ot, su = ot_ps.get(), su_ps.get()
            for kt in range(NKT):
                st = st_ps.get()
                p.mm(st[:, 0:N], knr[:, kt * 128:(kt + 1) * 128], qnr[:, t0:t0 + N], True, False)
                p.mm(st[:, 0:N], kpr[:, kt * 128:(kt + 1) * 128], qpr[:, t0:t0 + N], False, True)
                pk = pt.get()
                p.act(pk[:, 0:N], st[:, 0:N], AF.Exp, scale=scale)
                p.mm(ot[:, 0:N], vr[:, kt, :], pk[:, 0:N], kt == 0, kt == NKT - 1)
                p.mm(su[:, 0:N], ones[:, :], pk[:, 0:N], kt == 0, kt == NKT - 1)
            p.recip(rs[:, 0:N], su[:, 0:N])
            o = og.get()
            p.tt('dve', o[:, 0:N], ot[:, 0:N], rs[:, 0:N], ALU.mult)
            p.dma(p.dq(), io.attn_s[h, :, t0:t0 + N], o[:, 0:N])


class QPsum:
    def __init__(self, p, nbanks=8):
        self.q = []
        for i in range(nbanks):
            bank = p.ps("qb%d" % i, [128, 512])
            for j in range(4):
                self.q.append(T(bank.t[:, j * 128:(j + 1) * 128], "qb%d_%d" % (i, j), buf=bank.b))
        self.i = 0

    def get(self):
        t = self.q[self.i % len(self.q)]
        self.i += 1
        return t


def stage_s3(p, io, l):
    dbg = 0
    cw = sub(io.cw, l)
    qp_ = QPsum(p, 7)
    big = PsumPool(p, 1)
    C = p.sb("C", [128, 8, 128])
    ones = p.sb("ones", [128, 128])
    par = p.sb("par", [128, 2])
    p.dma('sp', C[:, :, :], io.cst.re("c p n -> p c n")[:, :, :])
    p.dma('sp', par[:, :], io.par[:, :])
    p.memset('pool', ones[:, :], 1.0)
    PEm = [C[:, 0, :], C[:, 2, :]]
    PSm = [C[:, 1, :], C[:, 3, :]]
    BD, CH, ident = C[:, 4, :], [C[:, 5, :], C[:, 6, :]], C[:, 7, :]
    bat = p.sb("bat", [128, NTB, NPAIR * 4])
    baf = p.sb("baf", [128, NTB, 32])
    baf4 = baf.re("p (r c) n -> p r c n", r=2)
    for rk in range(2):
        p.dma('sp', baf4[:, rk, :, :], io.g2_g.re("(c r p) n -> p r c n", r=2, p=128)[:, rk, :, H * 128:H * 128 + 32])
    for pi in range(NPAIR):
        for d in range(2):
            for k2 in range(2):
                ca = k2 * 16 + d * 8 + pi
                blend(p, bat[:, :, pi * 4 + d * 2 + k2], baf[:, :, ca], baf[:, :, ca + 4], par)
    alog_t = p.sb("alog_t", [128, 16])
    dtb_t = p.sb("dtb_t", [128, 16])
    p.dma('sp', alog_t[:, :], sub(io.alog, l)[:, :])
    p.dma('sp', dtb_t[:, :], sub(io.dtb, l)[:, :])
    nea = p.sb("nea", [128, 16])
    p.act(nea[:, :], alog_t[:, :], AF.Exp)
    p.ts('dve', nea[:, :], nea[:, :], -1.0, None, ALU.mult)
    gb = p.sb("gb", [128, NTB, NPAIR * 4])
    for t in range(NTB):
        p.tt('pool', gb[:, t, :], bat[:, t, :], dtb_t[:, :], ALU.add)
    p.act(gb[:, :, :], gb[:, :, :], AF.Exp)
    p.act(gb[:, :, :], gb[:, :, :], AF.Ln, bias=1.0)
    for t in range(NTB):
        p.tt('pool', gb[:, t, :], gb[:, t, :], nea[:, :], ALU.mult)
    sig = p.sb("sig", [128, NTB, NPAIR * 4])
    p.act(sig[:, :, :], bat[:, :, :], AF.Sigmoid)

    rawt = Ring([p.sb("rawt%d" % i, [128, BTOK]) for i in range(2)])
    cv = p.sb("cv", [128, 3, BTOK])
    cwt = p.sb("cwt", [128, 3, 5])
    sq = p.sb("sq", [128, 512])
    rn = p.sb("rn", [128, 512])
    oT = p.sb("oT", [128, BTOK])
    vec = {}
    for d in range(2):
        for nm in ("gc", "bg", "kd", "nb", "be", "g"):
            vec[(nm, d)] = p.sb("v_%s%d" % (nm, d), [128, NTB])
        vec[("egl", d)] = p.sb("v_egl%d" % d, [128, NTB, 2])
    S = [p.sb("S%d" % d, [128, 128]) for d in range(2)]

    def ring(nm, n=2):
        return [Ring([p.sb("%s%d_%d" % (nm, d, i), [128, 128]) for i in range(n)]) for d in range(2)]

    r_gpe, r_dm, r_dmt, r_egcb, r_t1, r_t2 = ring("gpe"), ring("dm"), ring("dmt"), ring("egcb"), ring("t1"), ring("t2")
    r_M, r_Mt, r_Pt = ring("M", 3), ring("Mt", 3), ring("Pt", 3)
    r_qkdt, r_kbg, r_kdec, r_vb, r_wT, r_u, r_qgT, r_vnew = (ring("qkdt"), ring("kbg"), ring("kdec"), ring("vb"),
                                                            ring("wT"), ring("u"), ring("qgT"), ring("vnew"))

    def prep_pair(pi):
        p.dma('sp', cwt[:, :, :], cw[pi, :, :, :])
        for w3 in range(3):
            r, r2 = rawt.get(), rawt.get()
            for rk in range(2):
                ra = (R_QKV // 128 + w3 * 8 + pi) * 256 + rk * 128
                p.dma('sp', r[:, rk * TOK:(rk + 1) * TOK], io.g1_g[ra:ra + 128, :])
                p.dma('pool', r2[:, rk * TOK:(rk + 1) * TOK], io.g1_g[ra + 1024:ra + 1152, :])
            blend(p, r[:, :], r[:, :], r2[:, :], par)
            eng = 'dve'
            p.ts(eng, cv[:, w3, :], r[:, :], cwt[:, w3, 2:3], None, ALU.mult)
            for s in (-2, -1, 1, 2):
                for (a, b) in SEGS:
                    lo, hi = max(a, a - s), min(b, b - s)
                    p.stt(eng, cv[:, w3, lo:hi], r[:, lo + s:hi + s], cwt[:, w3, s + 2:s + 3], cv[:, w3, lo:hi],
                          ALU.mult, ALU.add)
            p.act(cv[:, w3, :], cv[:, w3, :], AF.Silu)
        for w3 in range(2):
            for c0 in range(0, BTOK, 512):
                n = min(512, BTOK - c0)
                p.act(sq[:, 0:n], cv[:, w3, c0:c0 + n], AF.Square)
                ps = big.get()
                p.mm(ps[:, 0:n], ones[:, :], sq[:, 0:n])
                if w3 == 0:
                    p.ts('dve', rn[:, 0:n], ps[:, 0:n], EPS, float(DK), ALU.add, ALU.mult)
                else:
                    p.ts('dve', rn[:, 0:n], ps[:, 0:n], EPS, None, ALU.add)
                p.act(rn[:, 0:n], rn[:, 0:n], AF.Sqrt)
                p.recip(rn[:, 0:n], rn[:, 0:n])
                p.tt('dve', cv[:, w3, c0:c0 + n], cv[:, w3, c0:c0 + n], rn[:, 0:n], ALU.mult)
        p.memset('pool', oT[:, :], 0.0)
        for d in range(2):
            cb = pi * 4 + d * 2
            g, be = vec[("g", d)], vec[("be", d)]
            p.copy('pool', g[:, :], gb[:, :, cb + 1])
            p.copy('pool', be[:, :], sig[:, :, cb])
            ps = qp_.get()
            p.mm(ps[:, 0:NTB], PEm[d], g[:, :])
            gc = vec[("gc", d)]
            p.copy('act', gc[:, :], ps[:, 0:NTB])
            ps2 = qp_.get()
            p.mm(ps2[:, 0:NTB], BD, g[:, :])
            kd = vec[("kd", d)]
            p.tt('dve', kd[:, :], ps2[:, 0:NTB], gc[:, :], ALU.subtract)
            p.act(kd[:, :], kd[:, :], AF.Exp)
            bg = vec[("bg", d)]
            p.act(bg[:, :], gc[:, :], AF.Exp)
            p.tt('dve', bg[:, :], bg[:, :], be[:, :], ALU.mult)
            p.ts('dve', vec[("nb", d)][:, :], be[:, :], -1.0, None, ALU.mult)
            egl = vec[("egl", d)]
            for c in range(2):
                ps3 = qp_.get()
                p.mm(ps3[:, 0:NTB], CH[c], g[:, :])
                p.act(egl[:, :, c], ps3[:, 0:NTB], AF.Exp)
            p.memset('pool', S[d][:, :], 0.0)

    def tile_dir(d, t):
        c0 = t * 128
        kT, qT, vT = cv[:, 1, c0:c0 + 128], cv[:, 0, c0:c0 + 128], cv[:, 2, c0:c0 + 128]
        g, be, nb = vec[("g", d)], vec[("be", d)], vec[("nb", d)]
        bg, kd, egl = vec[("bg", d)], vec[("kd", d)], vec[("egl", d)]
        gpe = r_gpe[d].get()
        p.ts('pool', gpe[:, :], PEm[d], g[:, t:t + 1], None, ALU.mult)
        ps_d, ps_dt, ps_gb = qp_.get(), qp_.get(), qp_.get()
        p.mm(ps_d[:, :], gpe[:, :], PSm[d])
        p.mm(ps_dt[:, :], PSm[d], gpe[:, :])
        p.mm(ps_gb[:, :], ones[:, :], gpe[:, :])
        dm, dmt, egcb = r_dm[d].get(), r_dmt[d].get(), r_egcb[d].get()
        p.act(dm[:, :], ps_d[:, :], AF.Exp)
        p.act(dmt[:, :], ps_dt[:, :], AF.Exp)
        p.act(egcb[:, :], ps_gb[:, :], AF.Exp)
        if dbg == 1:
            return
        ps_G, ps_KQ = qp_.get(), qp_.get()
        p.mm(ps_G[:, :], kT, kT)
        p.mm(ps_KQ[:, :], kT, qT)
        if dbg == 12:
            return
        t1, t2 = r_t1[d].get(), r_t2[d].get()
        p.tt('dve', t1[:, :], dm[:, :], PSm[d], ALU.mult)
        p.tt('dve', t2[:, :], dmt[:, :], PEm[d], ALU.mult)
        if dbg == 13:
            return
        M = r_M[d].get()
        p.stt('dve', M[:, :], ps_G[:, :], nb[:, t:t + 1], t1[:, :], ALU.mult, ALU.mult)
        if dbg == 14:
            return
        qkdt = r_qkdt[d].get()
        p.tt('dve', qkdt[:, :], ps_KQ[:, :], t2[:, :], ALU.mult)
        if dbg == 2:
            return
        ps_t = qp_.get()
        p.transpose(ps_t[:, :], M[:, :], ident)
        Mt = r_Mt[d].get()
        p.copy('act', Mt[:, :], ps_t[:, :])
        Pt = r_Pt[d].get()
        p.tt('dve', Pt[:, :], Mt[:, :], ident, ALU.add)
        if dbg == 21:
            return
        for lv in range(5 if dbg != 22 else 1):
            last = lv == 4
            ps_a = qp_.get()
            p.mm(ps_a[:, :], Mt[:, :], M[:, :])
            if dbg == 23:
                return
            if not last:
                ps_b = qp_.get()
                p.mm(ps_b[:, :], M[:, :], Mt[:, :])
            if dbg == 24:
                return
            M2 = r_M[d].get()
            p.copy('act', M2[:, :], ps_a[:, :])
            if dbg == 25:
                return
            if not last:
                Mt2 = r_Mt[d].get()
                p.copy('act' if dbg in (26, 27) else 'dve', Mt2[:, :], ps_b[:, :])
            if dbg in (26, 28):
                return
            ps_c = qp_.get()
            p.mm(ps_c[:, :], M2[:, :], Pt[:, :])
            Pt2 = r_Pt[d].get()
            p.tt('dve', Pt2[:, :], ps_c[:, :], Pt[:, :], ALU.add)
            M, Pt = M2, Pt2
            if not last:
                Mt = Mt2
        Xt = Pt
        if dbg == 3:
            return
        ps_k, ps_v = qp_.get(), qp_.get()
        p.transpose(ps_k[:, :], kT, ident)
        p.transpose(ps_v[:, :], vT, ident)
        kbg, kdec, vb = r_kbg[d].get(), r_kdec[d].get(), r_vb[d].get()
        p.ts('dve', kbg[:, :], ps_k[:, :], bg[:, t:t + 1], None, ALU.mult)
        p.act(kdec[:, :], ps_k[:, :], AF.Copy, scale=kd[:, t:t + 1])
        p.act(vb[:, :], ps_v[:, :], AF.Copy, scale=be[:, t:t + 1])
        ps_w, ps_u = qp_.get(), qp_.get()
        p.mm(ps_w[:, :], kbg[:, :], Xt[:, :])
        p.mm(ps_u[:, :], Xt[:, :], vb[:, :])
        wT, u, qgT = r_wT[d].get(), r_u[d].get(), r_qgT[d].get()
        p.copy('act', wT[:, :], ps_w[:, :])
        p.copy('dve', u[:, :], ps_u[:, :])
        p.tt('dve', qgT[:, :], cv[:, 0, c0:c0 + 128], egcb[:, :], ALU.mult)
        if dbg == 4:
            return
        vnew = r_vnew[d].get()
        for c in ((0, 1) if d == 0 else (1, 0)):
            r0, r1 = c * 64, c * 64 + 64
            ps_ws = qp_.get()
            p.mm(ps_ws[:, :], wT[:, :], S[d][:, :])
            p.tt('dve', vnew[r0:r1, :], u[r0:r1, :], ps_ws[r0:r1, :], ALU.subtract)
            ps_o = qp_.get()
            p.mm(ps_o[:, 0:64], S[d][:, :], qgT[:, r0:r1], True, False)
            p.mm(ps_o[:, 0:64], vnew[r0:r1, :], qkdt[r0:r1, r0:r1], False, True)
            p.tt('dve', oT[:, c0 + r0:c0 + r1], oT[:, c0 + r0:c0 + r1], ps_o[:, 0:64], ALU.add)
            ps_s = qp_.get()
            p.mm(ps_s[:, :], kdec[r0:r1, :], vnew[r0:r1, :])
            p.stt('dve', S[d][:, :], S[d][:, :], egl[:, t, c:c + 1], ps_s[:, :], ALU.mult, ALU.add)

    for pi in range(NPAIR):
        prep_pair(pi)
        for s in range(NTB):
            tile_dir(0, FWD_ORDER[s])
            tile_dir(1, BWD_ORDER[s])
        p.dma('sp', io.og_s[pi * 128:(pi + 1) * 128, :], oT[:, :])


def stage_s4(p, io, l, modp, last):
    nexp = NEXD
    attn = io.attn_s.re("h p n -> p h n")
    ogg = io.og_g.re("(pi q r i) n -> q i r pi n", q=2, r=2, i=64)
    zT = io.z_s.re("h p n -> p h n")
    xT = io.x0T if l == 0 else io.xs
    o_x = io.o_x if last else io.xs
    w_out = sub(io.w_out, l).re("(c p) n -> p c n", p=128)
    rw = sub(io.rw, l).re("(c p) n -> p c n", p=128)
    wg = sub(io.wg, l).re("e (c p) n -> e p c n", p=128)
    wu = sub(io.wu, l).re("e (c p) n -> e p c n", p=128)
    wd = sub(io.wd, l).re("e (c p) n -> e p c n", p=128)
    pp = PsumPool(p)
    NM = 384
    ones = p.sb("ones", [128, 128])
    ident = p.sb("ident", [128, 128])
    bufA = p.sb("bufA", [128, 16, NM])
    bufB = p.sb("bufB", [128, 16 * NM])
    xb = p.sb("xb", [128, 16, NM])
    wgs = Ring([p.sb("wgs%d" % i, [128, 16, 256]) for i in range(2)])
    wgr = Ring([p.sb("wgr%d" % i, [128, 16, 256], F32R) for i in range(2)])
    wds = Ring([p.sb("wds%d" % i, [128, D]) for i in range(1)])
    wdr = Ring([p.sb("wdr%d" % i, [128, D], F32R) for i in range(2)])
    h2r = p.sb("h2r", [128, 16, NM], F32R)
    hid = p.sb("hid", [128, 2, NM], F32R)
    tmp = Ring([p.sb("tmp%d" % i, [128, NM]) for i in range(1)])
    rstd = p.sb("rstd", [128, NM])
    gn_t = p.sb("gn_t", [128, 1])
    vec_t = p.sb("vec_t", [128, 4, 16])
    mod_t = p.sb("mod_t", [128, 4, NB4, 16])
    rw_t = p.sb("rw_t", [128, 16, NE])
    rb_t = p.sb("rb_t", [128, NE])
    gates = p.sb("gates", [128, 3, NEX])
    sc = p.sb("sc", [128, NE])
    sel = p.sb("sel", [128, NE])
    selm = p.sb("selm", [128, NE])
    m8 = p.sb("m8", [128, 8, 8])
    grp = p.sb("grp", [128, 8])
    g8 = p.sb("g8", [128, 8])
    gmask = p.sb("gmask", [128, 8])
    nm = p.sb("nm", [128, 8])
    t8 = p.sb("t8", [128, 8])
    den = p.sb("den", [128, 1])
    yb = bufB.re("p (c n) -> p c n", n=NM)
    acc = bufB.re("p (t d) -> p t d", d=D)

    p.memset('pool', ones[:, :], 1.0)
    p.memset('pool', gates[:, :, :], 1.0)
    par = p.sb("par", [128, 2])
    bs_t = p.sb("bs_t", [128, NB4, 2])
    tmpv = p.sb("tmpv", [128, 16])
    p.dma('sp', par[:, :], io.par[:, :])
    p.dma('sp', bs_t[:, :, :], io.bsel4[:, :, :])
    p.dma('sp', ident[:, :], io.cst[7, :, :])
    p.dma('sp', gn_t[:, :], sub(io.gn, l)[:, :])
    p.dma('sp', vec_t[:, :, :], sub(io.gvec, l)[:, :, :])
    p.dma('sp', rw_t[:, :, :], rw[:, :, :])
    p.dma('sp', rb_t[:, :], sub(io.rb, l)[:, :])
    for slot, vi in enumerate((2, 4, 3, 5)):
        blockvecs(p, sub(mod_t, (slice(None), slot)), modp, l, vi, bs_t, NB4, tmpv)
    for b in range(NB4):
        p.tt('dve', mod_t[:, 0, b, :], mod_t[:, 0, b, :], vec_t[:, 1, :], ALU.mult)
        p.stt('dve', mod_t[:, 1, b, :], mod_t[:, 1, b, :], 1.0, vec_t[:, 2, :], ALU.add, ALU.mult)
        p.tt('dve', mod_t[:, 3, b, :], mod_t[:, 3, b, :], vec_t[:, 3, :], ALU.mult)

    def router(ti):
        ps = pp.get()
        for kc in range(16):
            p.mm(ps[:, 0:NE], bufA[:, kc, ti * 128:(ti + 1) * 128], rw_t[:, kc, :], kc == 0, kc == 15)
        p.act(sc[:, :], ps[:, 0:NE], AF.Sigmoid)
        p.tt('dve', sel[:, :], sc[:, :], rb_t[:, :], ALU.add)
        for g in range(8):
            p.max8(m8[:, g, :], sel[:, g * 8:(g + 1) * 8])
        p.tt('dve', grp[:, :], m8[:, :, 0], m8[:, :, 1], ALU.add)
        p.max8(g8[:, :], grp[:, :])
        p.ts('dve', gmask[:, :], grp[:, :], g8[:, 3:4], None, ALU.is_ge)
        p.ts('dve', nm[:, :], gmask[:, :], -1.0, 1.0e30, ALU.add, ALU.mult)
        for g in range(8):
            p.ts('dve', selm[:, g * 8:(g + 1) * 8], sel[:, g * 8:(g + 1) * 8], gmask[:, g:g + 1], nm[:, g:g + 1],
                 ALU.mult, ALU.add)
        p.max8(t8[:, :], selm[:, :])
        p.ts('dve', selm[:, :], selm[:, :], t8[:, 7:8], None, ALU.is_ge)
        p.tt('dve', selm[:, :], selm[:, :], sc[:, :], ALU.mult)
        p.reduce('dve', den[:, :], selm[:, :], ALU.add)
        p.recip(den[:, :], den[:, :])
        p.ts('dve', gates[:, ti, 0:NE], selm[:, :], den[:, 0:1], ROUTED_SCALE, ALU.mult, ALU.mult)

    def do_block(bi, t0, N):
        NTL = N // 128
        ogb = wgs.get().re("p c n -> p (c n)").re("p (c n) -> p c n", n=512)
        zb8 = wgs.get().re("p c n -> p (c n)").re("p (c n) -> p c n", n=512)
        sq = yb
        w1 = yb
        p.dma('sp', bufA[:, 0:8, 0:N], attn[:, :, t0:t0 + N])
        for q in range(2):
            for r in range(2):
                p.dma('pool', ogb[q * 64:(q + 1) * 64, r * 4:(r + 1) * 4, 0:N], ogg[q, :, r, :, t0:t0 + N])
                p.dma('pool', w1[q * 64:(q + 1) * 64, 8 + r * 4:8 + (r + 1) * 4, 0:N], ogg[q, :, r, :, TOK + t0:TOK + t0 + N])
        blend(p, ogb[:, 0:8, 0:N], ogb[:, 0:8, 0:N], w1[:, 8:16, 0:N], par)
        p.dma('sp', zb8[:, 0:8, 0:N], zT[:, :, t0:t0 + N])
        p.dma('pool', xb[:, :, 0:N], xT[:, :, t0:t0 + N])
        p.act(sq[:, 0:8, 0:N], ogb[:, 0:8, 0:N], AF.Square)
        p.act(zb8[:, 0:8, 0:N], zb8[:, 0:8, 0:N], AF.Silu)
        for h in range(H):
            ps = pp.get()
            p.mm(ps[:, 0:N], ones[:, :], sq[:, h, 0:N])
            rstd_from_ps(p, ps, N, rstd, DV)
            p.stt('dve', bufA[:, 8 + h, 0:N], ogb[:, h, 0:N], gn_t[:, 0:1], rstd[:, 0:N], ALU.mult, ALU.mult)
            p.tt('dve', bufA[:, 8 + h, 0:N], bufA[:, 8 + h, 0:N], zb8[:, h, 0:N], ALU.mult)
        for m in range(16):
            wt_ = wds.get()
            wo = wt_.re("p (c n) -> p c n", n=128)
            p.dma(p.dq(), wo[:, 0:16, :], w_out[:, :, m * 128:(m + 1) * 128])
            ps = pp.get()
            for kc in range(16):
                p.mm(ps[:, 0:N], wo[:, kc, :], bufA[:, kc, 0:N], kc == 0, kc == 15)
            p.copy('act' if m % 2 == 0 else 'dve', yb[:, m, 0:N], ps[:, 0:N])
        rms_rstd(p, pp, yb, 0, 16, N, ones, bufA, rstd, D)
        for c in range(16):
            p.stt('dve', yb[:, c, 0:N], yb[:, c, 0:N], mod_t[:, 0, bi, c:c + 1], rstd[:, 0:N], ALU.mult, ALU.mult)
            p.tt('pool', xb[:, c, 0:N], xb[:, c, 0:N], yb[:, c, 0:N], ALU.add)
        rms_rstd(p, pp, xb, 0, 16, N, ones, bufA, rstd, D)
        for c in range(16):
            p.stt('dve', bufA[:, c, 0:N], xb[:, c, 0:N], mod_t[:, 1, bi, c:c + 1], rstd[:, 0:N], ALU.mult, ALU.mult)
            p.act(bufA[:, c, 0:N], bufA[:, c, 0:N], AF.Identity, bias=mod_t[:, 2, bi, c:c + 1])
        for ti in range(NTL):
            router(ti)
        p.copy('act', h2r[:, :, 0:N], bufA[:, :, 0:N])
        for e in range(nexp):
            for f in range(2):
                ws, wr = wgs.get(), wgr.get()
                p.dma('sp', ws[:, :, 0:128], wg[e, :, :, f * 128:(f + 1) * 128])
                p.dma('pool', ws[:, :, 128:256], wu[e, :, :, f * 128:(f + 1) * 128])
                p.copy('act', wr[:, :, :], ws[:, :, :])
                psg, psu = pp.get(), pp.get()
                for kc in range(16):
                    p.mm(psg[:, 0:N], wr[:, kc, 0:128], h2r[:, kc, 0:N], kc == 0, kc == 15)
                for kc in range(16):
                    p.mm(psu[:, 0:N], wr[:, kc, 128:256], h2r[:, kc, 0:N], kc == 0, kc == 15)
                tm = tmp.get()
                p.act(tm[:, 0:N], psg[:, 0:N], AF.Silu)
                p.tt('dve', hid[:, f, 0:N], tm[:, 0:N], psu[:, 0:N], ALU.mult)
            wdf = []
            for f in range(2):
                ds, dr = wds.get(), wdr.get()
                p.dma(p.dq(), ds[:, :], wd[e, :, f, :])
                p.copy('pool', dr[:, :], ds[:, :])
                wdf.append(dr)
            for ti in range(NTL):
                for n4 in range(4):
                    ps = pp.get()
                    for f in range(2):
                        p.mm(ps[:, :], hid[:, f, ti * 128:(ti + 1) * 128], wdf[f][:, n4 * 512:(n4 + 1) * 512],
                             f == 0, f == 1)
                    a = acc[:, ti, n4 * 512:(n4 + 1) * 512]
                    if e == 0:
                        p.ts('dve', a, ps[:, :], gates[:, ti, e:e + 1], None, ALU.mult)
                    else:
                        p.stt('dve', a, ps[:, :], gates[:, ti, e:e + 1], a, ALU.mult, ALU.add)
        for ti in range(NTL):
            for c in range(16):
                ps = pp.get()
                p.transpose(ps[:, 0:128], acc[:, ti, c * 128:(c + 1) * 128], ident[:, :])
                p.copy('act' if c % 2 == 0 else 'dve', bufA[:, c, ti * 128:(ti + 1) * 128], ps[:, 0:128])
        rms_rstd(p, pp, bufA, 0, 16, N, ones, yb, rstd, D)
        for c in range(16):
            p.stt('dve', bufA[:, c, 0:N], bufA[:, c, 0:N], mod_t[:, 3, bi, c:c + 1], rstd[:, 0:N], ALU.mult, ALU.mult)
            p.tt('pool', xb[:, c, 0:N], xb[:, c, 0:N], bufA[:, c, 0:N], ALU.add)
        p.dma('sp', o_x[:, :, t0:t0 + N], xb[:, :, 0:N])

    for bi, (t0, N) in enumerate(BLOCKS4):
        do_block(bi, t0, N)


def build_fused(depth=DEPTH, upto=9):
    nc = new_nc()
    p = Prog(nc)
    _CCBUF.b.w = None
    _CCBUF.b.r = {}
    io = declare_io(p, depth)
    modp = p.sbp("modp", [128, depth, 96, 2])
    stage_a(p, io, depth, modp)
    p.flush(final=(upto == 1))
    for l in range(depth):
        if upto < 2:
            break
        stage_s1(p, io, l, modp)
        p.flush(final=(upto == 2))
        if upto < 3:
            break
        gather_chunks(p, io.g1_s, io.g1_g, 128, NG1)
        gather_chunks(p, io.g2_s, io.g2_g, 128, NT)
        p.flush(final=(upto == 3))
        if upto < 4:
            break
        stage_s2(p, io)
        p.flush(final=(upto == 4))
        if upto < 5:
            break
        stage_s3(p, io, l)
        p.flush()
        gather_chunks(p, io.og_s, io.og_g, 64, NPAIR * 2)
        p.flush(final=(upto == 5))
        if upto < 6:
            break
        stage_s4(p, io, l, modp, l == depth - 1)
        p.flush(final=(l == depth - 1))
    p.close()
    return nc


def _fm(v, n):
    return np.ascontiguousarray(np.asarray(v, np.float32).reshape(n, 128).T)


def _gdn_consts():
    i = np.arange(128)
    same = (i[:, None] // 64) == (i[None, :] // 64)
    PE_f = ((i[:, None] <= i[None, :]) & same).astype(np.float32)
    PS_f = ((i[:, None] > i[None, :]) & same).astype(np.float32)
    BD = same.astype(np.float32)
    CH0 = np.broadcast_to((i < 64)[:, None], (128, 128)).astype(np.float32)
    CH1 = np.broadcast_to((i >= 64)[:, None], (128, 128)).astype(np.float32)
    return np.ascontiguousarray(np.stack([PE_f, PS_f, PE_f.T, PS_f.T, BD, CH0, CH1, np.eye(128, dtype=np.float32)], 0))


def _rope_tables():
    s = np.arange(SEQ)
    r = (s // GRID_W).astype(np.float32)
    col = (s % GRID_W).astype(np.float32)
    half = ROPE // 2
    inv = np.power(np.float32(10000.0), -np.arange(0, half, 2, dtype=np.float32) / np.float32(half)).astype(np.float32)
    ar = r[:, None] * inv
    ac = col[:, None] * inv
    ang = np.concatenate([ar, ar, ac, ac], -1).astype(np.float32)
    cos = np.concatenate([np.ones((CTX, ROPE), np.float32), np.cos(ang).astype(np.float32)], 0)
    sin = np.concatenate([np.zeros((CTX, ROPE), np.float32), np.sin(ang).astype(np.float32)], 0)
    return cos, sin


def _rot_mat():
    R = np.zeros((64, 64), np.float32)
    for i in range(16):
        R[16 + i, i] = -1
        R[i, 16 + i] = 1
        R[48 + i, 32 + i] = -1
        R[32 + i, 48 + i] = 1
    return R


_NC = {}


def kernel(x, c, ctx, c_ctx, ada_w, ada_b, norm_mix_pre, norm_mix_post, norm_ffn_pre, norm_ffn_post,
           w_in, mla_q_norm, mla_w_q_up, mla_kv_norm, mla_w_kv_up, gdn_conv, gdn_a_log, gdn_dt_bias,
           gdn_norm, w_out, router_w, router_bias, exp_w_gate, exp_w_up, exp_w_down,
           sh_w_gate, sh_w_up, sh_w_down, depth=DEPTH):
    f = np.float32
    A = lambda a: np.ascontiguousarray(np.asarray(a, dtype=f)[:depth])
    x, c, ctx, c_ctx = [np.asarray(a, dtype=f) for a in (x, c, ctx, c_ctx)]
    L = depth
    cos, sin = _rope_tables()
    islat = np.ones(BTOK, f)
    islat[:CTX] = 0
    shared = dict(
        dr_rmat=_rot_mat(), dr_kmask=np.ascontiguousarray((-MASK_BIG * islat)[None]), dr_cst=_gdn_consts(),
        dr_ada_w=A(ada_w), dr_ada_b=np.ascontiguousarray(A(ada_b).reshape(L, 96, 128).transpose(0, 2, 1)),
        dr_gvec=np.ascontiguousarray(np.stack([A(v).reshape(L, 16, 128).transpose(0, 2, 1) for v in
                                               (norm_mix_pre, norm_mix_post, norm_ffn_pre, norm_ffn_post)], 2)),
        dr_w_in=A(w_in),
        dr_gq=np.ascontiguousarray(A(mla_q_norm).reshape(L, 4, 128).transpose(0, 2, 1)),
        dr_gkv=np.ascontiguousarray(A(mla_kv_norm).reshape(L, 2, 128).transpose(0, 2, 1)),
        dr_gn=np.ascontiguousarray(A(gdn_norm).reshape(L, 128, 1)),
        dr_w_out=A(w_out), dr_rw=A(router_w),
        dr_rb=np.ascontiguousarray(np.broadcast_to(A(router_bias)[:, None, :], (L, 128, NE))),
        dr_wg=np.ascontiguousarray(np.concatenate([A(exp_w_gate), A(sh_w_gate)[:, None]], 1)),
        dr_wu=np.ascontiguousarray(np.concatenate([A(exp_w_up), A(sh_w_up)[:, None]], 1)),
        dr_wd=np.ascontiguousarray(np.concatenate([A(exp_w_down), A(sh_w_down)[:, None]], 1)),
    )
    wq = A(mla_w_q_up).reshape(L, QR, H, QK)
    shared["dr_wq"] = np.ascontiguousarray(np.concatenate([wq[..., :NOPE].reshape(L, QR, -1), wq[..., NOPE:].reshape(L, QR, -1)], 2))
    wkv = A(mla_w_kv_up).reshape(L, KVR, H, NOPE + VD)
    shared["dr_wkv"] = np.ascontiguousarray(np.concatenate([wkv[..., :NOPE].reshape(L, KVR, -1), wkv[..., NOPE:].reshape(L, KVR, -1)], 2))
    conv, alog, dtb = A(gdn_conv), A(gdn_a_log), A(gdn_dt_bias)
    per_par = []
    for hf in range(2):
        heads = [hf * NPAIR + i for i in range(NPAIR)]
        cw = np.stack([np.stack([np.stack([conv[l][:, w * 1024 + h * 128:w * 1024 + (h + 1) * 128].T for w in range(3)], 1)
                                 for h in heads], 0) for l in range(L)], 0)
        alog_g = np.zeros((L, 128, 16), f)
        dtb_g = np.zeros((L, 128, 16), f)
        for pi, h in enumerate(heads):
            for d in range(2):
                alog_g[:, :, pi * 4 + d * 2 + 1] = alog[:, d, h][:, None]
                dtb_g[:, :, pi * 4 + d * 2 + 1] = dtb[:, d, h][:, None]
        bsel1 = np.zeros((128, NB, 2), f)
        bsel4 = np.zeros((128, NB4, 2), f)
        bsel1[:, :, 1] = 1
        bsel4[:, :, 1] = 1
        if hf == 0:
            bsel1[:, 0, :] = (1, 0)
            bsel4[:, 0, :] = (1, 0)
        par = np.zeros((128, 2), f)
        par[:, hf] = 1
        qmask = np.zeros((1, TOK), f)
        if hf == 0:
            qmask[0, :CTX] = 1
        per_par.append(dict(dr_cw=np.ascontiguousarray(cw), dr_alog=alog_g, dr_dtb=dtb_g, dr_bsel1=bsel1, dr_bsel4=bsel4,
                            dr_par=par, dr_qmask=qmask,
                            dr_cosT=np.ascontiguousarray(cos[hf * TOK:(hf + 1) * TOK].T),
                            dr_sinT=np.ascontiguousarray(sin[hf * TOK:(hf + 1) * TOK].T)))
    in_maps = []
    for k in range(8):
        b, hf = k // 2, k % 2
        tok = np.concatenate([ctx[b], x[b, :TOK - CTX]], 0) if hf == 0 else x[b, TOK - CTX:]
        c2 = np.stack([c[b], c_ctx], 1)
        m = dict(shared)
        m.update(per_par[hf])
        m["dr_x0T"] = np.ascontiguousarray(tok.T)
        m["dr_cT2"] = np.ascontiguousarray(c2.reshape(16, 128, 2).transpose(1, 0, 2))
        in_maps.append(m)
    if depth not in _NC:
        _NC[depth] = build_fused(depth)
    res = run_bass_kernel_spmd(_NC[depth], in_maps, core_ids=list(range(8))).results
    out = np.empty((BATCH, SEQ, D), f)
    for b in range(BATCH):
        out[b, :TOK - CTX] = res[2 * b]["dr_o_x"][:, CTX:].T
        out[b, TOK - CTX:] = res[2 * b + 1]["dr_o_x"].T
    return out
```
